# Optimizing a Trainium2 kernel written in Bass

```python
import jax
import jax.numpy as jnp
from jax import lax
import numpy as np

D_MODEL = 2048
BATCH = 8
SEQ = 2048
DEPTH = 2

GRID_W = 64
CTX_LEN = 256
EPS = 1e-6
ROPE_THETA = 10000.0
Q_BLOCK = 128
HEAD_DIM = 128
N_Q_HEADS = 8
N_KV_HEADS = 2
GQA_GROUP = N_Q_HEADS // N_KV_HEADS
ATTN_WIDTH = N_Q_HEADS * HEAD_DIM
KV_WIDTH = N_KV_HEADS * HEAD_DIM
CONV_WIDTH = D_MODEL - ATTN_WIDTH
CONV_K = 31
IN0_SPLITS = [ATTN_WIDTH, ATTN_WIDTH + KV_WIDTH, ATTN_WIDTH + 2 * KV_WIDTH,
              ATTN_WIDTH + 2 * KV_WIDTH + CONV_WIDTH]
IN0_WIDTH = ATTN_WIDTH + 2 * KV_WIDTH + 2 * CONV_WIDTH
GQA_SCALE = HEAD_DIM ** -0.5
MLA_HEADS = 16
Q_LORA = 1536
KV_LORA = 512
NOPE_DIM = 128
ROPE_DIM = 64
V_DIM = 128
QK_DIM = NOPE_DIM + ROPE_DIM
MLA_SCALE = QK_DIM ** -0.5
N_EXPERTS = 16
D_EXPERT = 1024
EC_FACTOR = 2

kernel_name = 'hybrid_conv_gqa_mla_ec_moe_dit'


def rmsnorm(x, g):
    xf = x.astype(jnp.float32)
    y = xf * lax.rsqrt(jnp.mean(xf * xf, axis=-1, keepdims=True) + EPS)
    return (y * g.astype(jnp.float32)).astype(x.dtype)


def layernorm(x, g, b):
    xf = x.astype(jnp.float32)
    mu = jnp.mean(xf, axis=-1, keepdims=True)
    xc = xf - mu
    y = xc * lax.rsqrt(jnp.mean(xc * xc, axis=-1, keepdims=True) + EPS)
    return (y * g.astype(jnp.float32) + b.astype(jnp.float32)).astype(x.dtype)


def modulate(h, shift, scale):
    return h * (1 + scale) + shift


def ada_params(cond, w, b):
    return jnp.split(jax.nn.silu(cond) @ w + b, 6, axis=-1)


def axial_rope_tables(n_tokens, d_rot):
    rows = n_tokens // GRID_W
    row = jnp.repeat(jnp.arange(rows, dtype=jnp.float32), GRID_W)
    col = jnp.tile(jnp.arange(GRID_W, dtype=jnp.float32), rows)
    n_axis = d_rot // 4
    inv_freq = ROPE_THETA ** (-jnp.arange(n_axis, dtype=jnp.float32) / n_axis)
    ang = jnp.concatenate([row[:, None] * inv_freq, col[:, None] * inv_freq], axis=-1)
    return jnp.cos(ang), jnp.sin(ang)


def apply_rope(x, cos, sin):
    half = x.shape[-1] // 2
    x1, x2 = x[..., :half], x[..., half:]
    c = cos[None, :, None, :].astype(x.dtype)
    s = sin[None, :, None, :].astype(x.dtype)
    return jnp.concatenate([x1 * c - x2 * s, x1 * s + x2 * c], axis=-1)


def sdpa(q, k, v, scale):
    s = jnp.einsum('bqhgd,bkhd->bhgqk', q, k).astype(jnp.float32) * scale
    p = jax.nn.softmax(s, axis=-1).astype(v.dtype)
    return jnp.einsum('bhgqk,bkhd->bqhgd', p, v)


def attend_latent(q_lat, k_lat, v_lat, k_ctx, v_ctx, scale):
    b, s = q_lat.shape[:2]
    keys = jnp.concatenate([k_ctx, k_lat], axis=1)
    vals = jnp.concatenate([v_ctx, v_lat], axis=1)
    nb = s // Q_BLOCK
    qb = jnp.moveaxis(q_lat.reshape((b, nb, Q_BLOCK) + q_lat.shape[2:]), 1, 0)
    ob = lax.map(lambda qq: sdpa(qq, keys, vals, scale), qb)
    return jnp.moveaxis(ob, 0, 1).reshape(b, s, -1)


def depthwise_conv(x, w, b):
    y = lax.conv_general_dilated(x, w.astype(x.dtype), window_strides=(1,),
                                 padding=[(CONV_K // 2, CONV_K // 2)],
                                 dimension_numbers=('NWC', 'WIO', 'NWC'),
                                 feature_group_count=x.shape[-1])
    return y + b


def conformer_branch(u, dw_w, dw_b, ln_g, ln_b):
    return jax.nn.silu(layernorm(depthwise_conv(u, dw_w, dw_b), ln_g, ln_b))


def mixer_ab(h_lat, h_ctx, need_ctx, w_in, q_norm_g, k_norm_g, dw_w, dw_b, ln_g, ln_b, w_out):
    cos, sin = axial_rope_tables(h_lat.shape[1], HEAD_DIM)

    def project(h, rope):
        b, l, _ = h.shape
        q, k, v, u, gt = jnp.split(h @ w_in, IN0_SPLITS, axis=-1)
        q = rmsnorm(q.reshape(b, l, N_Q_HEADS, HEAD_DIM), q_norm_g)
        k = rmsnorm(k.reshape(b, l, N_KV_HEADS, HEAD_DIM), k_norm_g)
        v = v.reshape(b, l, N_KV_HEADS, HEAD_DIM)
        if rope:
            q = apply_rope(q, cos, sin)
            k = apply_rope(k, cos, sin)
        q = q.reshape(b, l, N_KV_HEADS, GQA_GROUP, HEAD_DIM)
        return q, k, v, u * jax.nn.sigmoid(gt)

    q_l, k_l, v_l, u_l = project(h_lat, True)
    q_c, k_c, v_c, u_c = project(h_ctx, False)
    a_lat = attend_latent(q_l, k_l, v_l, k_c, v_c, GQA_SCALE)
    c_lat = conformer_branch(u_l, dw_w, dw_b, ln_g, ln_b)
    out_lat = jnp.concatenate([a_lat, c_lat], axis=-1) @ w_out
    out_ctx = None
    if need_ctx:
        b, l = h_ctx.shape[:2]
        a_ctx = sdpa(q_c, k_c, v_c, GQA_SCALE).reshape(b, l, -1)
        c_ctx_br = conformer_branch(u_c, dw_w, dw_b, ln_g, ln_b)
        out_ctx = jnp.concatenate([a_ctx, c_ctx_br], axis=-1) @ w_out
    return out_lat, out_ctx


def mixer_mla(h_lat, h_ctx, need_ctx, w_dqkv, q_lora_g, w_uq, kv_lora_g, w_ukv, w_out):
    cos, sin = axial_rope_tables(h_lat.shape[1], ROPE_DIM)

    def queries(h, rope):
        b, l, _ = h.shape
        cq = rmsnorm(h @ w_dqkv[:, :Q_LORA], q_lora_g)
        q = (cq @ w_uq).reshape(b, l, MLA_HEADS, QK_DIM)
        q_nope, q_pe = q[..., :NOPE_DIM], q[..., NOPE_DIM:]
        if rope:
            q_pe = apply_rope(q_pe, cos, sin)
        return jnp.concatenate([q_nope, q_pe], axis=-1)[:, :, :, None, :]

    def keys_values(h, rope):
        b, l, _ = h.shape
        z = h @ w_dqkv[:, Q_LORA:]
        ckv, k_pe = z[..., :KV_LORA], z[..., KV_LORA:]
        kv = (rmsnorm(ckv, kv_lora_g) @ w_ukv).reshape(b, l, MLA_HEADS, NOPE_DIM + V_DIM)
        k_nope, v = kv[..., :NOPE_DIM], kv[..., NOPE_DIM:]
        k_pe = k_pe[:, :, None, :]
        if rope:
            k_pe = apply_rope(k_pe, cos, sin)
        k = jnp.concatenate([k_nope, jnp.broadcast_to(k_pe, (b, l, MLA_HEADS, ROPE_DIM))], axis=-1)
        return k, v

    k_l, v_l = keys_values(h_lat, True)
    k_c, v_c = keys_values(h_ctx, False)
    out_lat = attend_latent(queries(h_lat, True), k_l, v_l, k_c, v_c, MLA_SCALE) @ w_out
    out_ctx = None
    if need_ctx:
        b, l = h_ctx.shape[:2]
        out_ctx = sdpa(queries(h_ctx, False), k_c, v_c, MLA_SCALE).reshape(b, l, -1) @ w_out
    return out_lat, out_ctx


def ec_moe(h, router, w_gate, w_up, w_down):
    n, d = h.shape[1], h.shape[2]
    cap = EC_FACTOR * n // N_EXPERTS
    aff = jax.nn.softmax((h @ router).astype(jnp.float32), axis=-1)
    gate, idx = lax.top_k(jnp.swapaxes(aff, 1, 2), cap)
    xs = jax.vmap(lambda hb, ib: hb[ib])(h, idx)
    hid = jax.nn.silu(jnp.einsum('becd,edf->becf', xs, w_gate)) * jnp.einsum('becd,edf->becf', xs, w_up)
    out = jnp.einsum('becf,efd->becd', hid, w_down) * gate[..., None].astype(h.dtype)
    return jax.vmap(lambda ob, ib: jnp.zeros((n, d), h.dtype).at[ib.reshape(-1)].add(ob.reshape(-1, d)))(out, idx)


def setup_inputs(seed: int = 0) -> dict:
    key = jax.random.key(seed)
    ks = iter(jax.random.split(key, 64))
    f32 = jnp.float32

    def w(shape, fan_in, mult=1.0):
        return jax.random.normal(next(ks), shape, f32) * (mult * fan_in ** -0.5)

    def gain(shape):
        return 1.0 + 0.02 * jax.random.normal(next(ks), shape, f32)

    def bias(shape):
        return 0.01 * jax.random.normal(next(ks), shape, f32)

    D, E, F = D_MODEL, N_EXPERTS, D_EXPERT
    return {
        'x': jax.random.normal(next(ks), (BATCH, SEQ, D), f32),
        'c': jax.random.normal(next(ks), (BATCH, D), f32),
        'ctx': jax.random.normal(next(ks), (BATCH, CTX_LEN, D), f32),
        'c_ctx': jax.random.normal(next(ks), (D,), f32),
        'l0_mod_w': w((D, 6 * D), D, 0.5),
        'l0_mod_b': bias((6 * D,)),
        'l0_norm1_g': gain((D,)),
        'l0_w_in': w((D, IN0_WIDTH), D),
        'l0_q_norm_g': gain((HEAD_DIM,)),
        'l0_k_norm_g': gain((HEAD_DIM,)),
        'l0_dw_w': w((CONV_K, 1, CONV_WIDTH), CONV_K),
        'l0_dw_b': bias((CONV_WIDTH,)),
        'l0_conv_ln_g': gain((CONV_WIDTH,)),
        'l0_conv_ln_b': bias((CONV_WIDTH,)),
        'l0_w_out': w((D, D), D),
        'l0_norm2_g': gain((D,)),
        'l0_router': w((D, E), D),
        'l0_w_gate': w((E, D, F), D),
        'l0_w_up': w((E, D, F), D),
        'l0_w_down': w((E, F, D), F),
        'l1_mod_w': w((D, 6 * D), D, 0.5),
        'l1_mod_b': bias((6 * D,)),
        'l1_norm1_g': gain((D,)),
        'l1_w_dqkv': w((D, Q_LORA + KV_LORA + ROPE_DIM), D),
        'l1_q_lora_norm_g': gain((Q_LORA,)),
        'l1_w_uq': w((Q_LORA, MLA_HEADS * QK_DIM), Q_LORA),
        'l1_kv_lora_norm_g': gain((KV_LORA,)),
        'l1_w_ukv': w((KV_LORA, MLA_HEADS * (NOPE_DIM + V_DIM)), KV_LORA),
        'l1_w_out': w((MLA_HEADS * V_DIM, D), MLA_HEADS * V_DIM),
        'l1_norm2_g': gain((D,)),
        'l1_router': w((D, E), D),
        'l1_w_gate': w((E, D, F), D),
        'l1_w_up': w((E, D, F), D),
        'l1_w_down': w((E, F, D), F),
        'final_norm_g': gain((D,)),
    }


def reference(x, c, ctx, c_ctx,
              l0_mod_w, l0_mod_b, l0_norm1_g, l0_w_in, l0_q_norm_g, l0_k_norm_g, l0_dw_w, l0_dw_b,
              l0_conv_ln_g, l0_conv_ln_b, l0_w_out, l0_norm2_g, l0_router, l0_w_gate, l0_w_up, l0_w_down,
              l1_mod_w, l1_mod_b, l1_norm1_g, l1_w_dqkv, l1_q_lora_norm_g, l1_w_uq, l1_kv_lora_norm_g,
              l1_w_ukv, l1_w_out, l1_norm2_g, l1_router, l1_w_gate, l1_w_up, l1_w_down,
              final_norm_g):
    layers = [
        (mixer_ab, (l0_w_in, l0_q_norm_g, l0_k_norm_g, l0_dw_w, l0_dw_b, l0_conv_ln_g, l0_conv_ln_b, l0_w_out),
         (l0_mod_w, l0_mod_b, l0_norm1_g, l0_norm2_g, l0_router, l0_w_gate, l0_w_up, l0_w_down)),
        (mixer_mla, (l1_w_dqkv, l1_q_lora_norm_g, l1_w_uq, l1_kv_lora_norm_g, l1_w_ukv, l1_w_out),
         (l1_mod_w, l1_mod_b, l1_norm1_g, l1_norm2_g, l1_router, l1_w_gate, l1_w_up, l1_w_down)),
    ]
    xc = ctx
    for i in range(DEPTH):
        mixer, mix_p, (mod_w, mod_b, g1, g2, router, wg, wu, wd) = layers[i]
        need_ctx = i < DEPTH - 1
        sh1, sc1, ga1, sh2, sc2, ga2 = [m[:, None, :] for m in ada_params(c, mod_w, mod_b)]
        csh1, csc1, cga1, csh2, csc2, cga2 = ada_params(c_ctx, mod_w, mod_b)
        h_lat = modulate(rmsnorm(x, g1), sh1, sc1)
        h_ctx = modulate(rmsnorm(xc, g1), csh1, csc1)
        m_lat, m_ctx = mixer(h_lat, h_ctx, need_ctx, *mix_p)
        x = x + ga1 * m_lat
        x = x + ga2 * ec_moe(modulate(rmsnorm(x, g2), sh2, sc2), router, wg, wu, wd)
        if need_ctx:
            xc = xc + cga1 * m_ctx
            xc = xc + cga2 * ec_moe(modulate(rmsnorm(xc, g2), csh2, csc2), router, wg, wu, wd)
    return rmsnorm(x, final_norm_g)
```

```python
import numpy as np
from concourse.bass_utils import run_bass_kernel_spmd

import contextlib
import concourse.bass as bass
import concourse.mybir as mybir

F32 = mybir.dt.float32
BF16 = mybir.dt.bfloat16
I32 = mybir.dt.int32
U32 = mybir.dt.uint32
AF = mybir.ActivationFunctionType
ALU = mybir.AluOpType
AX = mybir.AxisListType

ENGS = ("pe", "dve", "act", "pool", "sp")
COMPUTE = ("pe", "dve", "act", "pool")


class Buf:
    __slots__ = ("name", "last_w", "readers")

    def __init__(self, name):
        self.name = name
        self.last_w = None
        self.readers = []


class Op:
    __slots__ = ("eng", "fn", "idx", "waits", "signal", "seq", "tag", "tagcnt")

    def __init__(self, eng, fn):
        self.eng = eng
        self.fn = fn
        self.waits = {}
        self.signal = False
        self.seq = None
        self.tag = None
        self.tagcnt = None


class Prog:
    def __init__(self, nc):
        self.nc = nc
        self.ops = {e: [] for e in ENGS}
        self.tags = {}
        self.known = {e: {} for e in ENGS}
        self.barrier_pending = {e: None for e in ENGS}
        self.nbuf = 0

    def buf(self, name=None):
        self.nbuf += 1
        return Buf(name or f"b{self.nbuf}")

    def bufs(self, n, name=None):
        return [self.buf(f"{name}{i}") for i in range(n)]

    def _need(self, op, dep, waw=False):
        if dep is None or dep is op:
            return
        if dep.tag is not None:
            tg = self.tags[dep.tag]
            val = tg[0] * 16
            if op.tag == dep.tag:
                if waw:
                    return
                val = (tg[0] - 1) * 16
            tg[1] = max(tg[1], val)
            key = ("t", dep.tag)
        else:
            if dep.eng == "pe" and op.eng == "pe" and op.tag is None:
                return
            dep.signal = True
            key = ("e", dep.eng)
            val = dep
        cur = op.waits.get(key)
        if cur is None:
            op.waits[key] = val
        elif key[0] == "t":
            op.waits[key] = max(cur, val)
        else:
            op.waits[key] = cur if cur.idx >= val.idx else val

    def _record(self, op, reads, writes):
        lst = self.ops[op.eng]
        op.idx = len(lst)
        lst.append(op)
        bp = self.barrier_pending[op.eng]
        if bp is not None:
            for d in bp[0]:
                self._need(op, d)
            for tag, val in bp[1].items():
                key = ("t", tag)
                op.waits[key] = max(op.waits.get(key, 0), val)
                self.tags[tag][1] = max(self.tags[tag][1], val)
            self.barrier_pending[op.eng] = None
        for b in reads:
            self._need(op, b.last_w)
        for b in writes:
            self._need(op, b.last_w, waw=True)
            for r in b.readers:
                self._need(op, r)
        for b in reads:
            b.readers.append(op)
        for b in writes:
            b.last_w = op
            b.readers = []

    def op(self, eng, fn, reads=(), writes=()):
        o = Op(eng, fn)
        self._record(o, reads, writes)
        return o

    def dma(self, q, tag, fn, reads=(), writes=(), after=()):
        o = Op(q, fn)
        tg = self.tags.setdefault(tag, [0, 0])
        tg[0] += 1
        o.tag = tag
        o.tagcnt = tg[0]
        for (t2, v2) in after:
            tg2 = self.tags.setdefault(t2, [0, 0])
            tg2[1] = max(tg2[1], v2)
            o.waits[("t", t2)] = max(o.waits.get(("t", t2), 0), v2)
        if tg[1] > 0:
            o.waits[("t", tag)] = max(o.waits.get(("t", tag), 0), tg[1])
        self._record(o, reads, writes)
        return o

    def barrier(self):
        lasts = []
        for e in COMPUTE:
            for o in reversed(self.ops[e]):
                if o.tag is None:
                    lasts.append(o)
                    break
        tagvals = {t: v[0] * 16 for t, v in self.tags.items() if v[0] > 0}
        for e in ENGS:
            self.barrier_pending[e] = (lasts, dict(tagvals))

    def emit(self, final_tags=()):
        nc = self.nc
        for e in COMPUTE:
            n = 0
            for o in self.ops[e]:
                if o.signal:
                    n += 1
                    o.seq = n
        with contextlib.ExitStack() as st:
            esem = {e: st.enter_context(nc.semaphore(f"s_{e}")) for e in COMPUTE}
            tsem = {t: st.enter_context(nc.semaphore(f"t_{i}")) for i, t in enumerate(self.tags)}
            block = st.enter_context(nc.Block())

            def run(ename, eng):
                known = {}
                for o in self.ops[ename]:
                    for key, val in o.waits.items():
                        if key[0] == "t":
                            sem, v = tsem[key[1]], val
                        else:
                            sem, v = esem[key[1]], val.seq
                        if known.get(key, 0) >= v:
                            continue
                        known[key] = v
                        eng.wait_ge(sem, v)
                    ins = o.fn(eng)
                    if o.tag is not None:
                        ins.then_inc(tsem[o.tag], 16)
                    elif o.signal:
                        ins.then_inc(esem[ename], 1)
                if ename == "sp":
                    for t in final_tags:
                        eng.wait_ge(tsem[t], self.tags[t][0] * 16)

            @block.sync
            def _(eng):
                run("sp", eng)

            @block.scalar
            def _(eng):
                run("act", eng)

            @block.vector
            def _(eng):
                run("dve", eng)

            @block.gpsimd
            def _(eng):
                run("pool", eng)

            @block.tensor
            def _(eng):
                run("pe", eng)

    def stats(self):
        return {e: len(self.ops[e]) for e in ENGS}, len(self.tags)

D = 2048
S = 2048
C = 256
T = S + C
NB = T // 128
EPS = 1e-6
SBUF_BYTES = 212480
_DS = {F32: 4, BF16: 2, I32: 4, U32: 4}


class Tl:
    __slots__ = ("ap", "b", "tag")

    def __init__(self, ap, b, tag):
        self.ap, self.b, self.tag = ap, b, tag

    def __getitem__(self, k):
        return self.ap[k]


class Ring:
    def __init__(self, tiles):
        self.t = tiles
        self.i = 0

    def next(self):
        t = self.t[self.i % len(self.t)]
        self.i += 1
        return t


class K:
    def __init__(self, stop=None, dumps=(), noinput=()):
        self.noinput = noinput
        nc = bass.Bass("TRN2", target_bir_lowering=False)
        self.nc = nc
        self.P = Prog(nc)
        self.stop = stop
        self.big = nc.alloc_sbuf_tensor("big", [128, SBUF_BYTES], mybir.dt.uint8)
        self.top = 0
        self.ntile = 0
        self.psb = [nc.alloc_psum_tensor(f"ps{i}", [128, 512], F32) for i in range(8)]
        self.pst = [Tl(self.psb[i][:, :], self.P.buf(f"ps{i}"), None) for i in range(8)]
        self.pr = Ring(self.pst[0:6])
        self.din = {}
        self.dumps = dumps

    def inp(self, name, shape, dtype=F32):
        t = self.nc.dram_tensor(name, list(shape), dtype, kind="Internal" if name in self.noinput else "ExternalInput")
        self.din[name] = t
        return t

    def scratch(self, name, shape, dtype):
        kind = "ExternalOutput" if name in self.dumps else "Internal"
        return self.nc.dram_tensor(name, list(shape), dtype, kind=kind)

    def tile(self, shape, dtype, tag=None):
        n = 1
        for s in shape[1:]:
            n *= s
        nbytes = (n * _DS[dtype] + 63) // 64 * 64
        off = self.top
        self.top += nbytes
        assert self.top <= SBUF_BYTES, f"SBUF overflow {self.top}"
        ap = self.big[:, off:off + n * _DS[dtype]].bitcast(dtype)
        if len(shape) == 3:
            ap = ap.rearrange("p (a b) -> p a b", a=shape[1])
        if shape[0] < 128:
            ap = ap[0:shape[0]]
        self.ntile += 1
        return Tl(ap, self.P.buf(f"t{self.ntile}"), tag)

    def ring(self, tag, shape, dtype, n):
        return Ring([self.tile(shape, dtype, f"{tag}{i}") for i in range(n)])

    def mark(self):
        return self.top

    def cv_start(self, l):
        Ld = self.L[l]
        for e in range(16):
            for key in ("wg", "wu", "wd"):
                if key in self.WB[l]:
                    self._cvq.append((Ld[key][e], self.WB[l][key][e]))

    def cv_issue(self, n=1):
        for _ in range(n):
            if not self._cvq:
                return
            src, dst = self._cvq.pop(0)
            self.P.dma("pool", "cv", lambda en, src=src, dst=dst: en.dma_start(
                out=dst.rearrange("(c p) f -> p c f", p=128), in_=src.rearrange("(c p) f -> p c f", p=128)))

    def bg_step(self, k=1):
        self._cvc = getattr(self, "_cvc", 0) + 1
        if self._cvc % self.cv_div == 0:
            self.cv_issue()
        g = getattr(self, "_bg", None)
        self._bgc = getattr(self, "_bgc", 0) + 1
        if self._bgc % getattr(self, "bg_div", 1) != 0:
            return
        for _ in range(k):
            if g is None:
                return
            try:
                next(g)
            except StopIteration:
                self._bg = g = None

    def bg_drain(self):
        while getattr(self, "_bg", None) is not None:
            self.bg_step()

    def release(self, m):
        self.top = m
        self.P.barrier()

    def pipeline(self, n, stages):
        ns = len(stages)
        for step in range(n + ns - 1):
            for si in reversed(range(ns)):
                i = step - si
                if 0 <= i < n:
                    stages[si](i)

    def dma(self, q, tag, out, in_, reads=(), writes=()):
        return self.P.dma(q, tag, lambda e: e.dma_start(out=out, in_=in_), reads=reads, writes=writes)

    def load(self, q, t, src, ap=None, reads=()):
        return self.dma(q, t.tag, t.ap if ap is None else ap, src, reads=reads, writes=[t.b])

    def store(self, q, t, dst, ap=None, extra_w=()):
        return self.dma(q, t.tag, dst, t.ap if ap is None else ap, reads=[t.b], writes=list(extra_w))

    def mm(self, ps, out, lhsT, rhs, start, stop, reads):
        return self.P.op("pe", lambda e: e.matmul(out, lhsT, rhs, start=start, stop=stop), reads=reads, writes=[ps.b])

    def act(self, out, in_, func, reads, writes, **kw):
        return self.P.op("act", lambda e: e.activation(out, in_, func, **kw), reads=reads, writes=writes)

    def tt(self, eng, out, a, b, op, reads, writes):
        return self.P.op(eng, lambda e: e.tensor_tensor(out, a, b, op), reads=reads, writes=writes)

    def stt(self, out, in0, scalar, in1, op0, op1, reads, writes):
        return self.P.op("dve", lambda e: e.scalar_tensor_tensor(out, in0, scalar, in1, op0, op1), reads=reads, writes=writes)

    def recip(self, out, in_, reads, writes):
        return self.P.op("dve", lambda e: e.reciprocal(out, in_), reads=reads, writes=writes)

    def copy(self, eng, out, in_, reads, writes):
        if eng == "act":
            return self.P.op("act", lambda e: e.copy(out, in_), reads=reads, writes=writes)
        return self.P.op(eng, lambda e: e.tensor_copy(out, in_), reads=reads, writes=writes)

    def build(self):
        nc = self.nc
        I = self.inp
        self.x_d = I("x", [S, D])
        self.ctx_d = I("ctx", [C, D])
        self.cT_d = I("cT", [128, 16, 2])
        self.cst_d = I("consts", [7, 128, 128])
        self.cs0_d = I("cs0", [2, 128, S])
        self.cs1_d = I("cs1", [2, 64, S])
        L = []
        for l in range(2):
            d = dict(mod_w=I(f"l{l}_mod_w", [D, 6 * D]), mod_b=I(f"l{l}_mod_b", [1, 6 * D]),
                     g1=I(f"l{l}_norm1_g", [1, D]), g2=I(f"l{l}_norm2_g", [1, D]),
                     router=I(f"l{l}_router", [D, 16]), wg=I(f"l{l}_w_gate", [16, D, 1024]),
                     wu=I(f"l{l}_w_up", [16, D, 1024]), wd=I(f"l{l}_w_down", [16, 1024, D]),
                     w_out=I(f"l{l}_w_out", [D, D]))
            L.append(d)
        L[0].update(w_in=I("l0_w_in", [D, 3584]), qg=I("l0_q_norm_g", [128, 1]), kg=I("l0_k_norm_g", [128, 1]),
                    dwT=I("l0_dwT", [128, 8, 31]), cvec=I("l0_cvec", [128, 3, 8]))
        L[1].update(w_dqkv=I("l1_w_dqkv", [D, 2112]), gq=I("l1_gq", [128, 12]), w_uq=I("l1_w_uq", [1536, 3072]),
                    gkv=I("l1_gkv", [128, 4]), w_ukv=I("l1_w_ukv", [512, 4096]))
        self.fg_d = I("final_norm_g", [1, D])
        self.L = L
        self.out_d = nc.dram_tensor("out", [S, D], F32, kind="ExternalOutput")
        sc = self.scratch
        self.XR = sc("XR", [T, D], F32)
        self.ADA = [sc(f"ADA{l}", [2, 6 * D], F32) for l in range(2)]
        self.QT = sc("QT", [1024, T], BF16)
        self.KT = sc("KT", [256, T], BF16)
        self.V0 = sc("V0", [T, 256], BF16)
        self.UG = sc("UG", [1024, T], BF16)
        self.AT = sc("AT", [D, T], BF16)
        self.H2 = sc("H2", [T, D], BF16)
        self.CQT = sc("CQT", [1536, S], BF16)
        self.CKVT = sc("CKVT", [512, T], BF16)
        self.KPET = sc("KPET", [64, T], BF16)
        self.QNT = sc("QNT", [2048, S], BF16)
        self.QPT = sc("QPT", [1024, S], BF16)
        self.KNT = sc("KNT", [2048, T], BF16)
        self.V1 = sc("V1", [T, 2048], BF16)
        self.WB = [dict(wg=sc("WG0", [16, D, 1024], BF16), wu=sc("WU0", [16, D, 1024], BF16)),
                   dict(wg=sc("WG1", [16, D, 1024], BF16), wu=sc("WU1", [16, D, 1024], BF16), wd=sc("WD1", [16, 1024, D], BF16))]
        self._cvq = []
        self.cv_div = 3

        cb = self.tile([128, 7, 128], BF16, "cb")
        self.load("pool", cb, self.cst_d.ap().rearrange("c p f -> p c f"))
        self.cb = cb
        self.identb, self.o128, self.o1024, self.ones1 = cb[:, 0, :], cb[:, 1, :], cb[:, 2, :], cb[:, 3, :]
        self.rot128, self.rot64 = cb[:, 4, :], cb[0:64, 5, 0:64]
        idf = self.tile([128, 128], F32, "idf")
        self.load("sp", idf, self.cst_d[0])
        self.idf = idf
        self.affT = self.tile([16, T], F32)
        ept = self.tile([128, 1], F32)
        self.P.op("dve", lambda e: e.memset(ept.ap, EPS), writes=[ept.b])
        self._eps = ept
        self.idxT = self.tile([128, 3, 16], I32)
        self.gateT = self.tile([128, 3, 16], F32)

        m = self.mark()
        r = self.ring("x", [128, D], F32, 3)
        for tb in range(NB):
            t = r.next()
            src = self.x_d[tb * 128:(tb + 1) * 128, :] if tb < 16 else self.ctx_d[(tb - 16) * 128:(tb - 15) * 128, :]
            self.load("sp", t, src)
            self.store("act", t, self.XR[tb * 128:(tb + 1) * 128, :])
        self.release(m)

        def ada_then_mix0():
            m_bg = self.mark()
            self.ada_setup()
            for _ in self.ada_gen([(0, n) for n in range(8)]):
                pass
            self.P.barrier()
            self._bg = self.ada_gen([(0, n) for n in range(8, 24)] + [(1, n) for n in range(24)])
            self.layer0_mixer(m_bg)

        steps = [
            ("mix0", ada_then_mix0), ("moe0", lambda: self.moe(0)),
            ("mix1", self.layer1_mixer), ("moe1", lambda: self.moe(1)),
        ]
        for name, fn in steps:
            fn()
            if self.stop == name:
                break
        self.final()
        self.P.emit(final_tags=["fin0", "fin1"])
        return nc

    def ada_setup(self):
        ct = self.tile([128, 16, 2], F32, "c")
        sct = self.tile([128, 16, 2], BF16)
        self.load("sp", ct, self.cT_d.ap())
        self.act(sct.ap, ct.ap, AF.Silu, [ct.b], [sct.b])
        self._ada = dict(sct=sct, wr=self.ring("aw", [128, 16, 512], BF16, 2),
                         br=self.ring("ab", [2, 512], F32, 2), orr=self.ring("ao", [2, 512], F32, 1))

    def ada_gen(self, jobs):
        A = self._ada
        sct = A["sct"]
        loaded = {}

        def issue(idx):
            l, n = jobs[idx]
            Ld = self.L[l]
            w = A["wr"].next()
            self.load("pool", w, Ld["mod_w"][:, n * 512:(n + 1) * 512].rearrange("(c p) n -> p c n", p=128))
            bt = A["br"].next()
            self.load("sp", bt, Ld["mod_b"][0:1, n * 512:(n + 1) * 512].partition_broadcast(2))
            loaded[idx] = (w, bt)
        issue(0)
        for idx in range(len(jobs)):
            if idx + 1 < len(jobs):
                issue(idx + 1)
            l, n = jobs[idx]
            w, bt = loaded.pop(idx)
            ps = self.pst[7]
            for kc in range(16):
                self.mm(ps, ps[0:2, :], sct[:, kc, :], w[:, kc, :], kc == 0, kc == 15, [sct.b, w.b])
            o = A["orr"].next()
            self.tt("dve", o.ap, ps[0:2, :], bt.ap, ALU.add, [ps.b, bt.b], [o.b])
            self.store("sp", o, self.ADA[l][:, n * 512:(n + 1) * 512])
            yield

    def norm(self, l, which, nblk, HT, HTb, write_tok, post=()):
        m = self.mark()
        Ld = self.L[l]
        o_sh, o_sc = (0, 2048) if which == 1 else (6144, 8192)
        t1r = self.ring("t1", [128, D], F32, 2)
        gt = t1r.t[0]
        self.load("sp", gt, Ld["g1" if which == 1 else "g2"].ap().partition_broadcast(128))
        mods = []
        for r in range(2 if nblk > 16 else 1):
            a = self.tile([128, D], F32, f"a{r}")
            sh = self.tile([128, D], F32, f"s{r}")
            self.load("sp", a, self.ADA[l][r:r + 1, o_sc:o_sc + D].partition_broadcast(128))
            self.load("sp", sh, self.ADA[l][r:r + 1, o_sh:o_sh + D].partition_broadcast(128))
            self.stt(a.ap, a.ap, 1.0, gt.ap, ALU.add, ALU.mult, [a.b, gt.b], [a.b])
            mods.append((a, sh))
        xr = self.ring("x", [128, D], F32, 3)
        hbr = self.ring("hb", [128, D], BF16, 2)
        smr = self.ring("sm", [128, 1], F32, 12)
        st = {}

        def s0(tb):
            x = xr.next()
            self.load("sp" if tb % 2 == 0 else "act", x, self.XR[tb * 128:(tb + 1) * 128, :])
            st[tb] = dict(x=x)

        def s1(tb):
            x = st[tb]["x"]
            ss, sq, rs = smr.next(), smr.next(), smr.next()
            jt = t1r.t[tb % 2]
            self.act(jt.ap, x.ap, AF.Square, [x.b], [jt.b, ss.b], scale=float(D ** -0.5), accum_out=ss.ap)
            self.act(sq.ap, ss.ap, AF.Sqrt, [ss.b], [sq.b], bias=EPS_AP(self), scale=1.0)
            self.recip(rs.ap, sq.ap, [sq.b], [rs.b])
            st[tb]["rs"] = rs

        def s2(tb):
            a, sh = mods[0 if tb < 16 else 1]
            x, rs = st[tb]["x"], st[tb]["rs"]
            t1 = t1r.t[tb % 2]
            self.stt(t1.ap, x.ap, rs.ap, a.ap, ALU.mult, ALU.mult, [x.b, rs.b, a.b], [t1.b])
            hb = hbr.next()
            self.tt("pool", hb.ap, t1.ap, sh.ap, ALU.add, [t1.b, sh.b], [hb.b])
            if write_tok:
                self.store("sp", hb, self.H2[tb * 128:(tb + 1) * 128, :])
            st[tb]["hb"] = hb

        def s3(tb):
            hb = st[tb]["hb"]
            for half in range(2):
                ps = self.pr.next()
                pv = ps.ap.bitcast(BF16)
                for j in range(8):
                    fc = half * 8 + j
                    self.P.op("pe", lambda e, pv=pv, j=j, fc=fc, hb=hb: e.transpose(pv[:, j * 128:(j + 1) * 128], hb[:, fc * 128:(fc + 1) * 128], self.identb),
                              reads=[hb.b, self.cb.b], writes=[ps.b])
                self.copy("act" if half == 0 else "dve", HT[:, half * 8:(half + 1) * 8, tb * 128:(tb + 1) * 128],
                          pv.rearrange("p (c t) -> p c t", c=8), [ps.b], [HTb[tb]])
            self.bg_step()
        self.pipeline(nblk, [s0, s1, s2, s3] + list(post))
        self.release(m)

    def alloc_HT(self):
        HT = self.tile([128, 16, T], BF16)
        HTb = [self.P.buf(f"HT{i}") for i in range(NB)]
        return HT.ap, HTb

    def proj_fm(self, w, c0, mp, src, srcb, nk, t0, n):
        ps = self.pr.next()
        for kc in range(nk):
            self.mm(ps, ps[0:mp, 0:n], w[:, kc, c0:c0 + mp], src[:, kc, t0:t0 + n], kc == 0, kc == nk - 1, [w.b] + srcb)
        return ps

    def rope_tail(self, qn, mp, n, t0, cs, rot, dst, rings, is_ctx=False):
        if is_ctx:
            self.store("sp", qn, dst, ap=qn[0:mp, 0:n])
            return
        ps3 = self.pr.next()
        self.mm(ps3, ps3[0:mp, 0:n], rot, qn[0:mp, 0:n], True, True, [qn.b, self.cb.b])
        t1, t2, qo = rings["t1"].next(), rings["t2"].next(), rings["qo"].next()
        self.tt("pool", t1[0:mp, 0:n], qn[0:mp, 0:n], cs[0:mp, 0, t0:t0 + n], ALU.mult, [qn.b, cs.b], [t1.b])
        self.tt("dve", t2[0:mp, 0:n], ps3[0:mp, 0:n], cs[0:mp, 1, t0:t0 + n], ALU.mult, [ps3.b, cs.b], [t2.b])
        self.tt("pool", qo[0:mp, 0:n], t1[0:mp, 0:n], t2[0:mp, 0:n], ALU.add, [t1.b, t2.b], [qo.b])
        self.store("sp", qo, dst, ap=qo[0:mp, 0:n])

    def rope_rings(self):
        return dict(qn=self.ring("qn", [128, 512], BF16, 3), t1=self.ring("rt1", [128, 512], F32, 2),
                    t2=self.ring("rt2", [128, 512], F32, 2), qo=self.ring("qo", [128, 512], BF16, 2))

    def layer0_mixer(self, m_bg):
        Ld = self.L[0]
        m0 = self.mark()
        HT, HTb = self.alloc_HT()
        self.bg_div = 3
        self.norm(0, 1, NB, HT, HTb, False)
        self.bg_div = 3
        m = self.mark()
        gcol = self.tile([128, 2], F32, "gc")
        self.load("sp", gcol, Ld["qg"].ap(), ap=gcol[:, 0:1])
        self.load("sp", gcol, Ld["kg"].ap(), ap=gcol[:, 1:2])
        cs = self.tile([128, 2, S], BF16, "cs")
        self.load("pool", cs, self.cs0_d.ap().rearrange("c p t -> p c t"))
        wr = self.ring("w", [128, 16, 512], BF16, 3)
        rr = self.rope_rings()
        sqr = self.ring("sq", [128, 512], BF16, 3)
        sdr = self.ring("sd", [128, 512], F32, 1)
        rsr = self.ring("rs", [128, 512], F32, 2)
        TG = [(tg * 512, 512) for tg in range(4)] + [(2048, 256)]

        def hb_of(t0, n):
            return HTb[t0 // 128:(t0 + n) // 128]

        def loadw(p):
            w = wr.next()
            self.load("pool", w, Ld["w_in"][:, p * 512:(p + 1) * 512].rearrange("(c p) n -> p c n", p=128))
            return w

        items = []
        for p in range(2):
            for j in range(4):
                h = p * 4 + j
                for (t0, n) in TG:
                    items.append((p, j, gcol[:, 0:1], self.QT[h * 128:(h + 1) * 128, :], t0, n))
        for j in range(2):
            for (t0, n) in TG:
                items.append((2, j, gcol[:, 1:2], self.KT[j * 128:(j + 1) * 128, :], t0, n))
        wcur = {}

        def getw(p):
            if p not in wcur:
                wcur[p] = loadw(p)
            return wcur[p]
        stq = {}

        def qA(i):
            p, j, g_ap, dst, t0, n = items[i]
            w = getw(p)
            if j == 0 and t0 == 0 and p < 2:
                getw(p + 1)
            ps = self.proj_fm(w, j * 128, 128, HT, hb_of(t0, n), 16, t0, n)
            sq = sqr.next()
            self.act(sq[:, 0:n], ps[:, 0:n], AF.Square, [ps.b], [sq.b])
            stq[i] = [ps, sq]

        def qB(i):
            p, j, g_ap, dst, t0, n = items[i]
            ps, sq = stq[i]
            ps2 = self.pr.next()
            self.mm(ps2, ps2[:, 0:n], self.o128, sq[:, 0:n], True, True, [sq.b, self.cb.b])
            sd = sdr.next()
            self.act(sd[:, 0:n], ps2[:, 0:n], AF.Sqrt, [ps2.b], [sd.b], bias=EPS_AP(self), scale=1.0)
            rs = rsr.next()
            self.recip(rs[:, 0:n], sd[:, 0:n], [sd.b], [rs.b])
            qn = rr["qn"].next()
            self.stt(qn[:, 0:n], ps[:, 0:n], g_ap, rs[:, 0:n], ALU.mult, ALU.mult, [ps.b, rs.b, gcol.b], [qn.b])
            stq[i] = qn

        def qC(i):
            p, j, g_ap, dst, t0, n = items[i]
            self.rope_tail(stq.pop(i), 128, n, t0, cs, self.rot128, dst[:, t0:t0 + n], rr, is_ctx=(t0 >= S))
            self.bg_step()
        self.pipeline(len(items), [qA, qB, qC])
        w = getw(2)
        wun, wgn = loadw(3), loadw(5)
        vr = self.ring("v", [128, 256], BF16, 2)
        for tb in range(NB):
            ps = self.pr.next()
            for kc in range(16):
                self.mm(ps, ps[:, 0:256], HT[:, kc, tb * 128:(tb + 1) * 128], w[:, kc, 256:512], kc == 0, kc == 15, [w.b, HTb[tb]])
            v = vr.next()
            self.copy("act", v.ap, ps[:, 0:256], [ps.b], [v.b])
            self.store("sp", v, self.V0[tb * 128:(tb + 1) * 128, :])
        sgr = self.ring("sg", [128, 512], BF16, 2)
        ugr = self.ring("ug", [128, 512], BF16, 2)
        for p in range(2):
            if p == 1:
                wgn = loadw(6)
            wu, wg = wun, wgn
            if p == 0:
                wun = loadw(4)
            for j in range(4):
                c = p * 4 + j
                for (t0, n) in TG:
                    self.bg_step()
                    psu = self.proj_fm(wu, j * 128, 128, HT, hb_of(t0, n), 16, t0, n)
                    psg = self.proj_fm(wg, j * 128, 128, HT, hb_of(t0, n), 16, t0, n)
                    sg = sgr.next()
                    self.act(sg[:, 0:n], psg[:, 0:n], AF.Sigmoid, [psg.b], [sg.b])
                    ug = ugr.next()
                    self.tt("dve", ug[:, 0:n], psu[:, 0:n], sg[:, 0:n], ALU.mult, [psu.b, sg.b], [ug.b])
                    self.store("sp", ug, self.UG[c * 128:(c + 1) * 128, t0:t0 + n], ap=ug[:, 0:n])
        self.release(m0)
        self.bg_div = 1
        self.cv_start(0)
        self.attention(128 ** -0.5, 8, lambda h: (h // 4, self.KT[(h // 4) * 128:(h // 4 + 1) * 128, :], None),
                       lambda h: self.V0[:, (h // 4) * 128:(h // 4 + 1) * 128],
                       lambda h: (self.QT[h * 128:(h + 1) * 128, :], None), True, T)
        self.bg_drain()
        self.release(m_bg)
        if self.stop == "attn0":
            return
        self.conv0()
        self.outproj(0, 5)

    def attention(self, scale, nheads, k_src, v_src, q_src, with_ctx_q, qlen):
        m = self.mark()
        ktr = self.ring("kt", [128, T], BF16, 2)
        kpr = self.ring("kp", [64, T], BF16, 1)
        vr = self.ring("vv", [128, NB, 128], BF16, 2)
        qtr = self.ring("qt", [128, qlen], BF16, 2)
        qpr = self.ring("qp", [64, qlen], BF16, 2)
        ptr_ = self.ring("pT", [128, 512], BF16, 4)
        rsr = self.ring("rs", [128, 512], F32, 2)
        orr = self.ring("o", [128, 512], BF16, 2)
        Sr = Ring(self.pst[0:3])
        Or = Ring(self.pst[3:5])
        Ur = Ring(self.pst[5:7])
        groups = [(qg * 512, 512, list(range(NB))) for qg in range(4)]
        if with_ctx_q:
            groups.append((2048, 256, [16, 17]))
        kp = None
        last_k = None
        for h in range(nheads):
            kkey, ks, kps = k_src(h)
            if kkey != last_k:
                kt = ktr.next()
                self.load("sp", kt, ks)
                v = vr.next()
                self.load("act", v, v_src(h).rearrange("(b p) d -> p b d", p=128))
                last_k = kkey
            if kps is not None and kp is None:
                kp = kpr.next()
                self.load("sp", kp, kps)
            qs, qps = q_src(h)
            qt = qtr.next()
            self.load("sp", qt, qs)
            qp = None
            if qps is not None:
                qp = qpr.next()
                self.load("act", qp, qps)
            for (q0, n, kbs) in groups:
                self.bg_step()
                pso, psu = Or.next(), Ur.next()

                def smm(kb):
                    s = Sr.next()
                    rd = [kt.b, qt.b]
                    self.mm(s, s[:, 0:n], kt[:, kb * 128:(kb + 1) * 128], qt[:, q0:q0 + n], True, qp is None, rd)
                    if qp is not None:
                        self.mm(s, s[:, 0:n], kp[:, kb * 128:(kb + 1) * 128], qp[:, q0:q0 + n], False, True, [kp.b, qp.b])
                    return s
                pend = [smm(kb) for kb in kbs[:2]]
                for i, kb in enumerate(kbs):
                    s = pend.pop(0)
                    p = ptr_.next()
                    self.act(p[:, 0:n], s[:, 0:n], AF.Exp, [s.b], [p.b], scale=float(scale))
                    self.mm(pso, pso[:, 0:n], v[:, kb, :], p[:, 0:n], i == 0, i == len(kbs) - 1, [v.b, p.b])
                    self.mm(psu, psu[:, 0:n], self.ones1, p[:, 0:n], i == 0, i == len(kbs) - 1, [p.b, self.cb.b])
                    if i + 2 < len(kbs):
                        pend.append(smm(kbs[i + 2]))
                r = rsr.next()
                self.recip(r[:, 0:n], psu[:, 0:n], [psu.b], [r.b])
                o = orr.next()
                self.tt("dve", o[:, 0:n], pso[:, 0:n], r[:, 0:n], ALU.mult, [pso.b, r.b], [o.b])
                self.store("sp", o, self.AT[h * 128:(h + 1) * 128, q0:q0 + n], ap=o[:, 0:n])
        self.release(m)

    def conv0(self):
        Ld = self.L[0]
        m = self.mark()
        PW = 15 + S + 15 + 15 + C + 15
        CS0 = S + 30
        ugt = self.tile([128, 8, PW], BF16, "ugt")
        zb = self.P.buf("ugt_zero")
        self.P.op("pool", lambda e: e.memset(ugt.ap, 0.0), writes=[ugt.b, zb])
        for c in range(8):
            self.load("sp", ugt, self.UG[c * 128:(c + 1) * 128, 0:S], ap=ugt[:, c, 15:15 + S], reads=[zb])
            self.load("act", ugt, self.UG[c * 128:(c + 1) * 128, S:T], ap=ugt[:, c, CS0 + 15:CS0 + 15 + C], reads=[zb])
        dwt = self.tile([128, 8, 31], F32, "dwt")
        self.load("sp", dwt, Ld["dwT"].ap())
        cv = self.tile([128, 3, 8], F32, "cv")
        self.load("sp", cv, Ld["cvec"].ap())
        Dm = self.tile([128, 248, 128], BF16)
        for c in range(8):
            self.P.op("dve", lambda e, c=c: e.tensor_tensor(
                Dm[:, c * 31:(c + 1) * 31, :], self.identb.unsqueeze(1).to_broadcast([128, 31, 128]),
                dwt[:, c, :].unsqueeze(2).to_broadcast([128, 31, 128]), ALU.mult),
                reads=[self.cb.b, dwt.b], writes=[Dm.b])
        Yr = self.ring("Y", [128, 8, 512], F32, 2)
        ybr = self.ring("yb", [128, 512], BF16, 2)
        yqr = self.ring("yq", [128, 512], BF16, 2)
        st = [self.tile([128, 512], F32) for _ in range(5)]
        zr = self.ring("z", [128, 512], F32, 2)
        z2r = self.ring("z2", [128, 512], F32, 2)
        cor = self.ring("co", [128, 512], BF16, 2)
        Cr = Ring(self.pst[0:4])
        TG = [(tg * 512, 512, tg * 512) for tg in range(4)] + [(2048, 256, CS0)]
        for (t0, n, off) in TG:
            Y = Yr.next()
            s1, s2 = self.pst[4], self.pst[5]
            pend = None
            for c in range(8):
                self.bg_step()
                ps = Cr.next()
                for k in range(31):
                    self.mm(ps, ps[:, 0:n], Dm[:, c * 31 + k, :], ugt[:, c, off + k:off + k + n], k == 0, k == 30, [Dm.b, ugt.b])
                if pend is not None:
                    pc, pyb, pyq = pend
                    self.mm(s1, s1[:, 0:n], self.o1024, pyb[:, 0:n], pc == 0, False, [pyb.b, self.cb.b])
                    self.mm(s2, s2[:, 0:n], self.o1024, pyq[:, 0:n], pc == 0, False, [pyq.b, self.cb.b])
                self.act(Y[:, c, 0:n], ps[:, 0:n], AF.Identity, [ps.b, cv.b], [Y.b], bias=cv[:, 0, c:c + 1], scale=1.0)
                yb, yq = ybr.next(), yqr.next()
                self.copy("pool", yb[:, 0:n], Y[:, c, 0:n], [Y.b], [yb.b])
                self.tt("dve", yq[:, 0:n], Y[:, c, 0:n], Y[:, c, 0:n], ALU.mult, [Y.b], [yq.b])
                pend = (c, yb, yq)
            pc, pyb, pyq = pend
            self.mm(s1, s1[:, 0:n], self.o1024, pyb[:, 0:n], False, True, [pyb.b, self.cb.b])
            self.mm(s2, s2[:, 0:n], self.o1024, pyq[:, 0:n], False, True, [pyq.b, self.cb.b])
            mu, msq, var, rstd, nmr = st
            self.copy("act", mu[:, 0:n], s1[:, 0:n], [s1.b], [mu.b])
            self.tt("dve", msq[:, 0:n], mu[:, 0:n], mu[:, 0:n], ALU.mult, [mu.b], [msq.b])
            self.tt("dve", var[:, 0:n], s2[:, 0:n], msq[:, 0:n], ALU.subtract, [s2.b, msq.b], [var.b])
            self.act(msq[:, 0:n], var[:, 0:n], AF.Sqrt, [var.b], [msq.b], bias=EPS_AP(self), scale=1.0)
            self.recip(rstd[:, 0:n], msq[:, 0:n], [msq.b], [rstd.b])
            self.stt(nmr[:, 0:n], mu[:, 0:n], -1.0, rstd[:, 0:n], ALU.mult, ALU.mult, [mu.b, rstd.b], [nmr.b])
            for c in range(8):
                z, z2, co = zr.next(), z2r.next(), cor.next()
                self.tt("dve", z[:, 0:n], Y[:, c, 0:n], rstd[:, 0:n], ALU.mult, [Y.b, rstd.b], [z.b])
                self.tt("pool", z2[:, 0:n], z[:, 0:n], nmr[:, 0:n], ALU.add, [z.b, nmr.b], [z2.b])
                self.act(co[:, 0:n], z2[:, 0:n], AF.Silu, [z2.b, cv.b], [co.b], scale=cv[:, 1, c:c + 1], bias=cv[:, 2, c:c + 1])
                self.store("sp", co, self.AT[1024 + c * 128:1024 + (c + 1) * 128, t0:t0 + n], ap=co[:, 0:n])
        self.release(m)

    def outproj(self, l, ngroups):
        Ld = self.L[l]
        m = self.mark()
        W = self.tile([128, 16, D], BF16, "W")
        for p in range(4):
            self.load("pool", W, Ld["w_out"][:, p * 512:(p + 1) * 512].rearrange("(c p) n -> p c n", p=128), ap=W[:, :, p * 512:(p + 1) * 512])
        gas = []
        for r in range(2 if ngroups > 4 else 1):
            ga = self.tile([128, D], F32, f"ga{r}")
            self.load("sp", ga, self.ADA[l][r:r + 1, 4096:6144].partition_broadcast(128))
            gas.append(ga)
        atr = self.ring("at", [128, 16, 512], BF16, 2)
        xr = self.ring("x", [128, D], F32, 3)
        tr = self.ring("tm", [128, 512], F32, 3)
        for tg in range(ngroups):
            t0, n = tg * 512, (512 if tg < 4 else 256)
            ga = gas[0 if tg < 4 else 1]
            at = atr.next()
            self.load("sp", at, self.AT[:, t0:t0 + n].rearrange("(c p) t -> p c t", p=128), ap=at[:, :, 0:n])
            for bi in range(n // 128):
                tb = tg * 4 + bi
                self.bg_step()
                x = xr.next()
                self.load("act", x, self.XR[tb * 128:(tb + 1) * 128, :])
                for dc in range(4):
                    ps = self.pr.next()
                    for fc in range(16):
                        self.mm(ps, ps.ap, at[:, fc, bi * 128:(bi + 1) * 128], W[:, fc, dc * 512:(dc + 1) * 512], fc == 0, fc == 15, [at.b, W.b])
                    tm = tr.next()
                    self.tt("dve", tm.ap, ps.ap, ga[:, dc * 512:(dc + 1) * 512], ALU.mult, [ps.b, ga.b], [tm.b])
                    self.tt("pool", x[:, dc * 512:(dc + 1) * 512], x[:, dc * 512:(dc + 1) * 512], tm.ap, ALU.add, [x.b, tm.b], [x.b])
                self.store("sp", x, self.XR[tb * 128:(tb + 1) * 128, :])
        self.release(m)

    def moe(self, l):
        Ld = self.L[l]
        nblk = NB if l == 0 else 16
        nj = 288 if l == 0 else 256
        JB = [(0, 128), (128, 128), (256, 32)][:3 if l == 0 else 2]
        m0 = self.mark()
        HT, HTb = self.alloc_HT()
        rw = self.tile([128, 16, 16], BF16, "rw")
        self.load("pool", rw, Ld["router"].ap().rearrange("(c p) e -> p c e", p=128))
        smr = self.ring("sm2", [128, 1], F32, 12)
        exr = self.ring("ex", [128, 16], F32, 3)
        afr = self.ring("af", [128, 16], F32, 3)
        str_ = {}

        def rA(tb):
            ps = self.pr.next()
            for kc in range(16):
                self.mm(ps, ps[:, 0:16], HT[:, kc, tb * 128:(tb + 1) * 128], rw[:, kc, :], kc == 0, kc == 15, [rw.b, HTb[tb]])
            mx, nmx, ssum, rr = smr.next(), smr.next(), smr.next(), smr.next()
            self.P.op("dve", lambda e, mx=mx, ps=ps: e.reduce_max(mx.ap, ps[:, 0:16], AX.X), reads=[ps.b], writes=[mx.b])
            self.P.op("dve", lambda e, mx=mx, nmx=nmx: e.tensor_scalar(nmx.ap, mx.ap, -1.0, None, ALU.mult), reads=[mx.b], writes=[nmx.b])
            ex = exr.next()
            self.act(ex.ap, ps[:, 0:16], AF.Exp, [ps.b, nmx.b], [ex.b, ssum.b], bias=nmx.ap, scale=1.0, accum_out=ssum.ap)
            self.recip(rr.ap, ssum.ap, [ssum.b], [rr.b])
            af = afr.next()
            self.P.op("dve", lambda e, af=af, ex=ex, rr=rr: e.tensor_scalar(af.ap, ex.ap, rr.ap, None, ALU.mult), reads=[ex.b, rr.b], writes=[af.b])
            str_[tb] = af

        def rB(tb):
            af = str_.pop(tb)
            ps2 = self.pr.next()
            self.P.op("pe", lambda e, ps2=ps2, af=af: e.transpose(ps2[0:16, 0:128], af.ap, self.idf.ap), reads=[af.b, self.idf.b], writes=[ps2.b])
            self.copy("act", self.affT[:, tb * 128:(tb + 1) * 128], ps2[0:16, 0:128], [ps2.b], [self.affT.b])
        self.norm(l, 2, nblk, HT, HTb, True, post=[rA, rB])
        self.cv_issue(len(self._cvq))
        self.release(m0)
        m = self.mark()
        gas = [self.tile([128, D], BF16) for r in range(2 if l == 0 else 1)]
        slots = self.ring("ws", [128, 16 * 1024], BF16, 4)
        xsr = self.ring("xs", [128, D], BF16, len(JB))
        xtr = self.ring("xT", [128, 16, 288], BF16, 1)
        hdr = self.ring("hd", [128, 8, 288], BF16, 1)
        sgr = self.ring("sg", [128, 288], BF16, 2)
        yr = self.ring("y", [128, D], F32, 3)
        xrb = self.P.buf("xr_scatter")
        pending = []
        for r, ga in enumerate(gas):
            stg = yr.t[1 + r]
            self.load("sp", stg, self.ADA[l][r:r + 1, 10240:12288].partition_broadcast(128))
            self.copy("dve", ga.ap, stg.ap, [stg.b], [ga.b])

        def gather(e):
            out = []
            for jb, (j0, n) in enumerate(JB):
                xs = xsr.next()
                self.P.dma("pool", xs.tag, lambda en, xs=xs, n=n, jb=jb, e=e: en.indirect_dma_start(
                    out=xs[0:n, :], out_offset=None, in_=self.H2.ap(),
                    in_offset=bass.IndirectOffsetOnAxis(ap=self.idxT[0:n, jb, e:e + 1], axis=0)),
                    reads=[self.idxT.b], writes=[xs.b])
                out.append(xs)
            return out

        wpieces = {}
        wstate = [0]

        def ensure(k):
            while wstate[0] <= k and wstate[0] < 48:
                kk = wstate[0]
                e, which = divmod(kk, 3)
                sl = slots.next()
                key = ("wg", "wu", "wd")[which]
                pre = key in self.WB[l]
                src = (self.WB[l][key] if pre else Ld[key])[e]
                q = "sp" if pre else "pool"
                if which < 2:
                    self.load(q, sl, src.rearrange("(c p) f -> p c f", p=128), ap=sl.ap.rearrange("p (c f) -> p c f", c=16))
                else:
                    self.load(q, sl, src.rearrange("(c p) d -> p c d", p=128), ap=sl.ap.rearrange("p (c d) -> p c d", c=8))
                wpieces[kk] = sl
                wstate[0] += 1

        ensure(1)
        m2 = self.mark()
        work = Tl(yr.t[0].ap[0:16, :], yr.t[0].b, None)
        work2 = self.tile([16, 288], F32)
        vals = self.tile([16, 288], F32)
        idx = self.tile([16, 288], U32)
        idxf = work2

        def topk(src0, wk, rounds, o0):
            for r in range(rounds):
                src = src0 if r == 0 else wk.ap
                vs = vals[:, o0 + r * 8:o0 + (r + 1) * 8]
                ix = idx[:, o0 + r * 8:o0 + (r + 1) * 8]
                rd = [self.affT.b, wk.b]
                self.P.op("dve", lambda e, vs=vs, src=src: e.max(out=vs, in_=src), reads=rd, writes=[vals.b])
                self.P.op("dve", lambda e, vs=vs, ix=ix, src=src: e.max_index(out=ix, in_max=vs, in_values=src), reads=rd + [vals.b], writes=[idx.b])
                if r < rounds - 1:
                    self.P.op("dve", lambda e, vs=vs, src=src, wk=wk: e.match_replace(out=wk.ap, in_to_replace=vs, in_values=src, imm_value=-1.0),
                              reads=rd + [vals.b], writes=[wk.b])
        topk(self.affT[:, 0:S], work, 32, 0)
        if l == 0:
            topk(self.affT[:, S:T], Tl(work2.ap[:, 0:C], work2.b, None), 4, 256)
        self.copy("dve", idxf[:, 0:nj], idx[:, 0:nj], [idx.b], [idxf.b])
        if l == 0:
            self.P.op("dve", lambda e: e.tensor_scalar(idxf[:, 256:288], idxf[:, 256:288], float(S), None, ALU.add), reads=[idxf.b], writes=[idxf.b])
        for jb, (j0, n) in enumerate(JB):
            ps = self.pr.next()
            self.P.op("pe", lambda e, ps=ps, j0=j0, n=n: e.transpose(ps[0:n, 0:16], idxf[:, j0:j0 + n], self.idf[0:16, 0:16]), reads=[idxf.b, self.idf.b], writes=[ps.b])
            self.copy("dve", self.idxT[0:n, jb, :], ps[0:n, 0:16], [ps.b], [self.idxT.b])
            ps = self.pr.next()
            self.P.op("pe", lambda e, ps=ps, j0=j0, n=n: e.transpose(ps[0:n, 0:16], vals[:, j0:j0 + n], self.idf[0:16, 0:16]), reads=[vals.b, self.idf.b], writes=[ps.b])
            self.copy("act", self.gateT[0:n, jb, :], ps[0:n, 0:16], [ps.b], [self.gateT.b])
        self.release(m2)
        xs_next = gather(0)
        for e in range(16):
            xs_l = xs_next
            xt = xtr.next()
            for jb, (j0, n) in enumerate(JB):
                xs = xs_l[jb]
                for half in range(2):
                    ps = self.pr.next()
                    pv = ps.ap.bitcast(BF16)
                    for j in range(8):
                        fc = half * 8 + j
                        self.P.op("pe", lambda en, pv=pv, j=j, fc=fc, xs=xs, n=n: en.transpose(pv[:, j * 128:j * 128 + n], xs[0:n, fc * 128:(fc + 1) * 128], self.identb[0:n, 0:n]),
                                  reads=[xs.b, self.cb.b], writes=[ps.b])
                    self.copy("act" if half == 0 else "dve", xt[:, half * 8:(half + 1) * 8, j0:j0 + n],
                              pv.rearrange("p (c t) -> p c t", c=8)[:, :, 0:n], [ps.b], [xt.b])
            for fn in pending:
                fn()
            pending = []
            ensure(3 * e + 3)
            wg, wu, wd = wpieces[3 * e], wpieces[3 * e + 1], wpieces[3 * e + 2]
            wg3 = wg.ap.rearrange("p (c f) -> p c f", c=16)
            wu3 = wu.ap.rearrange("p (c f) -> p c f", c=16)
            wd3 = wd.ap.rearrange("p (c d) -> p c d", c=8)
            hd = hdr.next()
            for Fc in range(8):
                psg, psu = self.pr.next(), self.pr.next()
                for kc in range(16):
                    self.mm(psg, psg[:, 0:nj], wg3[:, kc, Fc * 128:(Fc + 1) * 128], xt[:, kc, 0:nj], kc == 0, kc == 15, [wg.b, xt.b])
                for kc in range(16):
                    self.mm(psu, psu[:, 0:nj], wu3[:, kc, Fc * 128:(Fc + 1) * 128], xt[:, kc, 0:nj], kc == 0, kc == 15, [wu.b, xt.b])
                sg = sgr.next()
                self.act(sg[:, 0:nj], psg[:, 0:nj], AF.Silu, [psg.b], [sg.b])
                self.tt("dve", hd[:, Fc, 0:nj], psu[:, 0:nj], sg[:, 0:nj], ALU.mult, [psu.b, sg.b], [hd.b])
            if e + 1 < 16:
                xs_next = gather(e + 1)
                ensure(3 * e + 5)
            for jb, (j0, n) in enumerate(JB):
                y = yr.next()
                ga = gas[0 if jb < 2 else 1]
                for dc in range(4):
                    ps = self.pr.next()
                    for Fc in range(8):
                        self.mm(ps, ps[0:n, :], hd[:, Fc, j0:j0 + n], wd3[:, Fc, dc * 512:(dc + 1) * 512], Fc == 0, Fc == 7, [hd.b, wd.b])
                    self.stt(y[0:n, dc * 512:(dc + 1) * 512], ps[0:n, :], self.gateT[0:n, jb, e:e + 1], ga[0:n, dc * 512:(dc + 1) * 512],
                             ALU.mult, ALU.mult, [ps.b, self.gateT.b, ga.b], [y.b])
                pending.append(lambda y=y, n=n, jb=jb, e=e: self.P.dma("pool", y.tag, lambda en: en.indirect_dma_start(
                    out=self.XR.ap(), out_offset=bass.IndirectOffsetOnAxis(ap=self.idxT[0:n, jb, e:e + 1], axis=0),
                    in_=y[0:n, :], in_offset=None, compute_op=ALU.add),
                    reads=[y.b, self.idxT.b, xrb], writes=[xrb]))
        for fn in pending:
            fn()
        self.release(m)

    def layer1_mixer(self):
        Ld = self.L[1]
        self.cv_start(1)
        m0 = self.mark()
        HT, HTb = self.alloc_HT()
        self.norm(1, 1, NB, HT, HTb, False)
        m = self.mark()
        TG = [(tg * 512, 512) for tg in range(4)] + [(2048, 256)]

        def hb_of(t0, n):
            return HTb[t0 // 128:(t0 + n) // 128]
        gq = self.tile([128, 12], F32, "gq")
        self.load("sp", gq, Ld["gq"].ap())
        gkv = self.tile([128, 4], F32, "gkv")
        self.load("sp", gkv, Ld["gkv"].ap())
        cs = self.tile([64, 2, S], BF16, "cs")
        self.load("pool", cs, self.cs1_d.ap().rearrange("c p t -> p c t"))
        wq = [self.tile([128, 16, 512], BF16, f"wq{i}") for i in range(4)]
        for i in range(4):
            self.load("pool", wq[i], Ld["w_dqkv"][:, i * 512:(i + 1) * 512].rearrange("(c p) n -> p c n", p=128))
        wk = self.tile([128, 16, 64], BF16, "wk")
        self.load("pool", wk, Ld["w_dqkv"][:, 2048:2112].rearrange("(c p) n -> p c n", p=128))
        raw = self.tile([128, 12, 512], F32)
        rawb = [self.P.buf(f"raw{i}") for i in range(12)]
        sqr = self.ring("sq", [128, 512], BF16, 3)
        sd = self.tile([128, 512], F32)
        rstd = self.tile([128, 512], F32)
        cqr = self.ring("cq", [128, 512], BF16, 3)
        rr = self.rope_rings()

        def lora(chunks, groups, gcol, nfeat, dst):
            nch = len(chunks)
            for (t0, n) in groups:
                pss = self.pst[7]
                stl = {}

                def lA(ci, t0=t0, n=n):
                    w, j = chunks[ci]
                    ps = self.proj_fm(w, j * 128, 128, HT, hb_of(t0, n), 16, t0, n)
                    self.copy("act", raw[:, ci, 0:n], ps[:, 0:n], [ps.b], [rawb[ci]])
                    sq = sqr.next()
                    self.tt("pool", sq[:, 0:n], raw[:, ci, 0:n], raw[:, ci, 0:n], ALU.mult, [rawb[ci]], [sq.b])
                    stl[ci] = sq

                def lB(ci, n=n):
                    self.bg_step()
                    sq = stl.pop(ci)
                    self.mm(pss, pss[:, 0:n], self.ones1, sq[:, 0:n], ci == 0, ci == nch - 1, [sq.b, self.cb.b])
                self.pipeline(nch, [lA, lB])
                self.act(sd[:, 0:n], pss[:, 0:n], AF.Sqrt, [pss.b], [sd.b], bias=EPS_AP(self), scale=1.0 / nfeat)
                self.recip(rstd[:, 0:n], sd[:, 0:n], [sd.b], [rstd.b])
                for ci in range(nch):
                    cq = cqr.next()
                    self.stt(cq[:, 0:n], raw[:, ci, 0:n], gcol[:, ci:ci + 1], rstd[:, 0:n], ALU.mult, ALU.mult, [rawb[ci], rstd.b, gcol.b], [cq.b])
                    self.store("sp", cq, dst[ci * 128:(ci + 1) * 128, t0:t0 + n], ap=cq[:, 0:n])
        lora([(wq[i // 4], i % 4) for i in range(12)], TG[:4], gq, 1536.0, self.CQT)
        lora([(wq[3], i) for i in range(4)], TG, gkv, 512.0, self.CKVT)
        stk = {}

        def kA(i):
            t0, n = TG[i]
            ps = self.proj_fm(wk, 0, 64, HT, hb_of(t0, n), 16, t0, n)
            qn = rr["qn"].next()
            self.copy("act", qn[0:64, 0:n], ps[0:64, 0:n], [ps.b], [qn.b])
            stk[i] = qn

        def kB(i):
            t0, n = TG[i]
            self.rope_tail(stk.pop(i), 64, n, t0, cs, self.rot64, self.KPET[:, t0:t0 + n], rr, is_ctx=(t0 >= S))
        self.pipeline(len(TG), [kA, kB])
        self.release(m)
        self.release(m0)
        m = self.mark()
        cs = self.tile([64, 2, S], BF16, "cs")
        self.load("pool", cs, self.cs1_d.ap().rearrange("c p t -> p c t"))
        cqt = self.tile([128, 12, S], BF16, "cqt")
        for c in range(12):
            self.load("sp" if c % 2 == 0 else "act", cqt, self.CQT[c * 128:(c + 1) * 128, :], ap=cqt[:, c, :])
        wr = self.ring("w", [128, 12, 192], BF16, 3)
        rr = self.rope_rings()
        qnr = self.ring("qnn", [128, 512], BF16, 3)
        items = [(h, t0, n) for h in range(16) for (t0, n) in TG[:4]]
        wcur = {}

        def getw(h):
            if h not in wcur:
                w = wr.next()
                self.load("pool", w, Ld["w_uq"][:, h * 192:(h + 1) * 192].rearrange("(c p) n -> p c n", p=128))
                wcur[h] = w
            return wcur[h]
        stu = {}

        def uA(i):
            h, t0, n = items[i]
            w = getw(h)
            if t0 == 0 and h + 1 < 16:
                getw(h + 1)
            ps = self.proj_fm(w, 0, 128, cqt.ap, [cqt.b], 12, t0, n)
            qn = qnr.next()
            self.copy("act", qn[:, 0:n], ps[:, 0:n], [ps.b], [qn.b])
            self.store("sp", qn, self.QNT[h * 128:(h + 1) * 128, t0:t0 + n], ap=qn[:, 0:n])
            ps = self.proj_fm(w, 128, 64, cqt.ap, [cqt.b], 12, t0, n)
            qp = rr["qn"].next()
            self.copy("dve", qp[0:64, 0:n], ps[0:64, 0:n], [ps.b], [qp.b])
            stu[i] = qp

        def uB(i):
            self.bg_step()
            h, t0, n = items[i]
            self.rope_tail(stu.pop(i), 64, n, t0, cs, self.rot64, self.QPT[h * 64:(h + 1) * 64, t0:t0 + n], rr)
        self.pipeline(len(items), [uA, uB])
        self.release(m)
        m = self.mark()
        ckt = self.tile([128, 4, T], BF16, "ckt")
        self.load("sp", ckt, self.CKVT.ap().rearrange("(c p) t -> p c t", p=128))
        W = self.tile([128, 4, 4096], BF16, "W")
        self.load("pool", W, Ld["w_ukv"].ap().rearrange("(c p) n -> p c n", p=128))
        knr = self.ring("kn", [128, 512], BF16, 3)
        for h in range(16):
            self.bg_step()
            for (t0, n) in TG:
                ps = self.proj_fm(W, h * 256, 128, ckt.ap, [ckt.b], 4, t0, n)
                kn = knr.next()
                self.copy("act" if h % 2 == 0 else "dve", kn[:, 0:n], ps[:, 0:n], [ps.b], [kn.b])
                self.store("sp", kn, self.KNT[h * 128:(h + 1) * 128, t0:t0 + n], ap=kn[:, 0:n])
        vr = self.ring("v", [128, 512], BF16, 3)
        for tb in range(NB):
            for hg in range(4):
                ps = self.pr.next()
                for hh in range(4):
                    h = hg * 4 + hh
                    for c in range(4):
                        self.mm(ps, ps[:, hh * 128:(hh + 1) * 128], ckt[:, c, tb * 128:(tb + 1) * 128], W[:, c, h * 256 + 128:h * 256 + 256], c == 0, c == 3, [ckt.b, W.b])
                v = vr.next()
                self.copy("act" if hg % 2 == 0 else "dve", v.ap, ps.ap, [ps.b], [v.b])
                self.store("sp", v, self.V1[tb * 128:(tb + 1) * 128, hg * 512:(hg + 1) * 512])
        self.release(m)
        kn_aps = [self.KNT[h * 128:(h + 1) * 128, :] for h in range(16)]
        kp_ap = self.KPET.ap()
        self.attention(192 ** -0.5, 16, lambda h: (h, kn_aps[h], kp_ap), lambda h: self.V1[:, h * 128:(h + 1) * 128],
                       lambda h: (self.QNT[h * 128:(h + 1) * 128, :], self.QPT[h * 64:(h + 1) * 64, :]), False, S)
        self.outproj(1, 4)

    def final(self):
        m = self.mark()
        gt = self.tile([128, D], F32, "g")
        self.load("sp", gt, self.fg_d.ap().partition_broadcast(128))
        xr = self.ring("x", [128, D], F32, 2)
        orr = self.ring("fin", [128, D], F32, 2)
        junk = self.tile([128, D], BF16)
        smr = self.ring("sm", [128, 1], F32, 6)
        for tb in range(16):
            x = xr.next()
            self.load("sp", x, self.XR[tb * 128:(tb + 1) * 128, :])
            ss, sq, rs = smr.next(), smr.next(), smr.next()
            self.act(junk.ap, x.ap, AF.Square, [x.b], [junk.b, ss.b], scale=float(D ** -0.5), accum_out=ss.ap)
            self.act(sq.ap, ss.ap, AF.Sqrt, [ss.b], [sq.b], bias=EPS_AP(self), scale=1.0)
            self.recip(rs.ap, sq.ap, [sq.b], [rs.b])
            o = orr.next()
            self.stt(o.ap, x.ap, rs.ap, gt.ap, ALU.mult, ALU.mult, [x.b, rs.b, gt.b], [o.b])
            self.store("act", o, self.out_d[tb * 128:(tb + 1) * 128, :])
        self.release(m)


def EPS_AP(k):
    return k._eps.ap


def _rope_tables(d_rot):
    rows = S // 64
    row = np.repeat(np.arange(rows, dtype=np.float32), 64)
    col = np.tile(np.arange(64, dtype=np.float32), rows)
    n_axis = d_rot // 4
    inv = (np.float32(10000.0) ** (-np.arange(n_axis, dtype=np.float32) / np.float32(n_axis))).astype(np.float32)
    ang = np.concatenate([row[:, None] * inv, col[:, None] * inv], axis=-1).astype(np.float32)
    half = d_rot // 2
    idx = np.arange(d_rot) % half
    cs = np.stack([np.cos(ang)[:, idx].T, np.sin(ang)[:, idx].T]).astype(np.float32)
    return np.ascontiguousarray(cs)


def _rot_lhsT(d_rot, pad=128):
    half = d_rot // 2
    m = np.zeros((pad, pad), np.float32)
    for j in range(half):
        m[j + half, j] = -1.0
        m[j, j + half] = 1.0
    return m


def _colmajor(v, nchunk):
    return np.ascontiguousarray(np.asarray(v, np.float32).reshape(nchunk, 128).T)


def _shared(inputs):
    f = lambda k: np.ascontiguousarray(np.asarray(inputs[k], np.float32))
    sh = {}
    consts = np.zeros((7, 128, 128), np.float32)
    consts[0] = np.eye(128, dtype=np.float32)
    consts[1] = 1.0 / 128
    consts[2] = 1.0 / 1024
    consts[3] = 1.0
    consts[4] = _rot_lhsT(128)
    consts[5] = _rot_lhsT(64)
    sh["consts"] = consts
    sh["cs0"] = _rope_tables(128)
    sh["cs1"] = _rope_tables(64)
    for l in range(2):
        sh[f"l{l}_mod_w"] = f(f"l{l}_mod_w")
        sh[f"l{l}_mod_b"] = f(f"l{l}_mod_b").reshape(1, -1)
        sh[f"l{l}_norm1_g"] = f(f"l{l}_norm1_g").reshape(1, -1)
        sh[f"l{l}_norm2_g"] = f(f"l{l}_norm2_g").reshape(1, -1)
        sh[f"l{l}_router"] = f(f"l{l}_router")
        sh[f"l{l}_w_gate"] = f(f"l{l}_w_gate")
        sh[f"l{l}_w_up"] = f(f"l{l}_w_up")
        sh[f"l{l}_w_down"] = f(f"l{l}_w_down")
        sh[f"l{l}_w_out"] = f(f"l{l}_w_out")
    sh["l0_w_in"] = f("l0_w_in")
    sh["l0_q_norm_g"] = f("l0_q_norm_g").reshape(128, 1)
    sh["l0_k_norm_g"] = f("l0_k_norm_g").reshape(128, 1)
    dw = f("l0_dw_w").reshape(31, 8, 128)
    sh["l0_dwT"] = np.ascontiguousarray(dw.transpose(2, 1, 0))
    sh["l0_cvec"] = np.ascontiguousarray(np.stack([_colmajor(inputs["l0_dw_b"], 8), _colmajor(inputs["l0_conv_ln_g"], 8),
                                                   _colmajor(inputs["l0_conv_ln_b"], 8)], axis=1))
    sh["l1_w_dqkv"] = f("l1_w_dqkv")
    sh["l1_gq"] = _colmajor(inputs["l1_q_lora_norm_g"], 12)
    sh["l1_w_uq"] = f("l1_w_uq")
    sh["l1_gkv"] = _colmajor(inputs["l1_kv_lora_norm_g"], 4)
    sh["l1_w_ukv"] = f("l1_w_ukv")
    sh["final_norm_g"] = f("final_norm_g").reshape(1, -1)
    return sh


def _core_map(inputs, sh, b):
    m = dict(sh)
    m["x"] = np.ascontiguousarray(np.asarray(inputs["x"][b], np.float32))
    m["ctx"] = np.ascontiguousarray(np.asarray(inputs["ctx"][b], np.float32))
    cc = np.stack([np.asarray(inputs["c"][b], np.float32), np.asarray(inputs["c_ctx"], np.float32)], axis=-1)
    m["cT"] = np.ascontiguousarray(cc.reshape(16, 128, 2).transpose(1, 0, 2))
    return m


def kernel(**inputs):
    nc = K().build()
    sh = _shared(inputs)
    nb = np.asarray(inputs["x"]).shape[0]
    in_maps = [_core_map(inputs, sh, b) for b in range(nb)]
    res = run_bass_kernel_spmd(nc, in_maps, core_ids=list(range(nb)))
    return np.stack([np.asarray(r["out"], np.float32) for r in res.results], axis=0)
```

```python
import numpy as np
from concourse.bass_utils import run_bass_kernel_spmd

import contextlib
import concourse.bass as bass
import concourse.mybir as mybir

F32 = mybir.dt.float32
BF16 = mybir.dt.bfloat16
I32 = mybir.dt.int32
U32 = mybir.dt.uint32
AF = mybir.ActivationFunctionType
ALU = mybir.AluOpType
AX = mybir.AxisListType

ENGS = ("pe", "dve", "act", "pool", "sp")
COMPUTE = ("pe", "dve", "act", "pool")


class Buf:
    __slots__ = ("name", "last_w", "readers")

    def __init__(self, name):
        self.name = name
        self.last_w = None
        self.readers = []


class Op:
    __slots__ = ("eng", "fn", "idx", "waits", "signal", "seq", "tag", "tagcnt")

    def __init__(self, eng, fn):
        self.eng = eng
        self.fn = fn
        self.waits = {}
        self.signal = False
        self.seq = None
        self.tag = None
        self.tagcnt = None


class Prog:
    def __init__(self, nc):
        self.nc = nc
        self.ops = {e: [] for e in ENGS}
        self.tags = {}
        self.known = {e: {} for e in ENGS}
        self.barrier_pending = {e: None for e in ENGS}
        self.nbuf = 0

    def buf(self, name=None):
        self.nbuf += 1
        return Buf(name or f"b{self.nbuf}")

    def bufs(self, n, name=None):
        return [self.buf(f"{name}{i}") for i in range(n)]

    def _need(self, op, dep, waw=False):
        if dep is None or dep is op:
            return
        if dep.tag is not None:
            tg = self.tags[dep.tag]
            val = tg[0] * 16
            if op.tag == dep.tag:
                if waw:
                    return
                val = (tg[0] - 1) * 16
            tg[1] = max(tg[1], val)
            key = ("t", dep.tag)
        else:
            if dep.eng == "pe" and op.eng == "pe" and op.tag is None:
                return
            dep.signal = True
            key = ("e", dep.eng)
            val = dep
        cur = op.waits.get(key)
        if cur is None:
            op.waits[key] = val
        elif key[0] == "t":
            op.waits[key] = max(cur, val)
        else:
            op.waits[key] = cur if cur.idx >= val.idx else val

    def _record(self, op, reads, writes):
        lst = self.ops[op.eng]
        op.idx = len(lst)
        lst.append(op)
        bp = self.barrier_pending[op.eng]
        if bp is not None:
            for d in bp[0]:
                self._need(op, d)
            for tag, val in bp[1].items():
                key = ("t", tag)
                op.waits[key] = max(op.waits.get(key, 0), val)
                self.tags[tag][1] = max(self.tags[tag][1], val)
            self.barrier_pending[op.eng] = None
        for b in reads:
            self._need(op, b.last_w)
        for b in writes:
            self._need(op, b.last_w, waw=True)
            for r in b.readers:
                self._need(op, r)
        for b in reads:
            b.readers.append(op)
        for b in writes:
            b.last_w = op
            b.readers = []

    def op(self, eng, fn, reads=(), writes=()):
        o = Op(eng, fn)
        self._record(o, reads, writes)
        return o

    def dma(self, q, tag, fn, reads=(), writes=(), after=()):
        o = Op(q, fn)
        tg = self.tags.setdefault(tag, [0, 0])
        tg[0] += 1
        o.tag = tag
        o.tagcnt = tg[0]
        for (t2, v2) in after:
            tg2 = self.tags.setdefault(t2, [0, 0])
            tg2[1] = max(tg2[1], v2)
            o.waits[("t", t2)] = max(o.waits.get(("t", t2), 0), v2)
        if tg[1] > 0:
            o.waits[("t", tag)] = max(o.waits.get(("t", tag), 0), tg[1])
        self._record(o, reads, writes)
        return o

    def barrier(self):
        lasts = []
        for e in COMPUTE:
            for o in reversed(self.ops[e]):
                if o.tag is None:
                    lasts.append(o)
                    break
        tagvals = {t: v[0] * 16 for t, v in self.tags.items() if v[0] > 0}
        for e in ENGS:
            self.barrier_pending[e] = (lasts, dict(tagvals))

    def emit(self, final_tags=()):
        nc = self.nc
        for e in COMPUTE:
            n = 0
            for o in self.ops[e]:
                if o.signal:
                    n += 1
                    o.seq = n
        with contextlib.ExitStack() as st:
            esem = {e: st.enter_context(nc.semaphore(f"s_{e}")) for e in COMPUTE}
            tsem = {t: st.enter_context(nc.semaphore(f"t_{i}")) for i, t in enumerate(self.tags)}
            block = st.enter_context(nc.Block())

            def run(ename, eng):
                known = {}
                for o in self.ops[ename]:
                    for key, val in o.waits.items():
                        if key[0] == "t":
                            sem, v = tsem[key[1]], val
                        else:
                            sem, v = esem[key[1]], val.seq
                        if known.get(key, 0) >= v:
                            continue
                        known[key] = v
                        eng.wait_ge(sem, v)
                    ins = o.fn(eng)
                    if o.tag is not None:
                        ins.then_inc(tsem[o.tag], 16)
                    elif o.signal:
                        ins.then_inc(esem[ename], 1)
                if ename == "sp":
                    for t in final_tags:
                        eng.wait_ge(tsem[t], self.tags[t][0] * 16)

            @block.sync
            def _(eng):
                run("sp", eng)

            @block.scalar
            def _(eng):
                run("act", eng)

            @block.vector
            def _(eng):
                run("dve", eng)

            @block.gpsimd
            def _(eng):
                run("pool", eng)

            @block.tensor
            def _(eng):
                run("pe", eng)

    def stats(self):
        return {e: len(self.ops[e]) for e in ENGS}, len(self.tags)

D = 2048
S = 2048
C = 256
T = S + C
NB = T // 128
EPS = 1e-6
SBUF_BYTES = 212480
_DS = {F32: 4, BF16: 2, I32: 4, U32: 4}


class Tl:
    __slots__ = ("ap", "b", "tag")

    def __init__(self, ap, b, tag):
        self.ap, self.b, self.tag = ap, b, tag

    def __getitem__(self, k):
        return self.ap[k]


class Ring:
    def __init__(self, tiles):
        self.t = tiles
        self.i = 0

    def next(self):
        t = self.t[self.i % len(self.t)]
        self.i += 1
        return t


class K:
    def __init__(self, stop=None, dumps=(), noinput=()):
        self.noinput = noinput
        nc = bass.Bass("TRN2", target_bir_lowering=False)
        self.nc = nc
        self.P = Prog(nc)
        self.stop = stop
        self.big = nc.alloc_sbuf_tensor("big", [128, SBUF_BYTES], mybir.dt.uint8)
        self.top = 0
        self.ntile = 0
        self.psb = [nc.alloc_psum_tensor(f"ps{i}", [128, 512], F32) for i in range(8)]
        self.pst = [Tl(self.psb[i][:, :], self.P.buf(f"ps{i}"), None) for i in range(8)]
        self.pr = Ring(self.pst[0:6])
        self.din = {}
        self.dumps = dumps

    def inp(self, name, shape, dtype=F32):
        t = self.nc.dram_tensor(name, list(shape), dtype, kind="Internal" if name in self.noinput else "ExternalInput")
        self.din[name] = t
        return t

    def scratch(self, name, shape, dtype):
        kind = "ExternalOutput" if name in self.dumps else "Internal"
        return self.nc.dram_tensor(name, list(shape), dtype, kind=kind)

    def tile(self, shape, dtype, tag=None):
        n = 1
        for s in shape[1:]:
            n *= s
        nbytes = (n * _DS[dtype] + 63) // 64 * 64
        off = self.top
        self.top += nbytes
        assert self.top <= SBUF_BYTES, f"SBUF overflow {self.top}"
        ap = self.big[:, off:off + n * _DS[dtype]].bitcast(dtype)
        if len(shape) == 3:
            ap = ap.rearrange("p (a b) -> p a b", a=shape[1])
        if shape[0] < 128:
            ap = ap[0:shape[0]]
        self.ntile += 1
        return Tl(ap, self.P.buf(f"t{self.ntile}"), tag)

    def ring(self, tag, shape, dtype, n):
        return Ring([self.tile(shape, dtype, f"{tag}{i}") for i in range(n)])

    def mark(self):
        return self.top

    def cv_start(self, l):
        Ld = self.L[l]
        for e in range(16):
            for key in ("wg", "wu", "wd"):
                if key in self.WB[l]:
                    self._cvq.append((Ld[key][e], self.WB[l][key][e]))

    def cv_issue(self, n=1):
        for _ in range(n):
            if not self._cvq:
                return
            src, dst = self._cvq.pop(0)
            self.P.dma("pool", "cv", lambda en, src=src, dst=dst: en.dma_start(
                out=dst.rearrange("(c p) f -> p c f", p=128), in_=src.rearrange("(c p) f -> p c f", p=128)))

    def bg_step(self, k=1):
        self._cvc = getattr(self, "_cvc", 0) + 1
        if self._cvc % self.cv_div == 0:
            self.cv_issue()
        g = getattr(self, "_bg", None)
        self._bgc = getattr(self, "_bgc", 0) + 1
        if self._bgc % getattr(self, "bg_div", 1) != 0:
            return
        for _ in range(k):
            if g is None:
                return
            try:
                next(g)
            except StopIteration:
                self._bg = g = None

    def bg_drain(self):
        while getattr(self, "_bg", None) is not None:
            self.bg_step()

    def release(self, m):
        self.top = m
        self.P.barrier()

    def pipeline(self, n, stages):
        ns = len(stages)
        for step in range(n + ns - 1):
            for si in reversed(range(ns)):
                i = step - si
                if 0 <= i < n:
                    stages[si](i)

    def dma(self, q, tag, out, in_, reads=(), writes=()):
        return self.P.dma(q, tag, lambda e: e.dma_start(out=out, in_=in_), reads=reads, writes=writes)

    def load(self, q, t, src, ap=None, reads=()):
        return self.dma(q, t.tag, t.ap if ap is None else ap, src, reads=reads, writes=[t.b])

    def store(self, q, t, dst, ap=None, extra_w=()):
        return self.dma(q, t.tag, dst, t.ap if ap is None else ap, reads=[t.b], writes=list(extra_w))

    def mm(self, ps, out, lhsT, rhs, start, stop, reads):
        return self.P.op("pe", lambda e: e.matmul(out, lhsT, rhs, start=start, stop=stop), reads=reads, writes=[ps.b])

    def act(self, out, in_, func, reads, writes, **kw):
        return self.P.op("act", lambda e: e.activation(out, in_, func, **kw), reads=reads, writes=writes)

    def tt(self, eng, out, a, b, op, reads, writes):
        return self.P.op(eng, lambda e: e.tensor_tensor(out, a, b, op), reads=reads, writes=writes)

    def stt(self, out, in0, scalar, in1, op0, op1, reads, writes):
        return self.P.op("dve", lambda e: e.scalar_tensor_tensor(out, in0, scalar, in1, op0, op1), reads=reads, writes=writes)

    def recip(self, out, in_, reads, writes):
        return self.P.op("dve", lambda e: e.reciprocal(out, in_), reads=reads, writes=writes)

    def copy(self, eng, out, in_, reads, writes):
        if eng == "act":
            return self.P.op("act", lambda e: e.copy(out, in_), reads=reads, writes=writes)
        return self.P.op(eng, lambda e: e.tensor_copy(out, in_), reads=reads, writes=writes)

    def build(self):
        nc = self.nc
        I = self.inp
        self.x_d = I("x", [S, D])
        self.ctx_d = I("ctx", [C, D])
        self.cT_d = I("cT", [128, 16, 2])
        self.cst_d = I("consts", [7, 128, 128])
        self.cs0_d = I("cs0", [2, 128, S])
        self.cs1_d = I("cs1", [2, 64, S])
        L = []
        for l in range(2):
            d = dict(mod_w=I(f"l{l}_mod_w", [D, 6 * D]), mod_b=I(f"l{l}_mod_b", [1, 6 * D]),
                     g1=I(f"l{l}_norm1_g", [1, D]), g2=I(f"l{l}_norm2_g", [1, D]),
                     router=I(f"l{l}_router", [D, 16]), wg=I(f"l{l}_w_gate", [16, D, 1024]),
                     wu=I(f"l{l}_w_up", [16, D, 1024]), wd=I(f"l{l}_w_down", [16, 1024, D]),
                     w_out=I(f"l{l}_w_out", [D, D]))
            L.append(d)
        L[0].update(w_in=I("l0_w_in", [D, 3584]), qg=I("l0_q_norm_g", [128, 1]), kg=I("l0_k_norm_g", [128, 1]),
                    dwT=I("l0_dwT", [128, 8, 31]), cvec=I("l0_cvec", [128, 3, 8]))
        L[1].update(w_dqkv=I("l1_w_dqkv", [D, 2112]), gq=I("l1_gq", [128, 12]), w_uq=I("l1_w_uq", [1536, 3072]),
                    gkv=I("l1_gkv", [128, 4]), w_ukv=I("l1_w_ukv", [512, 4096]))
        self.fg_d = I("final_norm_g", [1, D])
        self.L = L
        self.out_d = nc.dram_tensor("out", [S, D], F32, kind="ExternalOutput")
        sc = self.scratch
        self.XR = sc("XR", [T, D], F32)
        self.ADA = [sc(f"ADA{l}", [2, 6 * D], F32) for l in range(2)]
        self.QT = sc("QT", [1024, T], BF16)
        self.KT = sc("KT", [256, T], BF16)
        self.V0 = sc("V0", [T, 256], BF16)
        self.UG = sc("UG", [1024, T], BF16)
        self.AT = sc("AT", [D, T], BF16)
        self.H2 = sc("H2", [T, D], BF16)
        self.CQT = sc("CQT", [1536, S], BF16)
        self.CKVT = sc("CKVT", [512, T], BF16)
        self.KPET = sc("KPET", [64, T], BF16)
        self.QNT = sc("QNT", [2048, S], BF16)
        self.QPT = sc("QPT", [1024, S], BF16)
        self.KNT = sc("KNT", [2048, T], BF16)
        self.V1 = sc("V1", [T, 2048], BF16)
        self.WB = [dict(wg=sc("WG0", [16, D, 1024], BF16), wu=sc("WU0", [16, D, 1024], BF16)),
                   dict(wg=sc("WG1", [16, D, 1024], BF16), wu=sc("WU1", [16, D, 1024], BF16), wd=sc("WD1", [16, 1024, D], BF16))]
        self._cvq = []
        self.cv_div = 3

        cb = self.tile([128, 7, 128], BF16, "cb")
        self.load("pool", cb, self.cst_d.ap().rearrange("c p f -> p c f"))
        self.cb = cb
        self.identb, self.o128, self.o1024, self.ones1 = cb[:, 0, :], cb[:, 1, :], cb[:, 2, :], cb[:, 3, :]
        self.rot128, self.rot64 = cb[:, 4, :], cb[0:64, 5, 0:64]
        idf = self.tile([128, 128], F32, "idf")
        self.load("sp", idf, self.cst_d[0])
        self.idf = idf
        self.affT = self.tile([16, T], F32)
        ept = self.tile([128, 1], F32)
        self.P.op("dve", lambda e: e.memset(ept.ap, EPS), writes=[ept.b])
        self._eps = ept
        self.idxT = self.tile([128, 3, 16], I32)
        self.gateT = self.tile([128, 3, 16], F32)

        m = self.mark()
        r = self.ring("x", [128, D], F32, 3)
        for tb in range(NB):
            t = r.next()
            src = self.x_d[tb * 128:(tb + 1) * 128, :] if tb < 16 else self.ctx_d[(tb - 16) * 128:(tb - 15) * 128, :]
            self.load("sp", t, src)
            self.store("act", t, self.XR[tb * 128:(tb + 1) * 128, :])
        self.release(m)

        def ada_then_mix0():
            m_bg = self.mark()
            self.ada_setup()
            for _ in self.ada_gen([(0, n) for n in range(8)]):
                pass
            self.P.barrier()
            self._bg = self.ada_gen([(0, n) for n in range(8, 24)] + [(1, n) for n in range(24)])
            self.layer0_mixer(m_bg)

        steps = [
            ("mix0", ada_then_mix0), ("moe0", lambda: self.moe(0)),
            ("mix1", self.layer1_mixer), ("moe1", lambda: self.moe(1)),
        ]
        for name, fn in steps:
            fn()
            if self.stop == name:
                break
        self.final()
        self.P.emit(final_tags=["fin0", "fin1"])
        return nc

    def ada_setup(self):
        ct = self.tile([128, 16, 2], F32, "c")
        sct = self.tile([128, 16, 2], BF16)
        self.load("sp", ct, self.cT_d.ap())
        self.act(sct.ap, ct.ap, AF.Silu, [ct.b], [sct.b])
        self._ada = dict(sct=sct, wr=self.ring("aw", [128, 16, 512], BF16, 2),
                         br=self.ring("ab", [2, 512], F32, 2), orr=self.ring("ao", [2, 512], F32, 1))

    def ada_gen(self, jobs):
        A = self._ada
        sct = A["sct"]
        loaded = {}

        def issue(idx):
            l, n = jobs[idx]
            Ld = self.L[l]
            w = A["wr"].next()
            self.load("pool", w, Ld["mod_w"][:, n * 512:(n + 1) * 512].rearrange("(c p) n -> p c n", p=128))
            bt = A["br"].next()
            self.load("sp", bt, Ld["mod_b"][0:1, n * 512:(n + 1) * 512].partition_broadcast(2))
            loaded[idx] = (w, bt)
        issue(0)
        for idx in range(len(jobs)):
            if idx + 1 < len(jobs):
                issue(idx + 1)
            l, n = jobs[idx]
            w, bt = loaded.pop(idx)
            ps = self.pst[7]
            for kc in range(16):
                self.mm(ps, ps[0:2, :], sct[:, kc, :], w[:, kc, :], kc == 0, kc == 15, [sct.b, w.b])
            o = A["orr"].next()
            self.tt("dve", o.ap, ps[0:2, :], bt.ap, ALU.add, [ps.b, bt.b], [o.b])
            self.store("sp", o, self.ADA[l][:, n * 512:(n + 1) * 512])
            yield

    def norm(self, l, which, nblk, HT, HTb, write_tok, post=()):
        m = self.mark()
        Ld = self.L[l]
        o_sh, o_sc = (0, 2048) if which == 1 else (6144, 8192)
        t1r = self.ring("t1", [128, D], F32, 2)
        gt = t1r.t[0]
        self.load("sp", gt, Ld["g1" if which == 1 else "g2"].ap().partition_broadcast(128))
        mods = []
        for r in range(2 if nblk > 16 else 1):
            a = self.tile([128, D], F32, f"a{r}")
            sh = self.tile([128, D], F32, f"s{r}")
            self.load("sp", a, self.ADA[l][r:r + 1, o_sc:o_sc + D].partition_broadcast(128))
            self.load("sp", sh, self.ADA[l][r:r + 1, o_sh:o_sh + D].partition_broadcast(128))
            self.stt(a.ap, a.ap, 1.0, gt.ap, ALU.add, ALU.mult, [a.b, gt.b], [a.b])
            mods.append((a, sh))
        xr = self.ring("x", [128, D], F32, 3)
        hbr = self.ring("hb", [128, D], BF16, 2)
        smr = self.ring("sm", [128, 1], F32, 12)
        st = {}

        def s0(tb):
            x = xr.next()
            self.load("sp" if tb % 2 == 0 else "act", x, self.XR[tb * 128:(tb + 1) * 128, :])
            st[tb] = dict(x=x)

        def s1(tb):
            x = st[tb]["x"]
            ss, sq, rs = smr.next(), smr.next(), smr.next()
            jt = t1r.t[tb % 2]
            self.act(jt.ap, x.ap, AF.Square, [x.b], [jt.b, ss.b], scale=float(D ** -0.5), accum_out=ss.ap)
            self.act(sq.ap, ss.ap, AF.Sqrt, [ss.b], [sq.b], bias=EPS_AP(self), scale=1.0)
            self.recip(rs.ap, sq.ap, [sq.b], [rs.b])
            st[tb]["rs"] = rs

        def s2(tb):
            a, sh = mods[0 if tb < 16 else 1]
            x, rs = st[tb]["x"], st[tb]["rs"]
            t1 = t1r.t[tb % 2]
            self.stt(t1.ap, x.ap, rs.ap, a.ap, ALU.mult, ALU.mult, [x.b, rs.b, a.b], [t1.b])
            hb = hbr.next()
            self.tt("pool", hb.ap, t1.ap, sh.ap, ALU.add, [t1.b, sh.b], [hb.b])
            if write_tok:
                self.store("sp", hb, self.H2[tb * 128:(tb + 1) * 128, :])
            st[tb]["hb"] = hb

        def s3(tb):
            hb = st[tb]["hb"]
            for half in range(2):
                ps = self.pr.next()
                pv = ps.ap.bitcast(BF16)
                for j in range(8):
                    fc = half * 8 + j
                    self.P.op("pe", lambda e, pv=pv, j=j, fc=fc, hb=hb: e.transpose(pv[:, j * 128:(j + 1) * 128], hb[:, fc * 128:(fc + 1) * 128], self.identb),
                              reads=[hb.b, self.cb.b], writes=[ps.b])
                self.copy("act" if half == 0 else "dve", HT[:, half * 8:(half + 1) * 8, tb * 128:(tb + 1) * 128],
                          pv.rearrange("p (c t) -> p c t", c=8), [ps.b], [HTb[tb]])
            self.bg_step()
        self.pipeline(nblk, [s0, s1, s2, s3] + list(post))
        self.release(m)

    def alloc_HT(self):
        HT = self.tile([128, 16, T], BF16)
        HTb = [self.P.buf(f"HT{i}") for i in range(NB)]
        return HT.ap, HTb

    def proj_fm(self, w, c0, mp, src, srcb, nk, t0, n):
        ps = self.pr.next()
        for kc in range(nk):
            self.mm(ps, ps[0:mp, 0:n], w[:, kc, c0:c0 + mp], src[:, kc, t0:t0 + n], kc == 0, kc == nk - 1, [w.b] + srcb)
        return ps

    def rope_tail(self, qn, mp, n, t0, cs, rot, dst, rings, is_ctx=False):
        if is_ctx:
            self.store("sp", qn, dst, ap=qn[0:mp, 0:n])
            return
        ps3 = self.pr.next()
        self.mm(ps3, ps3[0:mp, 0:n], rot, qn[0:mp, 0:n], True, True, [qn.b, self.cb.b])
        t1, t2, qo = rings["t1"].next(), rings["t2"].next(), rings["qo"].next()
        self.tt("pool", t1[0:mp, 0:n], qn[0:mp, 0:n], cs[0:mp, 0, t0:t0 + n], ALU.mult, [qn.b, cs.b], [t1.b])
        self.tt("dve", t2[0:mp, 0:n], ps3[0:mp, 0:n], cs[0:mp, 1, t0:t0 + n], ALU.mult, [ps3.b, cs.b], [t2.b])
        self.tt("pool", qo[0:mp, 0:n], t1[0:mp, 0:n], t2[0:mp, 0:n], ALU.add, [t1.b, t2.b], [qo.b])
        self.store("sp", qo, dst, ap=qo[0:mp, 0:n])

    def rope_rings(self):
        return dict(qn=self.ring("qn", [128, 512], BF16, 3), t1=self.ring("rt1", [128, 512], F32, 2),
                    t2=self.ring("rt2", [128, 512], F32, 2), qo=self.ring("qo", [128, 512], BF16, 2))

    def layer0_mixer(self, m_bg):
        Ld = self.L[0]
        m0 = self.mark()
        HT, HTb = self.alloc_HT()
        self.bg_div = 3
        self.norm(0, 1, NB, HT, HTb, False)
        self.bg_div = 3
        m = self.mark()
        gcol = self.tile([128, 2], F32, "gc")
        self.load("sp", gcol, Ld["qg"].ap(), ap=gcol[:, 0:1])
        self.load("sp", gcol, Ld["kg"].ap(), ap=gcol[:, 1:2])
        cs = self.tile([128, 2, S], BF16, "cs")
        self.load("pool", cs, self.cs0_d.ap().rearrange("c p t -> p c t"))
        wr = self.ring("w", [128, 16, 512], BF16, 3)
        rr = self.rope_rings()
        sqr = self.ring("sq", [128, 512], BF16, 3)
        sdr = self.ring("sd", [128, 512], F32, 1)
        rsr = self.ring("rs", [128, 512], F32, 2)
        TG = [(tg * 512, 512) for tg in range(4)] + [(2048, 256)]

        def hb_of(t0, n):
            return HTb[t0 // 128:(t0 + n) // 128]

        def loadw(p):
            w = wr.next()
            self.load("pool", w, Ld["w_in"][:, p * 512:(p + 1) * 512].rearrange("(c p) n -> p c n", p=128))
            return w

        items = []
        for p in range(2):
            for j in range(4):
                h = p * 4 + j
                for (t0, n) in TG:
                    items.append((p, j, gcol[:, 0:1], self.QT[h * 128:(h + 1) * 128, :], t0, n))
        for j in range(2):
            for (t0, n) in TG:
                items.append((2, j, gcol[:, 1:2], self.KT[j * 128:(j + 1) * 128, :], t0, n))
        wcur = {}

        def getw(p):
            if p not in wcur:
                wcur[p] = loadw(p)
            return wcur[p]
        stq = {}

        def qA(i):
            p, j, g_ap, dst, t0, n = items[i]
            w = getw(p)
            if j == 0 and t0 == 0 and p < 2:
                getw(p + 1)
            ps = self.proj_fm(w, j * 128, 128, HT, hb_of(t0, n), 16, t0, n)
            sq = sqr.next()
            self.act(sq[:, 0:n], ps[:, 0:n], AF.Square, [ps.b], [sq.b])
            stq[i] = [ps, sq]

        def qB(i):
            p, j, g_ap, dst, t0, n = items[i]
            ps, sq = stq[i]
            ps2 = self.pr.next()
            self.mm(ps2, ps2[:, 0:n], self.o128, sq[:, 0:n], True, True, [sq.b, self.cb.b])
            sd = sdr.next()
            self.act(sd[:, 0:n], ps2[:, 0:n], AF.Sqrt, [ps2.b], [sd.b], bias=EPS_AP(self), scale=1.0)
            rs = rsr.next()
            self.recip(rs[:, 0:n], sd[:, 0:n], [sd.b], [rs.b])
            qn = rr["qn"].next()
            self.stt(qn[:, 0:n], ps[:, 0:n], g_ap, rs[:, 0:n], ALU.mult, ALU.mult, [ps.b, rs.b, gcol.b], [qn.b])
            stq[i] = qn

        def qC(i):
            p, j, g_ap, dst, t0, n = items[i]
            self.rope_tail(stq.pop(i), 128, n, t0, cs, self.rot128, dst[:, t0:t0 + n], rr, is_ctx=(t0 >= S))
            self.bg_step()
        self.pipeline(len(items), [qA, qB, qC])
        w = getw(2)
        wun, wgn = loadw(3), loadw(5)
        vr = self.ring("v", [128, 256], BF16, 2)
        for tb in range(NB):
            ps = self.pr.next()
            for kc in range(16):
                self.mm(ps, ps[:, 0:256], HT[:, kc, tb * 128:(tb + 1) * 128], w[:, kc, 256:512], kc == 0, kc == 15, [w.b, HTb[tb]])
            v = vr.next()
            self.copy("act", v.ap, ps[:, 0:256], [ps.b], [v.b])
            self.store("sp", v, self.V0[tb * 128:(tb + 1) * 128, :])
        sgr = self.ring("sg", [128, 512], BF16, 2)
        ugr = self.ring("ug", [128, 512], BF16, 2)
        for p in range(2):
            if p == 1:
                wgn = loadw(6)
            wu, wg = wun, wgn
            if p == 0:
                wun = loadw(4)
            for j in range(4):
                c = p * 4 + j
                for (t0, n) in TG:
                    self.bg_step()
                    psu = self.proj_fm(wu, j * 128, 128, HT, hb_of(t0, n), 16, t0, n)
                    psg = self.proj_fm(wg, j * 128, 128, HT, hb_of(t0, n), 16, t0, n)
                    sg = sgr.next()
                    self.act(sg[:, 0:n], psg[:, 0:n], AF.Sigmoid, [psg.b], [sg.b])
                    ug = ugr.next()
                    self.tt("dve", ug[:, 0:n], psu[:, 0:n], sg[:, 0:n], ALU.mult, [psu.b, sg.b], [ug.b])
                    self.store("sp", ug, self.UG[c * 128:(c + 1) * 128, t0:t0 + n], ap=ug[:, 0:n])
        self.release(m0)
        self.bg_div = 1
        self.cv_start(0)
        self.cv_div = 2
        self.attention(128 ** -0.5, 8, lambda h: (h // 4, self.KT[(h // 4) * 128:(h // 4 + 1) * 128, :], None),
                       lambda h: self.V0[:, (h // 4) * 128:(h // 4 + 1) * 128],
                       lambda h: (self.QT[h * 128:(h + 1) * 128, :], None), True, T)
        self.bg_drain()
        self.release(m_bg)
        if self.stop == "attn0":
            return
        self.conv0()
        self.outproj(0, 5)

    def attention(self, scale, nheads, k_src, v_src, q_src, with_ctx_q, qlen):
        m = self.mark()
        ktr = self.ring("kt", [128, T], BF16, 2)
        kpr = self.ring("kp", [64, T], BF16, 1)
        vr = self.ring("vv", [128, NB, 128], BF16, 2)
        qtr = self.ring("qt", [128, qlen], BF16, 2)
        qpr = self.ring("qp", [64, qlen], BF16, 2)
        ptr_ = self.ring("pT", [128, 512], BF16, 4)
        rsr = self.ring("rs", [128, 512], F32, 2)
        orr = self.ring("o", [128, 512], BF16, 2)
        Sr = Ring(self.pst[0:3])
        Or = Ring(self.pst[3:5])
        Ur = Ring(self.pst[5:7])
        groups = [(qg * 512, 512, list(range(NB))) for qg in range(4)]
        if with_ctx_q:
            groups.append((2048, 256, [16, 17]))
        kp = None
        last_k = None
        for h in range(nheads):
            kkey, ks, kps = k_src(h)
            if kkey != last_k:
                kt = ktr.next()
                self.load("sp", kt, ks)
                v = vr.next()
                self.load("act", v, v_src(h).rearrange("(b p) d -> p b d", p=128))
                last_k = kkey
            if kps is not None and kp is None:
                kp = kpr.next()
                self.load("sp", kp, kps)
            qs, qps = q_src(h)
            qt = qtr.next()
            self.load("sp", qt, qs)
            qp = None
            if qps is not None:
                qp = qpr.next()
                self.load("act", qp, qps)
            for (q0, n, kbs) in groups:
                self.bg_step()
                pso, psu = Or.next(), Ur.next()

                def smm(kb):
                    s = Sr.next()
                    rd = [kt.b, qt.b]
                    self.mm(s, s[:, 0:n], kt[:, kb * 128:(kb + 1) * 128], qt[:, q0:q0 + n], True, qp is None, rd)
                    if qp is not None:
                        self.mm(s, s[:, 0:n], kp[:, kb * 128:(kb + 1) * 128], qp[:, q0:q0 + n], False, True, [kp.b, qp.b])
                    return s
                pend = [smm(kb) for kb in kbs[:2]]
                for i, kb in enumerate(kbs):
                    s = pend.pop(0)
                    p = ptr_.next()
                    self.act(p[:, 0:n], s[:, 0:n], AF.Exp, [s.b], [p.b], scale=float(scale))
                    self.mm(pso, pso[:, 0:n], v[:, kb, :], p[:, 0:n], i == 0, i == len(kbs) - 1, [v.b, p.b])
                    self.mm(psu, psu[:, 0:n], self.ones1, p[:, 0:n], i == 0, i == len(kbs) - 1, [p.b, self.cb.b])
                    if i + 2 < len(kbs):
                        pend.append(smm(kbs[i + 2]))
                r = rsr.next()
                self.recip(r[:, 0:n], psu[:, 0:n], [psu.b], [r.b])
                o = orr.next()
                self.tt("dve", o[:, 0:n], pso[:, 0:n], r[:, 0:n], ALU.mult, [pso.b, r.b], [o.b])
                self.store("sp", o, self.AT[h * 128:(h + 1) * 128, q0:q0 + n], ap=o[:, 0:n])
        self.release(m)

    def conv0(self):
        Ld = self.L[0]
        m = self.mark()
        PW = 15 + S + 15 + 15 + C + 15
        CS0 = S + 30
        ugt = self.tile([128, 8, PW], BF16, "ugt")
        zb = self.P.buf("ugt_zero")
        self.P.op("pool", lambda e: e.memset(ugt.ap, 0.0), writes=[ugt.b, zb])
        for c in range(8):
            self.load("sp", ugt, self.UG[c * 128:(c + 1) * 128, 0:S], ap=ugt[:, c, 15:15 + S], reads=[zb])
            self.load("act", ugt, self.UG[c * 128:(c + 1) * 128, S:T], ap=ugt[:, c, CS0 + 15:CS0 + 15 + C], reads=[zb])
        dwt = self.tile([128, 8, 31], F32, "dwt")
        self.load("sp", dwt, Ld["dwT"].ap())
        cv = self.tile([128, 3, 8], F32, "cv")
        self.load("sp", cv, Ld["cvec"].ap())
        Dm = self.tile([128, 248, 128], BF16)
        for c in range(8):
            self.P.op("dve", lambda e, c=c: e.tensor_tensor(
                Dm[:, c * 31:(c + 1) * 31, :], self.identb.unsqueeze(1).to_broadcast([128, 31, 128]),
                dwt[:, c, :].unsqueeze(2).to_broadcast([128, 31, 128]), ALU.mult),
                reads=[self.cb.b, dwt.b], writes=[Dm.b])
        Yr = self.ring("Y", [128, 8, 512], F32, 2)
        ybr = self.ring("yb", [128, 512], BF16, 2)
        yqr = self.ring("yq", [128, 512], BF16, 2)
        st = [self.tile([128, 512], F32) for _ in range(5)]
        zr = self.ring("z", [128, 512], F32, 2)
        z2r = self.ring("z2", [128, 512], F32, 2)
        cor = self.ring("co", [128, 512], BF16, 2)
        Cr = Ring(self.pst[0:4])
        TG = [(tg * 512, 512, tg * 512) for tg in range(4)] + [(2048, 256, CS0)]
        for (t0, n, off) in TG:
            Y = Yr.next()
            s1, s2 = self.pst[4], self.pst[5]
            pend = None
            for c in range(8):
                self.bg_step()
                ps = Cr.next()
                for k in range(31):
                    self.mm(ps, ps[:, 0:n], Dm[:, c * 31 + k, :], ugt[:, c, off + k:off + k + n], k == 0, k == 30, [Dm.b, ugt.b])
                if pend is not None:
                    pc, pyb, pyq = pend
                    self.mm(s1, s1[:, 0:n], self.o1024, pyb[:, 0:n], pc == 0, False, [pyb.b, self.cb.b])
                    self.mm(s2, s2[:, 0:n], self.o1024, pyq[:, 0:n], pc == 0, False, [pyq.b, self.cb.b])
                self.act(Y[:, c, 0:n], ps[:, 0:n], AF.Identity, [ps.b, cv.b], [Y.b], bias=cv[:, 0, c:c + 1], scale=1.0)
                yb, yq = ybr.next(), yqr.next()
                self.copy("pool", yb[:, 0:n], Y[:, c, 0:n], [Y.b], [yb.b])
                self.tt("dve", yq[:, 0:n], Y[:, c, 0:n], Y[:, c, 0:n], ALU.mult, [Y.b], [yq.b])
                pend = (c, yb, yq)
            pc, pyb, pyq = pend
            self.mm(s1, s1[:, 0:n], self.o1024, pyb[:, 0:n], False, True, [pyb.b, self.cb.b])
            self.mm(s2, s2[:, 0:n], self.o1024, pyq[:, 0:n], False, True, [pyq.b, self.cb.b])
            mu, msq, var, rstd, nmr = st
            self.copy("act", mu[:, 0:n], s1[:, 0:n], [s1.b], [mu.b])
            self.tt("dve", msq[:, 0:n], mu[:, 0:n], mu[:, 0:n], ALU.mult, [mu.b], [msq.b])
            self.tt("dve", var[:, 0:n], s2[:, 0:n], msq[:, 0:n], ALU.subtract, [s2.b, msq.b], [var.b])
            self.act(msq[:, 0:n], var[:, 0:n], AF.Sqrt, [var.b], [msq.b], bias=EPS_AP(self), scale=1.0)
            self.recip(rstd[:, 0:n], msq[:, 0:n], [msq.b], [rstd.b])
            self.stt(nmr[:, 0:n], mu[:, 0:n], -1.0, rstd[:, 0:n], ALU.mult, ALU.mult, [mu.b, rstd.b], [nmr.b])
            for c in range(8):
                z, z2, co = zr.next(), z2r.next(), cor.next()
                self.tt("dve", z[:, 0:n], Y[:, c, 0:n], rstd[:, 0:n], ALU.mult, [Y.b, rstd.b], [z.b])
                self.tt("pool", z2[:, 0:n], z[:, 0:n], nmr[:, 0:n], ALU.add, [z.b, nmr.b], [z2.b])
                self.act(co[:, 0:n], z2[:, 0:n], AF.Silu, [z2.b, cv.b], [co.b], scale=cv[:, 1, c:c + 1], bias=cv[:, 2, c:c + 1])
                self.store("sp", co, self.AT[1024 + c * 128:1024 + (c + 1) * 128, t0:t0 + n], ap=co[:, 0:n])
        self.release(m)

    def outproj(self, l, ngroups):
        Ld = self.L[l]
        m = self.mark()
        W = self.tile([128, 16, D], BF16, "W")
        for p in range(4):
            self.load("pool", W, Ld["w_out"][:, p * 512:(p + 1) * 512].rearrange("(c p) n -> p c n", p=128), ap=W[:, :, p * 512:(p + 1) * 512])
        gas = []
        for r in range(2 if ngroups > 4 else 1):
            ga = self.tile([128, D], F32, f"ga{r}")
            self.load("sp", ga, self.ADA[l][r:r + 1, 4096:6144].partition_broadcast(128))
            gas.append(ga)
        atr = self.ring("at", [128, 16, 512], BF16, 2)
        xr = self.ring("x", [128, D], F32, 3)
        tr = self.ring("tm", [128, 512], F32, 3)
        for tg in range(ngroups):
            t0, n = tg * 512, (512 if tg < 4 else 256)
            ga = gas[0 if tg < 4 else 1]
            at = atr.next()
            self.load("sp", at, self.AT[:, t0:t0 + n].rearrange("(c p) t -> p c t", p=128), ap=at[:, :, 0:n])
            for bi in range(n // 128):
                tb = tg * 4 + bi
                self.bg_step()
                x = xr.next()
                self.load("act", x, self.XR[tb * 128:(tb + 1) * 128, :])
                for dc in range(4):
                    ps = self.pr.next()
                    for fc in range(16):
                        self.mm(ps, ps.ap, at[:, fc, bi * 128:(bi + 1) * 128], W[:, fc, dc * 512:(dc + 1) * 512], fc == 0, fc == 15, [at.b, W.b])
                    tm = tr.next()
                    self.tt("dve", tm.ap, ps.ap, ga[:, dc * 512:(dc + 1) * 512], ALU.mult, [ps.b, ga.b], [tm.b])
                    self.tt("pool", x[:, dc * 512:(dc + 1) * 512], x[:, dc * 512:(dc + 1) * 512], tm.ap, ALU.add, [x.b, tm.b], [x.b])
                self.store("sp", x, self.XR[tb * 128:(tb + 1) * 128, :])
        self.release(m)

    def moe(self, l):
        Ld = self.L[l]
        nblk = NB if l == 0 else 16
        nj = 288 if l == 0 else 256
        JB = [(0, 128), (128, 128), (256, 32)][:3 if l == 0 else 2]
        m0 = self.mark()
        HT, HTb = self.alloc_HT()
        rw = self.tile([128, 16, 16], BF16, "rw")
        self.load("pool", rw, Ld["router"].ap().rearrange("(c p) e -> p c e", p=128))
        smr = self.ring("sm2", [128, 1], F32, 12)
        exr = self.ring("ex", [128, 16], F32, 3)
        afr = self.ring("af", [128, 16], F32, 3)
        str_ = {}

        def rA(tb):
            ps = self.pr.next()
            for kc in range(16):
                self.mm(ps, ps[:, 0:16], HT[:, kc, tb * 128:(tb + 1) * 128], rw[:, kc, :], kc == 0, kc == 15, [rw.b, HTb[tb]])
            mx, nmx, ssum, rr = smr.next(), smr.next(), smr.next(), smr.next()
            self.P.op("dve", lambda e, mx=mx, ps=ps: e.reduce_max(mx.ap, ps[:, 0:16], AX.X), reads=[ps.b], writes=[mx.b])
            self.P.op("dve", lambda e, mx=mx, nmx=nmx: e.tensor_scalar(nmx.ap, mx.ap, -1.0, None, ALU.mult), reads=[mx.b], writes=[nmx.b])
            ex = exr.next()
            self.act(ex.ap, ps[:, 0:16], AF.Exp, [ps.b, nmx.b], [ex.b, ssum.b], bias=nmx.ap, scale=1.0, accum_out=ssum.ap)
            self.recip(rr.ap, ssum.ap, [ssum.b], [rr.b])
            af = afr.next()
            self.P.op("dve", lambda e, af=af, ex=ex, rr=rr: e.tensor_scalar(af.ap, ex.ap, rr.ap, None, ALU.mult), reads=[ex.b, rr.b], writes=[af.b])
            str_[tb] = af

        def rB(tb):
            af = str_.pop(tb)
            ps2 = self.pr.next()
            self.P.op("pe", lambda e, ps2=ps2, af=af: e.transpose(ps2[0:16, 0:128], af.ap, self.idf.ap), reads=[af.b, self.idf.b], writes=[ps2.b])
            self.copy("act", self.affT[:, tb * 128:(tb + 1) * 128], ps2[0:16, 0:128], [ps2.b], [self.affT.b])
        self.norm(l, 2, nblk, HT, HTb, True, post=[rA, rB])
        self.cv_issue(len(self._cvq))
        self.release(m0)
        m = self.mark()
        gas = [self.tile([128, D], BF16) for r in range(2 if l == 0 else 1)]
        slots = self.ring("ws", [128, 16 * 1024], BF16, 4)
        xsr = self.ring("xs", [128, D], BF16, len(JB))
        xtr = self.ring("xT", [128, 16, 288], BF16, 1)
        hdr = self.ring("hd", [128, 8, 288], BF16, 1)
        sgr = self.ring("sg", [128, 288], BF16, 2)
        yr = self.ring("y", [128, D], F32, 3)
        xrb = self.P.buf("xr_scatter")
        pending = []
        for r, ga in enumerate(gas):
            stg = yr.t[1 + r]
            self.load("sp", stg, self.ADA[l][r:r + 1, 10240:12288].partition_broadcast(128))
            self.copy("dve", ga.ap, stg.ap, [stg.b], [ga.b])

        def gather(e):
            out = []
            for jb, (j0, n) in enumerate(JB):
                xs = xsr.next()
                self.P.dma("pool", xs.tag, lambda en, xs=xs, n=n, jb=jb, e=e: en.indirect_dma_start(
                    out=xs[0:n, :], out_offset=None, in_=self.H2.ap(),
                    in_offset=bass.IndirectOffsetOnAxis(ap=self.idxT[0:n, jb, e:e + 1], axis=0)),
                    reads=[self.idxT.b], writes=[xs.b])
                out.append(xs)
            return out

        wpieces = {}
        wstate = [0]

        def ensure(k):
            while wstate[0] <= k and wstate[0] < 48:
                kk = wstate[0]
                e, which = divmod(kk, 3)
                sl = slots.next()
                key = ("wg", "wu", "wd")[which]
                pre = key in self.WB[l]
                src = (self.WB[l][key] if pre else Ld[key])[e]
                q = "sp" if pre else "pool"
                if which < 2:
                    self.load(q, sl, src.rearrange("(c p) f -> p c f", p=128), ap=sl.ap.rearrange("p (c f) -> p c f", c=16))
                else:
                    self.load(q, sl, src.rearrange("(c p) d -> p c d", p=128), ap=sl.ap.rearrange("p (c d) -> p c d", c=8))
                wpieces[kk] = sl
                wstate[0] += 1

        ensure(1)
        m2 = self.mark()
        work = Tl(yr.t[0].ap[0:16, :], yr.t[0].b, None)
        work2 = self.tile([16, 288], F32)
        vals = self.tile([16, 288], F32)
        idx = self.tile([16, 288], U32)
        idxf = work2

        def topk(src0, wk, rounds, o0):
            for r in range(rounds):
                src = src0 if r == 0 else wk.ap
                vs = vals[:, o0 + r * 8:o0 + (r + 1) * 8]
                ix = idx[:, o0 + r * 8:o0 + (r + 1) * 8]
                rd = [self.affT.b, wk.b]
                self.P.op("dve", lambda e, vs=vs, src=src: e.max(out=vs, in_=src), reads=rd, writes=[vals.b])
                self.P.op("dve", lambda e, vs=vs, ix=ix, src=src: e.max_index(out=ix, in_max=vs, in_values=src), reads=rd + [vals.b], writes=[idx.b])
                if r < rounds - 1:
                    self.P.op("dve", lambda e, vs=vs, src=src, wk=wk: e.match_replace(out=wk.ap, in_to_replace=vs, in_values=src, imm_value=-1.0),
                              reads=rd + [vals.b], writes=[wk.b])
        topk(self.affT[:, 0:S], work, 32, 0)
        if l == 0:
            topk(self.affT[:, S:T], Tl(work2.ap[:, 0:C], work2.b, None), 4, 256)
        self.copy("dve", idxf[:, 0:nj], idx[:, 0:nj], [idx.b], [idxf.b])
        if l == 0:
            self.P.op("dve", lambda e: e.tensor_scalar(idxf[:, 256:288], idxf[:, 256:288], float(S), None, ALU.add), reads=[idxf.b], writes=[idxf.b])
        for jb, (j0, n) in enumerate(JB):
            ps = self.pr.next()
            self.P.op("pe", lambda e, ps=ps, j0=j0, n=n: e.transpose(ps[0:n, 0:16], idxf[:, j0:j0 + n], self.idf[0:16, 0:16]), reads=[idxf.b, self.idf.b], writes=[ps.b])
            self.copy("dve", self.idxT[0:n, jb, :], ps[0:n, 0:16], [ps.b], [self.idxT.b])
            ps = self.pr.next()
            self.P.op("pe", lambda e, ps=ps, j0=j0, n=n: e.transpose(ps[0:n, 0:16], vals[:, j0:j0 + n], self.idf[0:16, 0:16]), reads=[vals.b, self.idf.b], writes=[ps.b])
            self.copy("act", self.gateT[0:n, jb, :], ps[0:n, 0:16], [ps.b], [self.gateT.b])
        self.release(m2)
        xs_next = gather(0)
        for e in range(16):
            xs_l = xs_next
            xt = xtr.next()
            for jb, (j0, n) in enumerate(JB):
                xs = xs_l[jb]
                for half in range(2):
                    ps = self.pr.next()
                    pv = ps.ap.bitcast(BF16)
                    for j in range(8):
                        fc = half * 8 + j
                        self.P.op("pe", lambda en, pv=pv, j=j, fc=fc, xs=xs, n=n: en.transpose(pv[:, j * 128:j * 128 + n], xs[0:n, fc * 128:(fc + 1) * 128], self.identb[0:n, 0:n]),
                                  reads=[xs.b, self.cb.b], writes=[ps.b])
                    self.copy("act" if half == 0 else "dve", xt[:, half * 8:(half + 1) * 8, j0:j0 + n],
                              pv.rearrange("p (c t) -> p c t", c=8)[:, :, 0:n], [ps.b], [xt.b])
            for fn in pending:
                fn()
            pending = []
            ensure(3 * e + 3)
            wg, wu, wd = wpieces[3 * e], wpieces[3 * e + 1], wpieces[3 * e + 2]
            wg3 = wg.ap.rearrange("p (c f) -> p c f", c=16)
            wu3 = wu.ap.rearrange("p (c f) -> p c f", c=16)
            wd3 = wd.ap.rearrange("p (c d) -> p c d", c=8)
            hd = hdr.next()
            for Fc in range(8):
                psg, psu = self.pr.next(), self.pr.next()
                for kc in range(16):
                    self.mm(psg, psg[:, 0:nj], wg3[:, kc, Fc * 128:(Fc + 1) * 128], xt[:, kc, 0:nj], kc == 0, kc == 15, [wg.b, xt.b])
                for kc in range(16):
                    self.mm(psu, psu[:, 0:nj], wu3[:, kc, Fc * 128:(Fc + 1) * 128], xt[:, kc, 0:nj], kc == 0, kc == 15, [wu.b, xt.b])
                sg = sgr.next()
                self.act(sg[:, 0:nj], psg[:, 0:nj], AF.Silu, [psg.b], [sg.b])
                self.tt("dve", hd[:, Fc, 0:nj], psu[:, 0:nj], sg[:, 0:nj], ALU.mult, [psu.b, sg.b], [hd.b])
            if e + 1 < 16:
                xs_next = gather(e + 1)
                ensure(3 * e + 5)
            for jb, (j0, n) in enumerate(JB):
                y = yr.next()
                ga = gas[0 if jb < 2 else 1]
                for dc in range(4):
                    ps = self.pr.next()
                    for Fc in range(8):
                        self.mm(ps, ps[0:n, :], hd[:, Fc, j0:j0 + n], wd3[:, Fc, dc * 512:(dc + 1) * 512], Fc == 0, Fc == 7, [hd.b, wd.b])
                    self.stt(y[0:n, dc * 512:(dc + 1) * 512], ps[0:n, :], self.gateT[0:n, jb, e:e + 1], ga[0:n, dc * 512:(dc + 1) * 512],
                             ALU.mult, ALU.mult, [ps.b, self.gateT.b, ga.b], [y.b])
                pending.append(lambda y=y, n=n, jb=jb, e=e: self.P.dma("pool", y.tag, lambda en: en.indirect_dma_start(
                    out=self.XR.ap(), out_offset=bass.IndirectOffsetOnAxis(ap=self.idxT[0:n, jb, e:e + 1], axis=0),
                    in_=y[0:n, :], in_offset=None, compute_op=ALU.add),
                    reads=[y.b, self.idxT.b, xrb], writes=[xrb]))
        for fn in pending:
            fn()
        self.release(m)

    def layer1_mixer(self):
        Ld = self.L[1]
        m0 = self.mark()
        HT, HTb = self.alloc_HT()
        self.norm(1, 1, NB, HT, HTb, False)
        m = self.mark()
        TG = [(tg * 512, 512) for tg in range(4)] + [(2048, 256)]

        def hb_of(t0, n):
            return HTb[t0 // 128:(t0 + n) // 128]
        gq = self.tile([128, 12], F32, "gq")
        self.load("sp", gq, Ld["gq"].ap())
        gkv = self.tile([128, 4], F32, "gkv")
        self.load("sp", gkv, Ld["gkv"].ap())
        cs = self.tile([64, 2, S], BF16, "cs")
        self.load("pool", cs, self.cs1_d.ap().rearrange("c p t -> p c t"))
        wq = [self.tile([128, 16, 512], BF16, f"wq{i}") for i in range(4)]
        for i in range(4):
            self.load("pool", wq[i], Ld["w_dqkv"][:, i * 512:(i + 1) * 512].rearrange("(c p) n -> p c n", p=128))
        wk = self.tile([128, 16, 64], BF16, "wk")
        self.load("pool", wk, Ld["w_dqkv"][:, 2048:2112].rearrange("(c p) n -> p c n", p=128))
        raw = self.tile([128, 12, 512], F32)
        rawb = [self.P.buf(f"raw{i}") for i in range(12)]
        sqr = self.ring("sq", [128, 512], BF16, 3)
        sd = self.tile([128, 512], F32)
        rstd = self.tile([128, 512], F32)
        cqr = self.ring("cq", [128, 512], BF16, 3)
        rr = self.rope_rings()

        def lora(chunks, groups, gcol, nfeat, dst):
            nch = len(chunks)
            for (t0, n) in groups:
                pss = self.pst[7]
                stl = {}

                def lA(ci, t0=t0, n=n):
                    w, j = chunks[ci]
                    ps = self.proj_fm(w, j * 128, 128, HT, hb_of(t0, n), 16, t0, n)
                    self.copy("act", raw[:, ci, 0:n], ps[:, 0:n], [ps.b], [rawb[ci]])
                    sq = sqr.next()
                    self.tt("pool", sq[:, 0:n], raw[:, ci, 0:n], raw[:, ci, 0:n], ALU.mult, [rawb[ci]], [sq.b])
                    stl[ci] = sq

                def lB(ci, n=n):
                    self.bg_step()
                    sq = stl.pop(ci)
                    self.mm(pss, pss[:, 0:n], self.ones1, sq[:, 0:n], ci == 0, ci == nch - 1, [sq.b, self.cb.b])
                self.pipeline(nch, [lA, lB])
                self.act(sd[:, 0:n], pss[:, 0:n], AF.Sqrt, [pss.b], [sd.b], bias=EPS_AP(self), scale=1.0 / nfeat)
                self.recip(rstd[:, 0:n], sd[:, 0:n], [sd.b], [rstd.b])
                for ci in range(nch):
                    cq = cqr.next()
                    self.stt(cq[:, 0:n], raw[:, ci, 0:n], gcol[:, ci:ci + 1], rstd[:, 0:n], ALU.mult, ALU.mult, [rawb[ci], rstd.b, gcol.b], [cq.b])
                    self.store("sp", cq, dst[ci * 128:(ci + 1) * 128, t0:t0 + n], ap=cq[:, 0:n])
        lora([(wq[i // 4], i % 4) for i in range(12)], TG[:4], gq, 1536.0, self.CQT)
        lora([(wq[3], i) for i in range(4)], TG, gkv, 512.0, self.CKVT)
        stk = {}

        def kA(i):
            t0, n = TG[i]
            ps = self.proj_fm(wk, 0, 64, HT, hb_of(t0, n), 16, t0, n)
            qn = rr["qn"].next()
            self.copy("act", qn[0:64, 0:n], ps[0:64, 0:n], [ps.b], [qn.b])
            stk[i] = qn

        def kB(i):
            t0, n = TG[i]
            self.rope_tail(stk.pop(i), 64, n, t0, cs, self.rot64, self.KPET[:, t0:t0 + n], rr, is_ctx=(t0 >= S))
        self.pipeline(len(TG), [kA, kB])
        self.release(m)
        self.release(m0)
        m = self.mark()
        cs = self.tile([64, 2, S], BF16, "cs")
        self.load("pool", cs, self.cs1_d.ap().rearrange("c p t -> p c t"))
        cqt = self.tile([128, 12, S], BF16, "cqt")
        for c in range(12):
            self.load("sp" if c % 2 == 0 else "act", cqt, self.CQT[c * 128:(c + 1) * 128, :], ap=cqt[:, c, :])
        wr = self.ring("w", [128, 12, 192], BF16, 3)
        rr = self.rope_rings()
        qnr = self.ring("qnn", [128, 512], BF16, 3)
        items = [(h, t0, n) for h in range(16) for (t0, n) in TG[:4]]
        wcur = {}

        def getw(h):
            if h not in wcur:
                w = wr.next()
                self.load("pool", w, Ld["w_uq"][:, h * 192:(h + 1) * 192].rearrange("(c p) n -> p c n", p=128))
                wcur[h] = w
            return wcur[h]
        stu = {}

        def uA(i):
            h, t0, n = items[i]
            w = getw(h)
            if t0 == 0 and h + 1 < 16:
                getw(h + 1)
            ps = self.proj_fm(w, 0, 128, cqt.ap, [cqt.b], 12, t0, n)
            qn = qnr.next()
            self.copy("act", qn[:, 0:n], ps[:, 0:n], [ps.b], [qn.b])
            self.store("sp", qn, self.QNT[h * 128:(h + 1) * 128, t0:t0 + n], ap=qn[:, 0:n])
            ps = self.proj_fm(w, 128, 64, cqt.ap, [cqt.b], 12, t0, n)
            qp = rr["qn"].next()
            self.copy("dve", qp[0:64, 0:n], ps[0:64, 0:n], [ps.b], [qp.b])
            stu[i] = qp

        def uB(i):
            self.bg_step()
            h, t0, n = items[i]
            self.rope_tail(stu.pop(i), 64, n, t0, cs, self.rot64, self.QPT[h * 64:(h + 1) * 64, t0:t0 + n], rr)
        self.pipeline(len(items), [uA, uB])
        self.release(m)
        m = self.mark()
        ckt = self.tile([128, 4, T], BF16, "ckt")
        self.load("sp", ckt, self.CKVT.ap().rearrange("(c p) t -> p c t", p=128))
        W = self.tile([128, 4, 4096], BF16, "W")
        self.load("pool", W, Ld["w_ukv"].ap().rearrange("(c p) n -> p c n", p=128))
        knr = self.ring("kn", [128, 512], BF16, 3)
        for h in range(16):
            self.bg_step()
            for (t0, n) in TG:
                ps = self.proj_fm(W, h * 256, 128, ckt.ap, [ckt.b], 4, t0, n)
                kn = knr.next()
                self.copy("act" if h % 2 == 0 else "dve", kn[:, 0:n], ps[:, 0:n], [ps.b], [kn.b])
                self.store("sp", kn, self.KNT[h * 128:(h + 1) * 128, t0:t0 + n], ap=kn[:, 0:n])
        vr = self.ring("v", [128, 512], BF16, 3)
        for tb in range(NB):
            for hg in range(4):
                ps = self.pr.next()
                for hh in range(4):
                    h = hg * 4 + hh
                    for c in range(4):
                        self.mm(ps, ps[:, hh * 128:(hh + 1) * 128], ckt[:, c, tb * 128:(tb + 1) * 128], W[:, c, h * 256 + 128:h * 256 + 256], c == 0, c == 3, [ckt.b, W.b])
                v = vr.next()
                self.copy("act" if hg % 2 == 0 else "dve", v.ap, ps.ap, [ps.b], [v.b])
                self.store("sp", v, self.V1[tb * 128:(tb + 1) * 128, hg * 512:(hg + 1) * 512])
        self.release(m)
        self.cv_start(1)
        self.cv_div = 1
        kn_aps = [self.KNT[h * 128:(h + 1) * 128, :] for h in range(16)]
        kp_ap = self.KPET.ap()
        self.attention(192 ** -0.5, 16, lambda h: (h, kn_aps[h], kp_ap), lambda h: self.V1[:, h * 128:(h + 1) * 128],
                       lambda h: (self.QNT[h * 128:(h + 1) * 128, :], self.QPT[h * 64:(h + 1) * 64, :]), False, S)
        self.outproj(1, 4)

    def final(self):
        m = self.mark()
        gt = self.tile([128, D], F32, "g")
        self.load("sp", gt, self.fg_d.ap().partition_broadcast(128))
        xr = self.ring("x", [128, D], F32, 2)
        orr = self.ring("fin", [128, D], F32, 2)
        junk = self.tile([128, D], BF16)
        smr = self.ring("sm", [128, 1], F32, 6)
        for tb in range(16):
            x = xr.next()
            self.load("sp", x, self.XR[tb * 128:(tb + 1) * 128, :])
            ss, sq, rs = smr.next(), smr.next(), smr.next()
            self.act(junk.ap, x.ap, AF.Square, [x.b], [junk.b, ss.b], scale=float(D ** -0.5), accum_out=ss.ap)
            self.act(sq.ap, ss.ap, AF.Sqrt, [ss.b], [sq.b], bias=EPS_AP(self), scale=1.0)
            self.recip(rs.ap, sq.ap, [sq.b], [rs.b])
            o = orr.next()
            self.stt(o.ap, x.ap, rs.ap, gt.ap, ALU.mult, ALU.mult, [x.b, rs.b, gt.b], [o.b])
            self.store("act", o, self.out_d[tb * 128:(tb + 1) * 128, :])
        self.release(m)


def EPS_AP(k):
    return k._eps.ap


def _rope_tables(d_rot):
    rows = S // 64
    row = np.repeat(np.arange(rows, dtype=np.float32), 64)
    col = np.tile(np.arange(64, dtype=np.float32), rows)
    n_axis = d_rot // 4
    inv = (np.float32(10000.0) ** (-np.arange(n_axis, dtype=np.float32) / np.float32(n_axis))).astype(np.float32)
    ang = np.concatenate([row[:, None] * inv, col[:, None] * inv], axis=-1).astype(np.float32)
    half = d_rot // 2
    idx = np.arange(d_rot) % half
    cs = np.stack([np.cos(ang)[:, idx].T, np.sin(ang)[:, idx].T]).astype(np.float32)
    return np.ascontiguousarray(cs)


def _rot_lhsT(d_rot, pad=128):
    half = d_rot // 2
    m = np.zeros((pad, pad), np.float32)
    for j in range(half):
        m[j + half, j] = -1.0
        m[j, j + half] = 1.0
    return m


def _colmajor(v, nchunk):
    return np.ascontiguousarray(np.asarray(v, np.float32).reshape(nchunk, 128).T)


def _shared(inputs):
    f = lambda k: np.ascontiguousarray(np.asarray(inputs[k], np.float32))
    sh = {}
    consts = np.zeros((7, 128, 128), np.float32)
    consts[0] = np.eye(128, dtype=np.float32)
    consts[1] = 1.0 / 128
    consts[2] = 1.0 / 1024
    consts[3] = 1.0
    consts[4] = _rot_lhsT(128)
    consts[5] = _rot_lhsT(64)
    sh["consts"] = consts
    sh["cs0"] = _rope_tables(128)
    sh["cs1"] = _rope_tables(64)
    for l in range(2):
        sh[f"l{l}_mod_w"] = f(f"l{l}_mod_w")
        sh[f"l{l}_mod_b"] = f(f"l{l}_mod_b").reshape(1, -1)
        sh[f"l{l}_norm1_g"] = f(f"l{l}_norm1_g").reshape(1, -1)
        sh[f"l{l}_norm2_g"] = f(f"l{l}_norm2_g").reshape(1, -1)
        sh[f"l{l}_router"] = f(f"l{l}_router")
        sh[f"l{l}_w_gate"] = f(f"l{l}_w_gate")
        sh[f"l{l}_w_up"] = f(f"l{l}_w_up")
        sh[f"l{l}_w_down"] = f(f"l{l}_w_down")
        sh[f"l{l}_w_out"] = f(f"l{l}_w_out")
    sh["l0_w_in"] = f("l0_w_in")
    sh["l0_q_norm_g"] = f("l0_q_norm_g").reshape(128, 1)
    sh["l0_k_norm_g"] = f("l0_k_norm_g").reshape(128, 1)
    dw = f("l0_dw_w").reshape(31, 8, 128)
    sh["l0_dwT"] = np.ascontiguousarray(dw.transpose(2, 1, 0))
    sh["l0_cvec"] = np.ascontiguousarray(np.stack([_colmajor(inputs["l0_dw_b"], 8), _colmajor(inputs["l0_conv_ln_g"], 8),
                                                   _colmajor(inputs["l0_conv_ln_b"], 8)], axis=1))
    sh["l1_w_dqkv"] = f("l1_w_dqkv")
    sh["l1_gq"] = _colmajor(inputs["l1_q_lora_norm_g"], 12)
    sh["l1_w_uq"] = f("l1_w_uq")
    sh["l1_gkv"] = _colmajor(inputs["l1_kv_lora_norm_g"], 4)
    sh["l1_w_ukv"] = f("l1_w_ukv")
    sh["final_norm_g"] = f("final_norm_g").reshape(1, -1)
    return sh


def _core_map(inputs, sh, b):
    m = dict(sh)
    m["x"] = np.ascontiguousarray(np.asarray(inputs["x"][b], np.float32))
    m["ctx"] = np.ascontiguousarray(np.asarray(inputs["ctx"][b], np.float32))
    cc = np.stack([np.asarray(inputs["c"][b], np.float32), np.asarray(inputs["c_ctx"], np.float32)], axis=-1)
    m["cT"] = np.ascontiguousarray(cc.reshape(16, 128, 2).transpose(1, 0, 2))
    return m


def kernel(**inputs):
    nc = K().build()
    sh = _shared(inputs)
    nb = np.asarray(inputs["x"]).shape[0]
    in_maps = [_core_map(inputs, sh, b) for b in range(nb)]
    res = run_bass_kernel_spmd(nc, in_maps, core_ids=list(range(nb)))
    return np.stack([np.asarray(r["out"], np.float32) for r in res.results], axis=0)
```

```python
import numpy as np
from concourse.bass_utils import run_bass_kernel_spmd

import contextlib
import concourse.bass as bass
import concourse.mybir as mybir

F32 = mybir.dt.float32
BF16 = mybir.dt.bfloat16
I32 = mybir.dt.int32
U32 = mybir.dt.uint32
AF = mybir.ActivationFunctionType
ALU = mybir.AluOpType
AX = mybir.AxisListType

ENGS = ("pe", "dve", "act", "pool", "sp")
COMPUTE = ("pe", "dve", "act", "pool")


class Buf:
    __slots__ = ("name", "last_w", "readers")

    def __init__(self, name):
        self.name = name
        self.last_w = None
        self.readers = []


class Op:
    __slots__ = ("eng", "fn", "idx", "waits", "signal", "seq", "tag", "tagcnt")

    def __init__(self, eng, fn):
        self.eng = eng
        self.fn = fn
        self.waits = {}
        self.signal = False
        self.seq = None
        self.tag = None
        self.tagcnt = None


class Prog:
    def __init__(self, nc):
        self.nc = nc
        self.ops = {e: [] for e in ENGS}
        self.tags = {}
        self.known = {e: {} for e in ENGS}
        self.barrier_pending = {e: None for e in ENGS}
        self.nbuf = 0

    def buf(self, name=None):
        self.nbuf += 1
        return Buf(name or f"b{self.nbuf}")

    def bufs(self, n, name=None):
        return [self.buf(f"{name}{i}") for i in range(n)]

    def _need(self, op, dep, waw=False):
        if dep is None or dep is op:
            return
        if dep.tag is not None:
            tg = self.tags[dep.tag]
            val = tg[0] * 16
            if op.tag == dep.tag:
                if waw:
                    return
                val = (tg[0] - 1) * 16
            tg[1] = max(tg[1], val)
            key = ("t", dep.tag)
        else:
            if dep.eng == "pe" and op.eng == "pe" and op.tag is None:
                return
            dep.signal = True
            key = ("e", dep.eng)
            val = dep
        cur = op.waits.get(key)
        if cur is None:
            op.waits[key] = val
        elif key[0] == "t":
            op.waits[key] = max(cur, val)
        else:
            op.waits[key] = cur if cur.idx >= val.idx else val

    def _record(self, op, reads, writes):
        lst = self.ops[op.eng]
        op.idx = len(lst)
        lst.append(op)
        bp = self.barrier_pending[op.eng]
        if bp is not None:
            for d in bp[0]:
                self._need(op, d)
            for tag, val in bp[1].items():
                key = ("t", tag)
                op.waits[key] = max(op.waits.get(key, 0), val)
                self.tags[tag][1] = max(self.tags[tag][1], val)
            self.barrier_pending[op.eng] = None
        for b in reads:
            self._need(op, b.last_w)
        for b in writes:
            self._need(op, b.last_w, waw=True)
            for r in b.readers:
                self._need(op, r)
        for b in reads:
            b.readers.append(op)
        for b in writes:
            b.last_w = op
            b.readers = []

    def op(self, eng, fn, reads=(), writes=()):
        o = Op(eng, fn)
        self._record(o, reads, writes)
        return o

    def dma(self, q, tag, fn, reads=(), writes=(), after=()):
        o = Op(q, fn)
        tg = self.tags.setdefault(tag, [0, 0])
        tg[0] += 1
        o.tag = tag
        o.tagcnt = tg[0]
        for (t2, v2) in after:
            tg2 = self.tags.setdefault(t2, [0, 0])
            tg2[1] = max(tg2[1], v2)
            o.waits[("t", t2)] = max(o.waits.get(("t", t2), 0), v2)
        if tg[1] > 0:
            o.waits[("t", tag)] = max(o.waits.get(("t", tag), 0), tg[1])
        self._record(o, reads, writes)
        return o

    def barrier(self):
        lasts = []
        for e in COMPUTE:
            for o in reversed(self.ops[e]):
                if o.tag is None:
                    lasts.append(o)
                    break
        tagvals = {t: v[0] * 16 for t, v in self.tags.items() if v[0] > 0}
        for e in ENGS:
            self.barrier_pending[e] = (lasts, dict(tagvals))

    def emit(self, final_tags=()):
        nc = self.nc
        for e in COMPUTE:
            n = 0
            for o in self.ops[e]:
                if o.signal:
                    n += 1
                    o.seq = n
        with contextlib.ExitStack() as st:
            esem = {e: st.enter_context(nc.semaphore(f"s_{e}")) for e in COMPUTE}
            tsem = {t: st.enter_context(nc.semaphore(f"t_{i}")) for i, t in enumerate(self.tags)}
            block = st.enter_context(nc.Block())

            def run(ename, eng):
                known = {}
                for o in self.ops[ename]:
                    for key, val in o.waits.items():
                        if key[0] == "t":
                            sem, v = tsem[key[1]], val
                        else:
                            sem, v = esem[key[1]], val.seq
                        if known.get(key, 0) >= v:
                            continue
                        known[key] = v
                        eng.wait_ge(sem, v)
                    ins = o.fn(eng)
                    if o.tag is not None:
                        ins.then_inc(tsem[o.tag], 16)
                    elif o.signal:
                        ins.then_inc(esem[ename], 1)
                if ename == "sp":
                    for t in final_tags:
                        eng.wait_ge(tsem[t], self.tags[t][0] * 16)

            @block.sync
            def _(eng):
                run("sp", eng)

            @block.scalar
            def _(eng):
                run("act", eng)

            @block.vector
            def _(eng):
                run("dve", eng)

            @block.gpsimd
            def _(eng):
                run("pool", eng)

            @block.tensor
            def _(eng):
                run("pe", eng)

    def stats(self):
        return {e: len(self.ops[e]) for e in ENGS}, len(self.tags)

D = 2048
S = 2048
C = 256
T = S + C
NB = T // 128
EPS = 1e-6
SBUF_BYTES = 212480
_DS = {F32: 4, BF16: 2, I32: 4, U32: 4}


class Tl:
    __slots__ = ("ap", "b", "tag")

    def __init__(self, ap, b, tag):
        self.ap, self.b, self.tag = ap, b, tag

    def __getitem__(self, k):
        return self.ap[k]


class Ring:
    def __init__(self, tiles):
        self.t = tiles
        self.i = 0

    def next(self):
        t = self.t[self.i % len(self.t)]
        self.i += 1
        return t


class K:
    def __init__(self, stop=None, dumps=(), noinput=()):
        self.noinput = noinput
        nc = bass.Bass("TRN2", target_bir_lowering=False)
        self.nc = nc
        self.P = Prog(nc)
        self.stop = stop
        self.big = nc.alloc_sbuf_tensor("big", [128, SBUF_BYTES], mybir.dt.uint8)
        self.top = 0
        self.ntile = 0
        self.psb = [nc.alloc_psum_tensor(f"ps{i}", [128, 512], F32) for i in range(8)]
        self.pst = [Tl(self.psb[i][:, :], self.P.buf(f"ps{i}"), None) for i in range(8)]
        self.pr = Ring(self.pst[0:6])
        self.din = {}
        self.dumps = dumps

    def inp(self, name, shape, dtype=F32):
        t = self.nc.dram_tensor(name, list(shape), dtype, kind="Internal" if name in self.noinput else "ExternalInput")
        self.din[name] = t
        return t

    def scratch(self, name, shape, dtype):
        kind = "ExternalOutput" if name in self.dumps else "Internal"
        return self.nc.dram_tensor(name, list(shape), dtype, kind=kind)

    def tile(self, shape, dtype, tag=None):
        n = 1
        for s in shape[1:]:
            n *= s
        nbytes = (n * _DS[dtype] + 63) // 64 * 64
        off = self.top
        self.top += nbytes
        assert self.top <= SBUF_BYTES, f"SBUF overflow {self.top}"
        ap = self.big[:, off:off + n * _DS[dtype]].bitcast(dtype)
        if len(shape) == 3:
            ap = ap.rearrange("p (a b) -> p a b", a=shape[1])
        if shape[0] < 128:
            ap = ap[0:shape[0]]
        self.ntile += 1
        return Tl(ap, self.P.buf(f"t{self.ntile}"), tag)

    def ring(self, tag, shape, dtype, n):
        return Ring([self.tile(shape, dtype, f"{tag}{i}") for i in range(n)])

    def mark(self):
        return self.top

    def cv_start(self, l):
        Ld = self.L[l]
        for e in range(16):
            for key in ("wg", "wu", "wd"):
                if key in self.WB[l]:
                    self._cvq.append((Ld[key][e], self.WB[l][key][e]))

    def cv_issue(self, n=1):
        for _ in range(n):
            if not self._cvq:
                return
            src, dst = self._cvq.pop(0)
            self.P.dma("pool", "cv", lambda en, src=src, dst=dst: en.dma_start(
                out=dst.rearrange("(c p) f -> p c f", p=128), in_=src.rearrange("(c p) f -> p c f", p=128)))

    def bg_step(self, k=1):
        self._cvc = getattr(self, "_cvc", 0) + 1
        if self._cvc % self.cv_div == 0:
            self.cv_issue()
        g = getattr(self, "_bg", None)
        self._bgc = getattr(self, "_bgc", 0) + 1
        if self._bgc % getattr(self, "bg_div", 1) != 0:
            return
        for _ in range(k):
            if g is None:
                return
            try:
                next(g)
            except StopIteration:
                self._bg = g = None

    def bg_drain(self):
        while getattr(self, "_bg", None) is not None:
            self.bg_step()

    def release(self, m):
        self.top = m
        self.P.barrier()

    def pipeline(self, n, stages):
        ns = len(stages)
        for step in range(n + ns - 1):
            for si in reversed(range(ns)):
                i = step - si
                if 0 <= i < n:
                    stages[si](i)

    def dma(self, q, tag, out, in_, reads=(), writes=()):
        return self.P.dma(q, tag, lambda e: e.dma_start(out=out, in_=in_), reads=reads, writes=writes)

    def load(self, q, t, src, ap=None, reads=()):
        return self.dma(q, t.tag, t.ap if ap is None else ap, src, reads=reads, writes=[t.b])

    def store(self, q, t, dst, ap=None, extra_w=()):
        return self.dma(q, t.tag, dst, t.ap if ap is None else ap, reads=[t.b], writes=list(extra_w))

    def mm(self, ps, out, lhsT, rhs, start, stop, reads):
        return self.P.op("pe", lambda e: e.matmul(out, lhsT, rhs, start=start, stop=stop), reads=reads, writes=[ps.b])

    def act(self, out, in_, func, reads, writes, **kw):
        return self.P.op("act", lambda e: e.activation(out, in_, func, **kw), reads=reads, writes=writes)

    def tt(self, eng, out, a, b, op, reads, writes):
        return self.P.op(eng, lambda e: e.tensor_tensor(out, a, b, op), reads=reads, writes=writes)

    def stt(self, out, in0, scalar, in1, op0, op1, reads, writes):
        return self.P.op("dve", lambda e: e.scalar_tensor_tensor(out, in0, scalar, in1, op0, op1), reads=reads, writes=writes)

    def recip(self, out, in_, reads, writes):
        return self.P.op("dve", lambda e: e.reciprocal(out, in_), reads=reads, writes=writes)

    def copy(self, eng, out, in_, reads, writes):
        if eng == "act":
            return self.P.op("act", lambda e: e.copy(out, in_), reads=reads, writes=writes)
        return self.P.op(eng, lambda e: e.tensor_copy(out, in_), reads=reads, writes=writes)

    def build(self):
        nc = self.nc
        I = self.inp
        self.x_d = I("x", [S, D])
        self.ctx_d = I("ctx", [C, D])
        self.cT_d = I("cT", [128, 16, 2])
        self.cst_d = I("consts", [7, 128, 128])
        self.cs0_d = I("cs0", [2, 128, S])
        self.cs1_d = I("cs1", [2, 64, S])
        L = []
        for l in range(2):
            d = dict(mod_w=I(f"l{l}_mod_w", [D, 6 * D]), mod_b=I(f"l{l}_mod_b", [1, 6 * D]),
                     g1=I(f"l{l}_norm1_g", [1, D]), g2=I(f"l{l}_norm2_g", [1, D]),
                     router=I(f"l{l}_router", [D, 16]), wg=I(f"l{l}_w_gate", [16, D, 1024]),
                     wu=I(f"l{l}_w_up", [16, D, 1024]), wd=I(f"l{l}_w_down", [16, 1024, D]),
                     w_out=I(f"l{l}_w_out", [D, D]))
            L.append(d)
        L[0].update(w_in=I("l0_w_in", [D, 3584]), qg=I("l0_q_norm_g", [128, 1]), kg=I("l0_k_norm_g", [128, 1]),
                    dwT=I("l0_dwT", [128, 8, 31]), cvec=I("l0_cvec", [128, 3, 8]))
        L[1].update(w_dqkv=I("l1_w_dqkv", [D, 2112]), gq=I("l1_gq", [128, 12]), w_uq=I("l1_w_uq", [1536, 3072]),
                    gkv=I("l1_gkv", [128, 4]), w_ukv=I("l1_w_ukv", [512, 4096]))
        self.fg_d = I("final_norm_g", [1, D])
        self.L = L
        self.out_d = nc.dram_tensor("out", [S, D], F32, kind="ExternalOutput")
        sc = self.scratch
        self.XR = sc("XR", [T, D], F32)
        self.ADA = [sc(f"ADA{l}", [2, 6 * D], F32) for l in range(2)]
        self.QT = sc("QT", [1024, T], BF16)
        self.KT = sc("KT", [256, T], BF16)
        self.V0 = sc("V0", [T, 256], BF16)
        self.UG = sc("UG", [1024, T], BF16)
        self.AT = sc("AT", [D, T], BF16)
        self.H2 = sc("H2", [T, D], BF16)
        self.CQT = sc("CQT", [1536, S], BF16)
        self.CKVT = sc("CKVT", [512, T], BF16)
        self.KPET = sc("KPET", [64, T], BF16)
        self.QNT = sc("QNT", [2048, S], BF16)
        self.QPT = sc("QPT", [1024, S], BF16)
        self.KNT = sc("KNT", [2048, T], BF16)
        self.V1 = sc("V1", [T, 2048], BF16)
        self.WB = [dict(wg=sc("WG0", [16, D, 1024], BF16), wu=sc("WU0", [16, D, 1024], BF16)),
                   dict(wg=sc("WG1", [16, D, 1024], BF16), wu=sc("WU1", [16, D, 1024], BF16), wd=sc("WD1", [16, 1024, D], BF16))]
        self._cvq = []
        self.cv_div = 3

        cb = self.tile([128, 7, 128], BF16, "cb")
        self.load("pool", cb, self.cst_d.ap().rearrange("c p f -> p c f"))
        self.cb = cb
        self.identb, self.o128, self.o1024, self.ones1 = cb[:, 0, :], cb[:, 1, :], cb[:, 2, :], cb[:, 3, :]
        self.rot128, self.rot64 = cb[:, 4, :], cb[0:64, 5, 0:64]
        idf = self.tile([128, 128], F32, "idf")
        self.load("sp", idf, self.cst_d[0])
        self.idf = idf
        self.affT = self.tile([16, T], F32)
        ept = self.tile([128, 1], F32)
        self.P.op("dve", lambda e: e.memset(ept.ap, EPS), writes=[ept.b])
        self._eps = ept
        self.idxT = self.tile([128, 3, 16], I32)
        self.gateT = self.tile([128, 3, 16], F32)

        m = self.mark()
        r = self.ring("x", [128, D], F32, 3)
        for tb in range(NB):
            t = r.next()
            src = self.x_d[tb * 128:(tb + 1) * 128, :] if tb < 16 else self.ctx_d[(tb - 16) * 128:(tb - 15) * 128, :]
            self.load("sp", t, src)
            self.store("act", t, self.XR[tb * 128:(tb + 1) * 128, :])
        self.release(m)

        def ada_then_mix0():
            m_bg = self.mark()
            self.ada_setup()
            for _ in self.ada_gen([(0, n) for n in range(8)]):
                pass
            self.P.barrier()
            self._bg = self.ada_gen([(0, n) for n in range(8, 24)] + [(1, n) for n in range(24)])
            self.layer0_mixer(m_bg)

        steps = [
            ("mix0", ada_then_mix0), ("moe0", lambda: self.moe(0)),
            ("mix1", self.layer1_mixer), ("moe1", lambda: self.moe(1)),
        ]
        for name, fn in steps:
            fn()
            if self.stop == name:
                break
        self.final()
        self.P.emit(final_tags=["fin0", "fin1"])
        return nc

    def ada_setup(self):
        ct = self.tile([128, 16, 2], F32, "c")
        sct = self.tile([128, 16, 2], BF16)
        self.load("sp", ct, self.cT_d.ap())
        self.act(sct.ap, ct.ap, AF.Silu, [ct.b], [sct.b])
        self._ada = dict(sct=sct, wr=self.ring("aw", [128, 16, 512], BF16, 2),
                         br=self.ring("ab", [2, 512], F32, 2), orr=self.ring("ao", [2, 512], F32, 1))

    def ada_gen(self, jobs):
        A = self._ada
        sct = A["sct"]
        loaded = {}

        def issue(idx):
            l, n = jobs[idx]
            Ld = self.L[l]
            w = A["wr"].next()
            self.load("pool", w, Ld["mod_w"][:, n * 512:(n + 1) * 512].rearrange("(c p) n -> p c n", p=128))
            bt = A["br"].next()
            self.load("sp", bt, Ld["mod_b"][0:1, n * 512:(n + 1) * 512].partition_broadcast(2))
            loaded[idx] = (w, bt)
        issue(0)
        for idx in range(len(jobs)):
            if idx + 1 < len(jobs):
                issue(idx + 1)
            l, n = jobs[idx]
            w, bt = loaded.pop(idx)
            ps = self.pst[7]
            for kc in range(16):
                self.mm(ps, ps[0:2, :], sct[:, kc, :], w[:, kc, :], kc == 0, kc == 15, [sct.b, w.b])
            o = A["orr"].next()
            self.tt("dve", o.ap, ps[0:2, :], bt.ap, ALU.add, [ps.b, bt.b], [o.b])
            self.store("sp", o, self.ADA[l][:, n * 512:(n + 1) * 512])
            yield

    def norm(self, l, which, nblk, HT, HTb, write_tok, post=()):
        m = self.mark()
        Ld = self.L[l]
        o_sh, o_sc = (0, 2048) if which == 1 else (6144, 8192)
        t1r = self.ring("t1", [128, D], F32, 2)
        xr = self.ring("x", [128, D], F32, 3)
        gt = t1r.t[0]
        self.load("sp", gt, Ld["g1" if which == 1 else "g2"].ap().partition_broadcast(128))
        mods = []
        for r in range(2 if nblk > 16 else 1):
            dt_ = F32 if r == 0 else BF16
            a = self.tile([128, D], dt_, f"a{r}")
            sh = self.tile([128, D], dt_, f"s{r}")
            if r == 0:
                self.load("sp", a, self.ADA[l][r:r + 1, o_sc:o_sc + D].partition_broadcast(128))
                self.load("sp", sh, self.ADA[l][r:r + 1, o_sh:o_sh + D].partition_broadcast(128))
                self.stt(a.ap, a.ap, 1.0, gt.ap, ALU.add, ALU.mult, [a.b, gt.b], [a.b])
            else:
                ta, ts = xr.t[0], xr.t[1]
                self.load("sp", ta, self.ADA[l][r:r + 1, o_sc:o_sc + D].partition_broadcast(128))
                self.load("sp", ts, self.ADA[l][r:r + 1, o_sh:o_sh + D].partition_broadcast(128))
                self.stt(a.ap, ta.ap, 1.0, gt.ap, ALU.add, ALU.mult, [ta.b, gt.b], [a.b])
                self.copy("pool", sh.ap, ts.ap, [ts.b], [sh.b])
            mods.append((a, sh))
        hbr = self.ring("hb", [128, D], BF16, 4)
        smr = self.ring("sm", [128, 1], F32, 15)
        tpr = Ring(self.pst[0:4])
        st = {}

        def s0(tb):
            x = xr.next()
            self.load("sp" if tb % 2 == 0 else "act", x, self.XR[tb * 128:(tb + 1) * 128, :])
            st[tb] = dict(x=x, hb=hbr.next())

        def s1(tb):
            x, hb = st[tb]["x"], st[tb]["hb"]
            ss, sq = smr.next(), smr.next()
            self.act(hb.ap, x.ap, AF.Square, [x.b], [hb.b, ss.b], scale=float(D ** -0.5), accum_out=ss.ap)
            self.act(sq.ap, ss.ap, AF.Sqrt, [ss.b], [sq.b], bias=EPS_AP(self), scale=1.0)
            st[tb]["sq"] = sq

        def s2(tb):
            a, sh = mods[0 if tb < 16 else 1]
            x, sq = st[tb]["x"], st[tb]["sq"]
            rs = smr.next()
            self.recip(rs.ap, sq.ap, [sq.b], [rs.b])
            t1 = t1r.t[tb % 2]
            self.stt(t1.ap, x.ap, rs.ap, a.ap, ALU.mult, ALU.mult, [x.b, rs.b, a.b], [t1.b])

        def s3(tb):
            a, sh = mods[0 if tb < 16 else 1]
            t1, hb = t1r.t[tb % 2], st[tb]["hb"]
            self.tt("pool", hb.ap, t1.ap, sh.ap, ALU.add, [t1.b, sh.b], [hb.b])
            if write_tok:
                self.store("sp", hb, self.H2[tb * 128:(tb + 1) * 128, :])

        def s4(tb):
            hb = st[tb]["hb"]
            pss = []
            for half in range(2):
                ps = tpr.next()
                pv = ps.ap.bitcast(BF16)
                for j in range(8):
                    fc = half * 8 + j
                    self.P.op("pe", lambda e, pv=pv, j=j, fc=fc, hb=hb: e.transpose(pv[:, j * 128:(j + 1) * 128], hb[:, fc * 128:(fc + 1) * 128], self.identb),
                              reads=[hb.b, self.cb.b], writes=[ps.b])
                pss.append((ps, pv))
            st[tb]["ps"] = pss

        def s5(tb):
            for half, (ps, pv) in enumerate(st[tb]["ps"]):
                self.copy("act" if half == 0 else "dve", HT[:, half * 8:(half + 1) * 8, tb * 128:(tb + 1) * 128],
                          pv.rearrange("p (c t) -> p c t", c=8), [ps.b], [HTb[tb]])
            self.bg_step()
        self.pipeline(nblk, [s0, s1, s2, s3, s4, s5] + list(post))
        self.release(m)

    def alloc_HT(self):
        HT = self.tile([128, 16, T], BF16)
        HTb = [self.P.buf(f"HT{i}") for i in range(NB)]
        return HT.ap, HTb

    def proj_fm(self, w, c0, mp, src, srcb, nk, t0, n):
        ps = self.pr.next()
        for kc in range(nk):
            self.mm(ps, ps[0:mp, 0:n], w[:, kc, c0:c0 + mp], src[:, kc, t0:t0 + n], kc == 0, kc == nk - 1, [w.b] + srcb)
        return ps

    def rope_tail(self, qn, mp, n, t0, cs, rot, dst, rings, is_ctx=False):
        if is_ctx:
            self.store("sp", qn, dst, ap=qn[0:mp, 0:n])
            return
        ps3 = self.pr.next()
        self.mm(ps3, ps3[0:mp, 0:n], rot, qn[0:mp, 0:n], True, True, [qn.b, self.cb.b])
        t1, t2, qo = rings["t1"].next(), rings["t2"].next(), rings["qo"].next()
        self.tt("pool", t1[0:mp, 0:n], qn[0:mp, 0:n], cs[0:mp, 0, t0:t0 + n], ALU.mult, [qn.b, cs.b], [t1.b])
        self.tt("dve", t2[0:mp, 0:n], ps3[0:mp, 0:n], cs[0:mp, 1, t0:t0 + n], ALU.mult, [ps3.b, cs.b], [t2.b])
        self.tt("pool", qo[0:mp, 0:n], t1[0:mp, 0:n], t2[0:mp, 0:n], ALU.add, [t1.b, t2.b], [qo.b])
        self.store("sp", qo, dst, ap=qo[0:mp, 0:n])

    def rope_rings(self):
        return dict(qn=self.ring("qn", [128, 512], BF16, 3), t1=self.ring("rt1", [128, 512], F32, 2),
                    t2=self.ring("rt2", [128, 512], F32, 2), qo=self.ring("qo", [128, 512], BF16, 2))

    def layer0_mixer(self, m_bg):
        Ld = self.L[0]
        m0 = self.mark()
        HT, HTb = self.alloc_HT()
        self.bg_div = 3
        self.norm(0, 1, NB, HT, HTb, False)
        self.bg_div = 3
        m = self.mark()
        gcol = self.tile([128, 2], F32, "gc")
        self.load("sp", gcol, Ld["qg"].ap(), ap=gcol[:, 0:1])
        self.load("sp", gcol, Ld["kg"].ap(), ap=gcol[:, 1:2])
        cs = self.tile([128, 2, S], BF16, "cs")
        self.load("pool", cs, self.cs0_d.ap().rearrange("c p t -> p c t"))
        wr = self.ring("w", [128, 16, 512], BF16, 3)
        rr = self.rope_rings()
        sqr = self.ring("sq", [128, 512], BF16, 3)
        sdr = self.ring("sd", [128, 512], F32, 1)
        rsr = self.ring("rs", [128, 512], F32, 2)
        TG = [(tg * 512, 512) for tg in range(4)] + [(2048, 256)]

        def hb_of(t0, n):
            return HTb[t0 // 128:(t0 + n) // 128]

        def loadw(p):
            w = wr.next()
            self.load("pool", w, Ld["w_in"][:, p * 512:(p + 1) * 512].rearrange("(c p) n -> p c n", p=128))
            return w

        items = []
        for p in range(2):
            for j in range(4):
                h = p * 4 + j
                for (t0, n) in TG:
                    items.append((p, j, gcol[:, 0:1], self.QT[h * 128:(h + 1) * 128, :], t0, n))
        for j in range(2):
            for (t0, n) in TG:
                items.append((2, j, gcol[:, 1:2], self.KT[j * 128:(j + 1) * 128, :], t0, n))
        wcur = {}

        def getw(p):
            if p not in wcur:
                wcur[p] = loadw(p)
            return wcur[p]
        stq = {}

        def qA(i):
            p, j, g_ap, dst, t0, n = items[i]
            w = getw(p)
            if j == 0 and t0 == 0 and p < 2:
                getw(p + 1)
            ps = self.proj_fm(w, j * 128, 128, HT, hb_of(t0, n), 16, t0, n)
            sq = sqr.next()
            self.act(sq[:, 0:n], ps[:, 0:n], AF.Square, [ps.b], [sq.b])
            stq[i] = [ps, sq]

        def qB(i):
            p, j, g_ap, dst, t0, n = items[i]
            ps, sq = stq[i]
            ps2 = self.pr.next()
            self.mm(ps2, ps2[:, 0:n], self.o128, sq[:, 0:n], True, True, [sq.b, self.cb.b])
            sd = sdr.next()
            self.act(sd[:, 0:n], ps2[:, 0:n], AF.Sqrt, [ps2.b], [sd.b], bias=EPS_AP(self), scale=1.0)
            rs = rsr.next()
            self.recip(rs[:, 0:n], sd[:, 0:n], [sd.b], [rs.b])
            qn = rr["qn"].next()
            self.stt(qn[:, 0:n], ps[:, 0:n], g_ap, rs[:, 0:n], ALU.mult, ALU.mult, [ps.b, rs.b, gcol.b], [qn.b])
            stq[i] = qn

        def qC(i):
            p, j, g_ap, dst, t0, n = items[i]
            self.rope_tail(stq.pop(i), 128, n, t0, cs, self.rot128, dst[:, t0:t0 + n], rr, is_ctx=(t0 >= S))
            self.bg_step()
        self.pipeline(len(items), [qA, qB, qC])
        w = getw(2)
        wun, wgn = loadw(3), loadw(5)
        vr = self.ring("v", [128, 256], BF16, 2)
        for tb in range(NB):
            ps = self.pr.next()
            for kc in range(16):
                self.mm(ps, ps[:, 0:256], HT[:, kc, tb * 128:(tb + 1) * 128], w[:, kc, 256:512], kc == 0, kc == 15, [w.b, HTb[tb]])
            v = vr.next()
            self.copy("act", v.ap, ps[:, 0:256], [ps.b], [v.b])
            self.store("sp", v, self.V0[tb * 128:(tb + 1) * 128, :])
        sgr = self.ring("sg", [128, 512], BF16, 2)
        ugr = self.ring("ug", [128, 512], BF16, 2)
        for p in range(2):
            if p == 1:
                wgn = loadw(6)
            wu, wg = wun, wgn
            if p == 0:
                wun = loadw(4)
            for j in range(4):
                c = p * 4 + j
                for (t0, n) in TG:
                    self.bg_step()
                    psu = self.proj_fm(wu, j * 128, 128, HT, hb_of(t0, n), 16, t0, n)
                    psg = self.proj_fm(wg, j * 128, 128, HT, hb_of(t0, n), 16, t0, n)
                    sg = sgr.next()
                    self.act(sg[:, 0:n], psg[:, 0:n], AF.Sigmoid, [psg.b], [sg.b])
                    ug = ugr.next()
                    self.tt("dve", ug[:, 0:n], psu[:, 0:n], sg[:, 0:n], ALU.mult, [psu.b, sg.b], [ug.b])
                    self.store("sp", ug, self.UG[c * 128:(c + 1) * 128, t0:t0 + n], ap=ug[:, 0:n])
        self.release(m0)
        self.bg_div = 1
        self.cv_start(0)
        self.attention(128 ** -0.5, 8, lambda h: (h // 4, self.KT[(h // 4) * 128:(h // 4 + 1) * 128, :], None),
                       lambda h: self.V0[:, (h // 4) * 128:(h // 4 + 1) * 128],
                       lambda h: (self.QT[h * 128:(h + 1) * 128, :], None), True, T)
        self.bg_drain()
        self.release(m_bg)
        if self.stop == "attn0":
            return
        self.conv0()
        self.outproj(0, 5)

    def attention(self, scale, nheads, k_src, v_src, q_src, with_ctx_q, qlen):
        m = self.mark()
        ktr = self.ring("kt", [128, T], BF16, 2)
        kpr = self.ring("kp", [64, T], BF16, 1)
        vr = self.ring("vv", [128, NB, 128], BF16, 2)
        qtr = self.ring("qt", [128, qlen], BF16, 2)
        qpr = self.ring("qp", [64, qlen], BF16, 2)
        ptr_ = self.ring("pT", [128, 512], BF16, 4)
        rsr = self.ring("rs", [128, 512], F32, 2)
        orr = self.ring("o", [128, 512], BF16, 2)
        Sr = Ring(self.pst[0:3])
        Or = Ring(self.pst[3:5])
        Ur = Ring(self.pst[5:7])
        groups = [(qg * 512, 512, list(range(NB))) for qg in range(4)]
        if with_ctx_q:
            groups.append((2048, 256, [16, 17]))
        kp = None
        last_k = None
        for h in range(nheads):
            kkey, ks, kps = k_src(h)
            if kkey != last_k:
                kt = ktr.next()
                self.load("sp", kt, ks)
                v = vr.next()
                self.load("act", v, v_src(h).rearrange("(b p) d -> p b d", p=128))
                last_k = kkey
            if kps is not None and kp is None:
                kp = kpr.next()
                self.load("sp", kp, kps)
            qs, qps = q_src(h)
            qt = qtr.next()
            self.load("sp", qt, qs)
            qp = None
            if qps is not None:
                qp = qpr.next()
                self.load("act", qp, qps)
            for (q0, n, kbs) in groups:
                self.bg_step()
                pso, psu = Or.next(), Ur.next()

                def smm(kb):
                    s = Sr.next()
                    rd = [kt.b, qt.b]
                    self.mm(s, s[:, 0:n], kt[:, kb * 128:(kb + 1) * 128], qt[:, q0:q0 + n], True, qp is None, rd)
                    if qp is not None:
                        self.mm(s, s[:, 0:n], kp[:, kb * 128:(kb + 1) * 128], qp[:, q0:q0 + n], False, True, [kp.b, qp.b])
                    return s
                pend = [smm(kb) for kb in kbs[:2]]
                for i, kb in enumerate(kbs):
                    s = pend.pop(0)
                    p = ptr_.next()
                    self.act(p[:, 0:n], s[:, 0:n], AF.Exp, [s.b], [p.b], scale=float(scale))
                    self.mm(pso, pso[:, 0:n], v[:, kb, :], p[:, 0:n], i == 0, i == len(kbs) - 1, [v.b, p.b])
                    self.mm(psu, psu[:, 0:n], self.ones1, p[:, 0:n], i == 0, i == len(kbs) - 1, [p.b, self.cb.b])
                    if i + 2 < len(kbs):
                        pend.append(smm(kbs[i + 2]))
                r = rsr.next()
                self.recip(r[:, 0:n], psu[:, 0:n], [psu.b], [r.b])
                o = orr.next()
                self.tt("dve", o[:, 0:n], pso[:, 0:n], r[:, 0:n], ALU.mult, [pso.b, r.b], [o.b])
                self.store("sp", o, self.AT[h * 128:(h + 1) * 128, q0:q0 + n], ap=o[:, 0:n])
        self.release(m)

    def conv0(self):
        Ld = self.L[0]
        m = self.mark()
        PW = 15 + S + 15 + 15 + C + 15
        CS0 = S + 30
        ugt = self.tile([128, 8, PW], BF16, "ugt")
        zb = self.P.buf("ugt_zero")
        self.P.op("pool", lambda e: e.memset(ugt.ap, 0.0), writes=[ugt.b, zb])
        for c in range(8):
            self.load("sp", ugt, self.UG[c * 128:(c + 1) * 128, 0:S], ap=ugt[:, c, 15:15 + S], reads=[zb])
            self.load("act", ugt, self.UG[c * 128:(c + 1) * 128, S:T], ap=ugt[:, c, CS0 + 15:CS0 + 15 + C], reads=[zb])
        dwt = self.tile([128, 8, 31], F32, "dwt")
        self.load("sp", dwt, Ld["dwT"].ap())
        cv = self.tile([128, 3, 8], F32, "cv")
        self.load("sp", cv, Ld["cvec"].ap())
        Dm = self.tile([128, 248, 128], BF16)
        for c in range(8):
            self.P.op("dve", lambda e, c=c: e.tensor_tensor(
                Dm[:, c * 31:(c + 1) * 31, :], self.identb.unsqueeze(1).to_broadcast([128, 31, 128]),
                dwt[:, c, :].unsqueeze(2).to_broadcast([128, 31, 128]), ALU.mult),
                reads=[self.cb.b, dwt.b], writes=[Dm.b])
        Yr = self.ring("Y", [128, 8, 512], F32, 2)
        ybr = self.ring("yb", [128, 512], BF16, 2)
        yqr = self.ring("yq", [128, 512], BF16, 2)
        st = [self.tile([128, 512], F32) for _ in range(5)]
        zr = self.ring("z", [128, 512], F32, 2)
        z2r = self.ring("z2", [128, 512], F32, 2)
        cor = self.ring("co", [128, 512], BF16, 2)
        Cr = Ring(self.pst[0:4])
        TG = [(tg * 512, 512, tg * 512) for tg in range(4)] + [(2048, 256, CS0)]
        for (t0, n, off) in TG:
            Y = Yr.next()
            s1, s2 = self.pst[4], self.pst[5]
            pend = None
            for c in range(8):
                self.bg_step()
                ps = Cr.next()
                for k in range(31):
                    self.mm(ps, ps[:, 0:n], Dm[:, c * 31 + k, :], ugt[:, c, off + k:off + k + n], k == 0, k == 30, [Dm.b, ugt.b])
                if pend is not None:
                    pc, pyb, pyq = pend
                    self.mm(s1, s1[:, 0:n], self.o1024, pyb[:, 0:n], pc == 0, False, [pyb.b, self.cb.b])
                    self.mm(s2, s2[:, 0:n], self.o1024, pyq[:, 0:n], pc == 0, False, [pyq.b, self.cb.b])
                self.act(Y[:, c, 0:n], ps[:, 0:n], AF.Identity, [ps.b, cv.b], [Y.b], bias=cv[:, 0, c:c + 1], scale=1.0)
                yb, yq = ybr.next(), yqr.next()
                self.copy("pool", yb[:, 0:n], Y[:, c, 0:n], [Y.b], [yb.b])
                self.tt("dve", yq[:, 0:n], Y[:, c, 0:n], Y[:, c, 0:n], ALU.mult, [Y.b], [yq.b])
                pend = (c, yb, yq)
            pc, pyb, pyq = pend
            self.mm(s1, s1[:, 0:n], self.o1024, pyb[:, 0:n], False, True, [pyb.b, self.cb.b])
            self.mm(s2, s2[:, 0:n], self.o1024, pyq[:, 0:n], False, True, [pyq.b, self.cb.b])
            mu, msq, var, rstd, nmr = st
            self.copy("act", mu[:, 0:n], s1[:, 0:n], [s1.b], [mu.b])
            self.tt("dve", msq[:, 0:n], mu[:, 0:n], mu[:, 0:n], ALU.mult, [mu.b], [msq.b])
            self.tt("dve", var[:, 0:n], s2[:, 0:n], msq[:, 0:n], ALU.subtract, [s2.b, msq.b], [var.b])
            self.act(msq[:, 0:n], var[:, 0:n], AF.Sqrt, [var.b], [msq.b], bias=EPS_AP(self), scale=1.0)
            self.recip(rstd[:, 0:n], msq[:, 0:n], [msq.b], [rstd.b])
            self.stt(nmr[:, 0:n], mu[:, 0:n], -1.0, rstd[:, 0:n], ALU.mult, ALU.mult, [mu.b, rstd.b], [nmr.b])
            for c in range(8):
                z, z2, co = zr.next(), z2r.next(), cor.next()
                self.tt("dve", z[:, 0:n], Y[:, c, 0:n], rstd[:, 0:n], ALU.mult, [Y.b, rstd.b], [z.b])
                self.tt("pool", z2[:, 0:n], z[:, 0:n], nmr[:, 0:n], ALU.add, [z.b, nmr.b], [z2.b])
                self.act(co[:, 0:n], z2[:, 0:n], AF.Silu, [z2.b, cv.b], [co.b], scale=cv[:, 1, c:c + 1], bias=cv[:, 2, c:c + 1])
                self.store("sp", co, self.AT[1024 + c * 128:1024 + (c + 1) * 128, t0:t0 + n], ap=co[:, 0:n])
        self.release(m)

    def outproj(self, l, ngroups):
        Ld = self.L[l]
        m = self.mark()
        W = self.tile([128, 16, D], BF16, "W")
        for p in range(4):
            self.load("pool", W, Ld["w_out"][:, p * 512:(p + 1) * 512].rearrange("(c p) n -> p c n", p=128), ap=W[:, :, p * 512:(p + 1) * 512])
        gas = []
        for r in range(2 if ngroups > 4 else 1):
            ga = self.tile([128, D], F32, f"ga{r}")
            self.load("sp", ga, self.ADA[l][r:r + 1, 4096:6144].partition_broadcast(128))
            gas.append(ga)
        atr = self.ring("at", [128, 16, 512], BF16, 2)
        xr = self.ring("x", [128, D], F32, 3)
        tr = self.ring("tm", [128, 512], F32, 3)
        for tg in range(ngroups):
            t0, n = tg * 512, (512 if tg < 4 else 256)
            ga = gas[0 if tg < 4 else 1]
            at = atr.next()
            self.load("sp", at, self.AT[:, t0:t0 + n].rearrange("(c p) t -> p c t", p=128), ap=at[:, :, 0:n])
            for bi in range(n // 128):
                tb = tg * 4 + bi
                self.bg_step()
                x = xr.next()
                self.load("act", x, self.XR[tb * 128:(tb + 1) * 128, :])
                for dc in range(4):
                    ps = self.pr.next()
                    for fc in range(16):
                        self.mm(ps, ps.ap, at[:, fc, bi * 128:(bi + 1) * 128], W[:, fc, dc * 512:(dc + 1) * 512], fc == 0, fc == 15, [at.b, W.b])
                    tm = tr.next()
                    self.tt("dve", tm.ap, ps.ap, ga[:, dc * 512:(dc + 1) * 512], ALU.mult, [ps.b, ga.b], [tm.b])
                    self.tt("pool", x[:, dc * 512:(dc + 1) * 512], x[:, dc * 512:(dc + 1) * 512], tm.ap, ALU.add, [x.b, tm.b], [x.b])
                self.store("sp", x, self.XR[tb * 128:(tb + 1) * 128, :])
        self.release(m)

    def moe(self, l):
        Ld = self.L[l]
        nblk = NB if l == 0 else 16
        nj = 288 if l == 0 else 256
        JB = [(0, 128), (128, 128), (256, 32)][:3 if l == 0 else 2]
        m0 = self.mark()
        HT, HTb = self.alloc_HT()
        rw = self.tile([128, 16, 16], BF16, "rw")
        self.load("pool", rw, Ld["router"].ap().rearrange("(c p) e -> p c e", p=128))
        smr = self.ring("sm2", [128, 1], F32, 12)
        exr = self.ring("ex", [128, 16], F32, 3)
        afr = self.ring("af", [128, 16], F32, 3)
        str_ = {}

        rpr = Ring(self.pst[4:8])

        def rA(tb):
            ps = rpr.next()
            for kc in range(16):
                self.mm(ps, ps[:, 0:16], HT[:, kc, tb * 128:(tb + 1) * 128], rw[:, kc, :], kc == 0, kc == 15, [rw.b, HTb[tb]])
            mx, nmx, ssum, rr = smr.next(), smr.next(), smr.next(), smr.next()
            self.P.op("dve", lambda e, mx=mx, ps=ps: e.reduce_max(mx.ap, ps[:, 0:16], AX.X), reads=[ps.b], writes=[mx.b])
            self.P.op("dve", lambda e, mx=mx, nmx=nmx: e.tensor_scalar(nmx.ap, mx.ap, -1.0, None, ALU.mult), reads=[mx.b], writes=[nmx.b])
            ex = exr.next()
            self.act(ex.ap, ps[:, 0:16], AF.Exp, [ps.b, nmx.b], [ex.b, ssum.b], bias=nmx.ap, scale=1.0, accum_out=ssum.ap)
            self.recip(rr.ap, ssum.ap, [ssum.b], [rr.b])
            af = afr.next()
            self.P.op("dve", lambda e, af=af, ex=ex, rr=rr: e.tensor_scalar(af.ap, ex.ap, rr.ap, None, ALU.mult), reads=[ex.b, rr.b], writes=[af.b])
            str_[tb] = af

        def rB(tb):
            af = str_.pop(tb)
            ps2 = rpr.next()
            self.P.op("pe", lambda e, ps2=ps2, af=af: e.transpose(ps2[0:16, 0:128], af.ap, self.idf.ap), reads=[af.b, self.idf.b], writes=[ps2.b])
            self.copy("act", self.affT[:, tb * 128:(tb + 1) * 128], ps2[0:16, 0:128], [ps2.b], [self.affT.b])
        self.norm(l, 2, nblk, HT, HTb, True, post=[rA, rB])
        self.cv_issue(len(self._cvq))
        self.release(m0)
        m = self.mark()
        gas = [self.tile([128, D], BF16) for r in range(2 if l == 0 else 1)]
        slots = self.ring("ws", [128, 16 * 1024], BF16, 4)
        xsr = self.ring("xs", [128, D], BF16, len(JB))
        xtr = self.ring("xT", [128, 16, 288], BF16, 1)
        hdr = self.ring("hd", [128, 8, 288], BF16, 1)
        sgr = self.ring("sg", [128, 288], BF16, 2)
        yr = self.ring("y", [128, D], F32, 3)
        xrb = self.P.buf("xr_scatter")
        pending = []
        for r, ga in enumerate(gas):
            stg = yr.t[1 + r]
            self.load("sp", stg, self.ADA[l][r:r + 1, 10240:12288].partition_broadcast(128))
            self.copy("dve", ga.ap, stg.ap, [stg.b], [ga.b])

        def gather(e):
            out = []
            for jb, (j0, n) in enumerate(JB):
                xs = xsr.next()
                self.P.dma("pool", xs.tag, lambda en, xs=xs, n=n, jb=jb, e=e: en.indirect_dma_start(
                    out=xs[0:n, :], out_offset=None, in_=self.H2.ap(),
                    in_offset=bass.IndirectOffsetOnAxis(ap=self.idxT[0:n, jb, e:e + 1], axis=0)),
                    reads=[self.idxT.b], writes=[xs.b])
                out.append(xs)
            return out

        wpieces = {}
        wstate = [0]

        def ensure(k):
            while wstate[0] <= k and wstate[0] < 48:
                kk = wstate[0]
                e, which = divmod(kk, 3)
                sl = slots.next()
                key = ("wg", "wu", "wd")[which]
                pre = key in self.WB[l]
                src = (self.WB[l][key] if pre else Ld[key])[e]
                q = "sp" if pre else "pool"
                if which < 2:
                    self.load(q, sl, src.rearrange("(c p) f -> p c f", p=128), ap=sl.ap.rearrange("p (c f) -> p c f", c=16))
                else:
                    self.load(q, sl, src.rearrange("(c p) d -> p c d", p=128), ap=sl.ap.rearrange("p (c d) -> p c d", c=8))
                wpieces[kk] = sl
                wstate[0] += 1

        ensure(1)
        m2 = self.mark()
        work = Tl(yr.t[0].ap[0:16, :], yr.t[0].b, None)
        work2 = self.tile([16, 288], F32)
        vals = self.tile([16, 288], F32)
        idx = self.tile([16, 288], U32)
        idxf = work2

        def topk(src0, wk, rounds, o0):
            for r in range(rounds):
                src = src0 if r == 0 else wk.ap
                vs = vals[:, o0 + r * 8:o0 + (r + 1) * 8]
                ix = idx[:, o0 + r * 8:o0 + (r + 1) * 8]
                rd = [self.affT.b, wk.b]
                self.P.op("dve", lambda e, vs=vs, src=src: e.max(out=vs, in_=src), reads=rd, writes=[vals.b])
                self.P.op("dve", lambda e, vs=vs, ix=ix, src=src: e.max_index(out=ix, in_max=vs, in_values=src), reads=rd + [vals.b], writes=[idx.b])
                if r < rounds - 1:
                    self.P.op("dve", lambda e, vs=vs, src=src, wk=wk: e.match_replace(out=wk.ap, in_to_replace=vs, in_values=src, imm_value=-1.0),
                              reads=rd + [vals.b], writes=[wk.b])
        topk(self.affT[:, 0:S], work, 32, 0)
        if l == 0:
            topk(self.affT[:, S:T], Tl(work2.ap[:, 0:C], work2.b, None), 4, 256)
        self.copy("dve", idxf[:, 0:nj], idx[:, 0:nj], [idx.b], [idxf.b])
        if l == 0:
            self.P.op("dve", lambda e: e.tensor_scalar(idxf[:, 256:288], idxf[:, 256:288], float(S), None, ALU.add), reads=[idxf.b], writes=[idxf.b])
        for jb, (j0, n) in enumerate(JB):
            ps = self.pr.next()
            self.P.op("pe", lambda e, ps=ps, j0=j0, n=n: e.transpose(ps[0:n, 0:16], idxf[:, j0:j0 + n], self.idf[0:16, 0:16]), reads=[idxf.b, self.idf.b], writes=[ps.b])
            self.copy("dve", self.idxT[0:n, jb, :], ps[0:n, 0:16], [ps.b], [self.idxT.b])
            ps = self.pr.next()
            self.P.op("pe", lambda e, ps=ps, j0=j0, n=n: e.transpose(ps[0:n, 0:16], vals[:, j0:j0 + n], self.idf[0:16, 0:16]), reads=[vals.b, self.idf.b], writes=[ps.b])
            self.copy("act", self.gateT[0:n, jb, :], ps[0:n, 0:16], [ps.b], [self.gateT.b])
        self.release(m2)
        xs_next = gather(0)
        for e in range(16):
            xs_l = xs_next
            xt = xtr.next()
            for jb, (j0, n) in enumerate(JB):
                xs = xs_l[jb]
                for half in range(2):
                    ps = self.pr.next()
                    pv = ps.ap.bitcast(BF16)
                    for j in range(8):
                        fc = half * 8 + j
                        self.P.op("pe", lambda en, pv=pv, j=j, fc=fc, xs=xs, n=n: en.transpose(pv[:, j * 128:j * 128 + n], xs[0:n, fc * 128:(fc + 1) * 128], self.identb[0:n, 0:n]),
                                  reads=[xs.b, self.cb.b], writes=[ps.b])
                    self.copy("act" if half == 0 else "dve", xt[:, half * 8:(half + 1) * 8, j0:j0 + n],
                              pv.rearrange("p (c t) -> p c t", c=8)[:, :, 0:n], [ps.b], [xt.b])
            for fn in pending:
                fn()
            pending = []
            ensure(3 * e + 3)
            wg, wu, wd = wpieces[3 * e], wpieces[3 * e + 1], wpieces[3 * e + 2]
            wg3 = wg.ap.rearrange("p (c f) -> p c f", c=16)
            wu3 = wu.ap.rearrange("p (c f) -> p c f", c=16)
            wd3 = wd.ap.rearrange("p (c d) -> p c d", c=8)
            hd = hdr.next()
            for Fc in range(8):
                psg, psu = self.pr.next(), self.pr.next()
                for kc in range(16):
                    self.mm(psg, psg[:, 0:nj], wg3[:, kc, Fc * 128:(Fc + 1) * 128], xt[:, kc, 0:nj], kc == 0, kc == 15, [wg.b, xt.b])
                for kc in range(16):
                    self.mm(psu, psu[:, 0:nj], wu3[:, kc, Fc * 128:(Fc + 1) * 128], xt[:, kc, 0:nj], kc == 0, kc == 15, [wu.b, xt.b])
                sg = sgr.next()
                self.act(sg[:, 0:nj], psg[:, 0:nj], AF.Silu, [psg.b], [sg.b])
                self.tt("dve", hd[:, Fc, 0:nj], psu[:, 0:nj], sg[:, 0:nj], ALU.mult, [psu.b, sg.b], [hd.b])
            if e + 1 < 16:
                xs_next = gather(e + 1)
                ensure(3 * e + 5)
            for jb, (j0, n) in enumerate(JB):
                y = yr.next()
                ga = gas[0 if jb < 2 else 1]
                for dc in range(4):
                    ps = self.pr.next()
                    for Fc in range(8):
                        self.mm(ps, ps[0:n, :], hd[:, Fc, j0:j0 + n], wd3[:, Fc, dc * 512:(dc + 1) * 512], Fc == 0, Fc == 7, [hd.b, wd.b])
                    self.stt(y[0:n, dc * 512:(dc + 1) * 512], ps[0:n, :], self.gateT[0:n, jb, e:e + 1], ga[0:n, dc * 512:(dc + 1) * 512],
                             ALU.mult, ALU.mult, [ps.b, self.gateT.b, ga.b], [y.b])
                pending.append(lambda y=y, n=n, jb=jb, e=e: self.P.dma("pool", y.tag, lambda en: en.indirect_dma_start(
                    out=self.XR.ap(), out_offset=bass.IndirectOffsetOnAxis(ap=self.idxT[0:n, jb, e:e + 1], axis=0),
                    in_=y[0:n, :], in_offset=None, compute_op=ALU.add),
                    reads=[y.b, self.idxT.b, xrb], writes=[xrb]))
        for fn in pending:
            fn()
        self.release(m)

    def layer1_mixer(self):
        Ld = self.L[1]
        self.cv_start(1)
        m0 = self.mark()
        HT, HTb = self.alloc_HT()
        self.norm(1, 1, NB, HT, HTb, False)
        m = self.mark()
        TG = [(tg * 512, 512) for tg in range(4)] + [(2048, 256)]

        def hb_of(t0, n):
            return HTb[t0 // 128:(t0 + n) // 128]
        gq = self.tile([128, 12], F32, "gq")
        self.load("sp", gq, Ld["gq"].ap())
        gkv = self.tile([128, 4], F32, "gkv")
        self.load("sp", gkv, Ld["gkv"].ap())
        cs = self.tile([64, 2, S], BF16, "cs")
        self.load("pool", cs, self.cs1_d.ap().rearrange("c p t -> p c t"))
        wq = [self.tile([128, 16, 512], BF16, f"wq{i}") for i in range(4)]
        for i in range(4):
            self.load("pool", wq[i], Ld["w_dqkv"][:, i * 512:(i + 1) * 512].rearrange("(c p) n -> p c n", p=128))
        wk = self.tile([128, 16, 64], BF16, "wk")
        self.load("pool", wk, Ld["w_dqkv"][:, 2048:2112].rearrange("(c p) n -> p c n", p=128))
        raw = self.tile([128, 12, 512], F32)
        rawb = [self.P.buf(f"raw{i}") for i in range(12)]
        sqr = self.ring("sq", [128, 512], BF16, 3)
        sd = self.tile([128, 512], F32)
        rstd = self.tile([128, 512], F32)
        cqr = self.ring("cq", [128, 512], BF16, 3)
        rr = self.rope_rings()

        def lora(chunks, groups, gcol, nfeat, dst):
            nch = len(chunks)
            for (t0, n) in groups:
                pss = self.pst[7]
                stl = {}

                def lA(ci, t0=t0, n=n):
                    w, j = chunks[ci]
                    ps = self.proj_fm(w, j * 128, 128, HT, hb_of(t0, n), 16, t0, n)
                    self.copy("act", raw[:, ci, 0:n], ps[:, 0:n], [ps.b], [rawb[ci]])
                    sq = sqr.next()
                    self.tt("pool", sq[:, 0:n], raw[:, ci, 0:n], raw[:, ci, 0:n], ALU.mult, [rawb[ci]], [sq.b])
                    stl[ci] = sq

                def lB(ci, n=n):
                    self.bg_step()
                    sq = stl.pop(ci)
                    self.mm(pss, pss[:, 0:n], self.ones1, sq[:, 0:n], ci == 0, ci == nch - 1, [sq.b, self.cb.b])
                self.pipeline(nch, [lA, lB])
                self.act(sd[:, 0:n], pss[:, 0:n], AF.Sqrt, [pss.b], [sd.b], bias=EPS_AP(self), scale=1.0 / nfeat)
                self.recip(rstd[:, 0:n], sd[:, 0:n], [sd.b], [rstd.b])
                for ci in range(nch):
                    cq = cqr.next()
                    self.stt(cq[:, 0:n], raw[:, ci, 0:n], gcol[:, ci:ci + 1], rstd[:, 0:n], ALU.mult, ALU.mult, [rawb[ci], rstd.b, gcol.b], [cq.b])
                    self.store("sp", cq, dst[ci * 128:(ci + 1) * 128, t0:t0 + n], ap=cq[:, 0:n])
        lora([(wq[i // 4], i % 4) for i in range(12)], TG[:4], gq, 1536.0, self.CQT)
        lora([(wq[3], i) for i in range(4)], TG, gkv, 512.0, self.CKVT)
        stk = {}

        def kA(i):
            t0, n = TG[i]
            ps = self.proj_fm(wk, 0, 64, HT, hb_of(t0, n), 16, t0, n)
            qn = rr["qn"].next()
            self.copy("act", qn[0:64, 0:n], ps[0:64, 0:n], [ps.b], [qn.b])
            stk[i] = qn

        def kB(i):
            t0, n = TG[i]
            self.rope_tail(stk.pop(i), 64, n, t0, cs, self.rot64, self.KPET[:, t0:t0 + n], rr, is_ctx=(t0 >= S))
        self.pipeline(len(TG), [kA, kB])
        self.release(m)
        self.release(m0)
        m = self.mark()
        cs = self.tile([64, 2, S], BF16, "cs")
        self.load("pool", cs, self.cs1_d.ap().rearrange("c p t -> p c t"))
        cqt = self.tile([128, 12, S], BF16, "cqt")
        for c in range(12):
            self.load("sp" if c % 2 == 0 else "act", cqt, self.CQT[c * 128:(c + 1) * 128, :], ap=cqt[:, c, :])
        wr = self.ring("w", [128, 12, 192], BF16, 3)
        rr = self.rope_rings()
        qnr = self.ring("qnn", [128, 512], BF16, 3)
        items = [(h, t0, n) for h in range(16) for (t0, n) in TG[:4]]
        wcur = {}

        def getw(h):
            if h not in wcur:
                w = wr.next()
                self.load("pool", w, Ld["w_uq"][:, h * 192:(h + 1) * 192].rearrange("(c p) n -> p c n", p=128))
                wcur[h] = w
            return wcur[h]
        stu = {}

        def uA(i):
            h, t0, n = items[i]
            w = getw(h)
            if t0 == 0 and h + 1 < 16:
                getw(h + 1)
            ps = self.proj_fm(w, 0, 128, cqt.ap, [cqt.b], 12, t0, n)
            qn = qnr.next()
            self.copy("act", qn[:, 0:n], ps[:, 0:n], [ps.b], [qn.b])
            self.store("sp", qn, self.QNT[h * 128:(h + 1) * 128, t0:t0 + n], ap=qn[:, 0:n])
            ps = self.proj_fm(w, 128, 64, cqt.ap, [cqt.b], 12, t0, n)
            qp = rr["qn"].next()
            self.copy("dve", qp[0:64, 0:n], ps[0:64, 0:n], [ps.b], [qp.b])
            stu[i] = qp

        def uB(i):
            self.bg_step()
            h, t0, n = items[i]
            self.rope_tail(stu.pop(i), 64, n, t0, cs, self.rot64, self.QPT[h * 64:(h + 1) * 64, t0:t0 + n], rr)
        self.pipeline(len(items), [uA, uB])
        self.release(m)
        m = self.mark()
        ckt = self.tile([128, 4, T], BF16, "ckt")
        self.load("sp", ckt, self.CKVT.ap().rearrange("(c p) t -> p c t", p=128))
        W = self.tile([128, 4, 4096], BF16, "W")
        self.load("pool", W, Ld["w_ukv"].ap().rearrange("(c p) n -> p c n", p=128))
        knr = self.ring("kn", [128, 512], BF16, 3)
        for h in range(16):
            self.bg_step()
            for (t0, n) in TG:
                ps = self.proj_fm(W, h * 256, 128, ckt.ap, [ckt.b], 4, t0, n)
                kn = knr.next()
                self.copy("act" if h % 2 == 0 else "dve", kn[:, 0:n], ps[:, 0:n], [ps.b], [kn.b])
                self.store("sp", kn, self.KNT[h * 128:(h + 1) * 128, t0:t0 + n], ap=kn[:, 0:n])
        vr = self.ring("v", [128, 512], BF16, 3)
        for tb in range(NB):
            for hg in range(4):
                ps = self.pr.next()
                for hh in range(4):
                    h = hg * 4 + hh
                    for c in range(4):
                        self.mm(ps, ps[:, hh * 128:(hh + 1) * 128], ckt[:, c, tb * 128:(tb + 1) * 128], W[:, c, h * 256 + 128:h * 256 + 256], c == 0, c == 3, [ckt.b, W.b])
                v = vr.next()
                self.copy("act" if hg % 2 == 0 else "dve", v.ap, ps.ap, [ps.b], [v.b])
                self.store("sp", v, self.V1[tb * 128:(tb + 1) * 128, hg * 512:(hg + 1) * 512])
        self.release(m)
        kn_aps = [self.KNT[h * 128:(h + 1) * 128, :] for h in range(16)]
        kp_ap = self.KPET.ap()
        self.attention(192 ** -0.5, 16, lambda h: (h, kn_aps[h], kp_ap), lambda h: self.V1[:, h * 128:(h + 1) * 128],
                       lambda h: (self.QNT[h * 128:(h + 1) * 128, :], self.QPT[h * 64:(h + 1) * 64, :]), False, S)
        self.outproj(1, 4)

    def final(self):
        m = self.mark()
        gt = self.tile([128, D], F32, "g")
        self.load("sp", gt, self.fg_d.ap().partition_broadcast(128))
        xr = self.ring("x", [128, D], F32, 3)
        orr = self.ring("fin", [128, D], F32, 2)
        junk = self.ring("jk", [128, D], BF16, 2)
        smr = self.ring("sm", [128, 1], F32, 12)
        st = {}

        def s0(tb):
            x = xr.next()
            self.load("sp", x, self.XR[tb * 128:(tb + 1) * 128, :])
            st[tb] = dict(x=x)

        def s1(tb):
            x = st[tb]["x"]
            ss, sq, jk = smr.next(), smr.next(), junk.next()
            self.act(jk.ap, x.ap, AF.Square, [x.b], [jk.b, ss.b], scale=float(D ** -0.5), accum_out=ss.ap)
            self.act(sq.ap, ss.ap, AF.Sqrt, [ss.b], [sq.b], bias=EPS_AP(self), scale=1.0)
            st[tb]["sq"] = sq

        def s2(tb):
            d = st.pop(tb)
            rs, o = smr.next(), orr.next()
            self.recip(rs.ap, d["sq"].ap, [d["sq"].b], [rs.b])
            self.stt(o.ap, d["x"].ap, rs.ap, gt.ap, ALU.mult, ALU.mult, [d["x"].b, rs.b, gt.b], [o.b])
            self.store("act", o, self.out_d[tb * 128:(tb + 1) * 128, :])
        self.pipeline(16, [s0, s1, s2])
        self.release(m)


def EPS_AP(k):
    return k._eps.ap


def _rope_tables(d_rot):
    rows = S // 64
    row = np.repeat(np.arange(rows, dtype=np.float32), 64)
    col = np.tile(np.arange(64, dtype=np.float32), rows)
    n_axis = d_rot // 4
    inv = (np.float32(10000.0) ** (-np.arange(n_axis, dtype=np.float32) / np.float32(n_axis))).astype(np.float32)
    ang = np.concatenate([row[:, None] * inv, col[:, None] * inv], axis=-1).astype(np.float32)
    half = d_rot // 2
    idx = np.arange(d_rot) % half
    cs = np.stack([np.cos(ang)[:, idx].T, np.sin(ang)[:, idx].T]).astype(np.float32)
    return np.ascontiguousarray(cs)


def _rot_lhsT(d_rot, pad=128):
    half = d_rot // 2
    m = np.zeros((pad, pad), np.float32)
    for j in range(half):
        m[j + half, j] = -1.0
        m[j, j + half] = 1.0
    return m


def _colmajor(v, nchunk):
    return np.ascontiguousarray(np.asarray(v, np.float32).reshape(nchunk, 128).T)


def _shared(inputs):
    f = lambda k: np.ascontiguousarray(np.asarray(inputs[k], np.float32))
    sh = {}
    consts = np.zeros((7, 128, 128), np.float32)
    consts[0] = np.eye(128, dtype=np.float32)
    consts[1] = 1.0 / 128
    consts[2] = 1.0 / 1024
    consts[3] = 1.0
    consts[4] = _rot_lhsT(128)
    consts[5] = _rot_lhsT(64)
    sh["consts"] = consts
    sh["cs0"] = _rope_tables(128)
    sh["cs1"] = _rope_tables(64)
    for l in range(2):
        sh[f"l{l}_mod_w"] = f(f"l{l}_mod_w")
        sh[f"l{l}_mod_b"] = f(f"l{l}_mod_b").reshape(1, -1)
        sh[f"l{l}_norm1_g"] = f(f"l{l}_norm1_g").reshape(1, -1)
        sh[f"l{l}_norm2_g"] = f(f"l{l}_norm2_g").reshape(1, -1)
        sh[f"l{l}_router"] = f(f"l{l}_router")
        sh[f"l{l}_w_gate"] = f(f"l{l}_w_gate")
        sh[f"l{l}_w_up"] = f(f"l{l}_w_up")
        sh[f"l{l}_w_down"] = f(f"l{l}_w_down")
        sh[f"l{l}_w_out"] = f(f"l{l}_w_out")
    sh["l0_w_in"] = f("l0_w_in")
    sh["l0_q_norm_g"] = f("l0_q_norm_g").reshape(128, 1)
    sh["l0_k_norm_g"] = f("l0_k_norm_g").reshape(128, 1)
    dw = f("l0_dw_w").reshape(31, 8, 128)
    sh["l0_dwT"] = np.ascontiguousarray(dw.transpose(2, 1, 0))
    sh["l0_cvec"] = np.ascontiguousarray(np.stack([_colmajor(inputs["l0_dw_b"], 8), _colmajor(inputs["l0_conv_ln_g"], 8),
                                                   _colmajor(inputs["l0_conv_ln_b"], 8)], axis=1))
    sh["l1_w_dqkv"] = f("l1_w_dqkv")
    sh["l1_gq"] = _colmajor(inputs["l1_q_lora_norm_g"], 12)
    sh["l1_w_uq"] = f("l1_w_uq")
    sh["l1_gkv"] = _colmajor(inputs["l1_kv_lora_norm_g"], 4)
    sh["l1_w_ukv"] = f("l1_w_ukv")
    sh["final_norm_g"] = f("final_norm_g").reshape(1, -1)
    return sh


def _core_map(inputs, sh, b):
    m = dict(sh)
    m["x"] = np.ascontiguousarray(np.asarray(inputs["x"][b], np.float32))
    m["ctx"] = np.ascontiguousarray(np.asarray(inputs["ctx"][b], np.float32))
    cc = np.stack([np.asarray(inputs["c"][b], np.float32), np.asarray(inputs["c_ctx"], np.float32)], axis=-1)
    m["cT"] = np.ascontiguousarray(cc.reshape(16, 128, 2).transpose(1, 0, 2))
    return m


def kernel(**inputs):
    nc = K().build()
    sh = _shared(inputs)
    nb = np.asarray(inputs["x"]).shape[0]
    in_maps = [_core_map(inputs, sh, b) for b in range(nb)]
    res = run_bass_kernel_spmd(nc, in_maps, core_ids=list(range(nb)))
    return np.stack([np.asarray(r["out"], np.float32) for r in res.results], axis=0)
```

```python
import numpy as np
from concourse.bass_utils import run_bass_kernel_spmd

import contextlib
import concourse.bass as bass
import concourse.mybir as mybir

F32 = mybir.dt.float32
BF16 = mybir.dt.bfloat16
I32 = mybir.dt.int32
U32 = mybir.dt.uint32
AF = mybir.ActivationFunctionType
ALU = mybir.AluOpType
AX = mybir.AxisListType

ENGS = ("pe", "dve", "act", "pool", "sp")
COMPUTE = ("pe", "dve", "act", "pool")


class Buf:
    __slots__ = ("name", "last_w", "readers")

    def __init__(self, name):
        self.name = name
        self.last_w = None
        self.readers = []


class Op:
    __slots__ = ("eng", "fn", "idx", "waits", "signal", "seq", "tag", "tagcnt")

    def __init__(self, eng, fn):
        self.eng = eng
        self.fn = fn
        self.waits = {}
        self.signal = False
        self.seq = None
        self.tag = None
        self.tagcnt = None


class Prog:
    def __init__(self, nc):
        self.nc = nc
        self.ops = {e: [] for e in ENGS}
        self.tags = {}
        self.known = {e: {} for e in ENGS}
        self.barrier_pending = {e: None for e in ENGS}
        self.nbuf = 0

    def buf(self, name=None):
        self.nbuf += 1
        return Buf(name or f"b{self.nbuf}")

    def bufs(self, n, name=None):
        return [self.buf(f"{name}{i}") for i in range(n)]

    def _need(self, op, dep, waw=False):
        if dep is None or dep is op:
            return
        if dep.tag is not None:
            tg = self.tags[dep.tag]
            val = tg[0] * 16
            if op.tag == dep.tag:
                if waw:
                    return
                val = (tg[0] - 1) * 16
            tg[1] = max(tg[1], val)
            key = ("t", dep.tag)
        else:
            if dep.eng == "pe" and op.eng == "pe" and op.tag is None:
                return
            dep.signal = True
            key = ("e", dep.eng)
            val = dep
        cur = op.waits.get(key)
        if cur is None:
            op.waits[key] = val
        elif key[0] == "t":
            op.waits[key] = max(cur, val)
        else:
            op.waits[key] = cur if cur.idx >= val.idx else val

    def _record(self, op, reads, writes):
        lst = self.ops[op.eng]
        op.idx = len(lst)
        lst.append(op)
        bp = self.barrier_pending[op.eng]
        if bp is not None:
            for d in bp[0]:
                self._need(op, d)
            for tag, val in bp[1].items():
                key = ("t", tag)
                op.waits[key] = max(op.waits.get(key, 0), val)
                self.tags[tag][1] = max(self.tags[tag][1], val)
            self.barrier_pending[op.eng] = None
        for b in reads:
            self._need(op, b.last_w)
        for b in writes:
            self._need(op, b.last_w, waw=True)
            for r in b.readers:
                self._need(op, r)
        for b in reads:
            b.readers.append(op)
        for b in writes:
            b.last_w = op
            b.readers = []

    def op(self, eng, fn, reads=(), writes=()):
        o = Op(eng, fn)
        self._record(o, reads, writes)
        return o

    def dma(self, q, tag, fn, reads=(), writes=(), after=()):
        o = Op(q, fn)
        tg = self.tags.setdefault(tag, [0, 0])
        tg[0] += 1
        o.tag = tag
        o.tagcnt = tg[0]
        for (t2, v2) in after:
            tg2 = self.tags.setdefault(t2, [0, 0])
            tg2[1] = max(tg2[1], v2)
            o.waits[("t", t2)] = max(o.waits.get(("t", t2), 0), v2)
        if tg[1] > 0:
            o.waits[("t", tag)] = max(o.waits.get(("t", tag), 0), tg[1])
        self._record(o, reads, writes)
        return o

    def barrier(self):
        lasts = []
        for e in COMPUTE:
            for o in reversed(self.ops[e]):
                if o.tag is None:
                    lasts.append(o)
                    break
        tagvals = {t: v[0] * 16 for t, v in self.tags.items() if v[0] > 0}
        for e in ENGS:
            self.barrier_pending[e] = (lasts, dict(tagvals))

    def emit(self, final_tags=()):
        nc = self.nc
        for e in COMPUTE:
            n = 0
            for o in self.ops[e]:
                if o.signal:
                    n += 1
                    o.seq = n
        with contextlib.ExitStack() as st:
            esem = {e: st.enter_context(nc.semaphore(f"s_{e}")) for e in COMPUTE}
            tsem = {t: st.enter_context(nc.semaphore(f"t_{i}")) for i, t in enumerate(self.tags)}
            block = st.enter_context(nc.Block())

            def run(ename, eng):
                known = {}
                for o in self.ops[ename]:
                    for key, val in o.waits.items():
                        if key[0] == "t":
                            sem, v = tsem[key[1]], val
                        else:
                            sem, v = esem[key[1]], val.seq
                        if known.get(key, 0) >= v:
                            continue
                        known[key] = v
                        eng.wait_ge(sem, v)
                    ins = o.fn(eng)
                    if o.tag is not None:
                        ins.then_inc(tsem[o.tag], 16)
                    elif o.signal:
                        ins.then_inc(esem[ename], 1)
                if ename == "sp":
                    for t in final_tags:
                        eng.wait_ge(tsem[t], self.tags[t][0] * 16)

            @block.sync
            def _(eng):
                run("sp", eng)

            @block.scalar
            def _(eng):
                run("act", eng)

            @block.vector
            def _(eng):
                run("dve", eng)

            @block.gpsimd
            def _(eng):
                run("pool", eng)

            @block.tensor
            def _(eng):
                run("pe", eng)

    def stats(self):
        return {e: len(self.ops[e]) for e in ENGS}, len(self.tags)

D = 2048
S = 2048
C = 256
T = S + C
NB = T // 128
EPS = 1e-6
SBUF_BYTES = 212480
_DS = {F32: 4, BF16: 2, I32: 4, U32: 4}


class Tl:
    __slots__ = ("ap", "b", "tag")

    def __init__(self, ap, b, tag):
        self.ap, self.b, self.tag = ap, b, tag

    def __getitem__(self, k):
        return self.ap[k]


class Ring:
    def __init__(self, tiles):
        self.t = tiles
        self.i = 0

    def next(self):
        t = self.t[self.i % len(self.t)]
        self.i += 1
        return t


class K:
    def __init__(self, stop=None, dumps=(), noinput=()):
        self.noinput = noinput
        nc = bass.Bass("TRN2", target_bir_lowering=False)
        self.nc = nc
        self.P = Prog(nc)
        self.stop = stop
        self.big = nc.alloc_sbuf_tensor("big", [128, SBUF_BYTES], mybir.dt.uint8)
        self.top = 0
        self.ntile = 0
        self.psb = [nc.alloc_psum_tensor(f"ps{i}", [128, 512], F32) for i in range(8)]
        self.pst = [Tl(self.psb[i][:, :], self.P.buf(f"ps{i}"), None) for i in range(8)]
        self.pr = Ring(self.pst[0:6])
        self.din = {}
        self.dumps = dumps

    def inp(self, name, shape, dtype=F32):
        t = self.nc.dram_tensor(name, list(shape), dtype, kind="Internal" if name in self.noinput else "ExternalInput")
        self.din[name] = t
        return t

    def scratch(self, name, shape, dtype):
        kind = "ExternalOutput" if name in self.dumps else "Internal"
        return self.nc.dram_tensor(name, list(shape), dtype, kind=kind)

    def tile(self, shape, dtype, tag=None):
        n = 1
        for s in shape[1:]:
            n *= s
        nbytes = (n * _DS[dtype] + 63) // 64 * 64
        off = self.top
        self.top += nbytes
        assert self.top <= SBUF_BYTES, f"SBUF overflow {self.top}"
        ap = self.big[:, off:off + n * _DS[dtype]].bitcast(dtype)
        if len(shape) == 3:
            ap = ap.rearrange("p (a b) -> p a b", a=shape[1])
        if shape[0] < 128:
            ap = ap[0:shape[0]]
        self.ntile += 1
        return Tl(ap, self.P.buf(f"t{self.ntile}"), tag)

    def ring(self, tag, shape, dtype, n):
        return Ring([self.tile(shape, dtype, f"{tag}{i}") for i in range(n)])

    def mark(self):
        return self.top

    def cv_start(self, l):
        Ld = self.L[l]
        for e in range(16):
            for key in ("wg", "wu", "wd"):
                if key in self.WB[l]:
                    self._cvq.append((Ld[key][e], self.WB[l][key][e]))

    def cv_issue(self, n=1):
        for _ in range(n):
            if not self._cvq:
                return
            src, dst = self._cvq.pop(0)
            self.P.dma("pool", "cv", lambda en, src=src, dst=dst: en.dma_start(
                out=dst.rearrange("(c p) f -> p c f", p=128), in_=src.rearrange("(c p) f -> p c f", p=128)))

    def bg_step(self, k=1):
        self._cvc = getattr(self, "_cvc", 0) + 1
        if self._cvc % self.cv_div == 0:
            self.cv_issue()
        g = getattr(self, "_bg", None)
        self._bgc = getattr(self, "_bgc", 0) + 1
        if self._bgc % getattr(self, "bg_div", 1) != 0:
            return
        for _ in range(k):
            if g is None:
                return
            try:
                next(g)
            except StopIteration:
                self._bg = g = None

    def bg_drain(self):
        while getattr(self, "_bg", None) is not None:
            self.bg_step()

    def release(self, m):
        self.top = m
        self.P.barrier()

    def pipeline(self, n, stages):
        ns = len(stages)
        for step in range(n + ns - 1):
            for si in reversed(range(ns)):
                i = step - si
                if 0 <= i < n:
                    stages[si](i)

    def dma(self, q, tag, out, in_, reads=(), writes=()):
        return self.P.dma(q, tag, lambda e: e.dma_start(out=out, in_=in_), reads=reads, writes=writes)

    def load(self, q, t, src, ap=None, reads=()):
        return self.dma(q, t.tag, t.ap if ap is None else ap, src, reads=reads, writes=[t.b])

    def store(self, q, t, dst, ap=None, extra_w=()):
        return self.dma(q, t.tag, dst, t.ap if ap is None else ap, reads=[t.b], writes=list(extra_w))

    def mm(self, ps, out, lhsT, rhs, start, stop, reads):
        return self.P.op("pe", lambda e: e.matmul(out, lhsT, rhs, start=start, stop=stop), reads=reads, writes=[ps.b])

    def act(self, out, in_, func, reads, writes, **kw):
        return self.P.op("act", lambda e: e.activation(out, in_, func, **kw), reads=reads, writes=writes)

    def tt(self, eng, out, a, b, op, reads, writes):
        return self.P.op(eng, lambda e: e.tensor_tensor(out, a, b, op), reads=reads, writes=writes)

    def stt(self, out, in0, scalar, in1, op0, op1, reads, writes):
        return self.P.op("dve", lambda e: e.scalar_tensor_tensor(out, in0, scalar, in1, op0, op1), reads=reads, writes=writes)

    def recip(self, out, in_, reads, writes):
        return self.P.op("dve", lambda e: e.reciprocal(out, in_), reads=reads, writes=writes)

    def copy(self, eng, out, in_, reads, writes):
        if eng == "act":
            return self.P.op("act", lambda e: e.copy(out, in_), reads=reads, writes=writes)
        return self.P.op(eng, lambda e: e.tensor_copy(out, in_), reads=reads, writes=writes)

    def build(self):
        nc = self.nc
        I = self.inp
        self.x_d = I("x", [S, D])
        self.ctx_d = I("ctx", [C, D])
        self.cT_d = I("cT", [128, 16, 2])
        self.cst_d = I("consts", [7, 128, 128])
        self.cs0_d = I("cs0", [2, 128, S])
        self.cs1_d = I("cs1", [2, 64, S])
        L = []
        for l in range(2):
            d = dict(mod_w=I(f"l{l}_mod_w", [D, 6 * D]), mod_b=I(f"l{l}_mod_b", [1, 6 * D]),
                     g1=I(f"l{l}_norm1_g", [1, D]), g2=I(f"l{l}_norm2_g", [1, D]),
                     router=I(f"l{l}_router", [D, 16]), wg=I(f"l{l}_w_gate", [16, D, 1024]),
                     wu=I(f"l{l}_w_up", [16, D, 1024]), wd=I(f"l{l}_w_down", [16, 1024, D]),
                     w_out=I(f"l{l}_w_out", [D, D]))
            L.append(d)
        L[0].update(w_in=I("l0_w_in", [D, 3584]), qg=I("l0_q_norm_g", [128, 1]), kg=I("l0_k_norm_g", [128, 1]),
                    dwT=I("l0_dwT", [128, 8, 31]), cvec=I("l0_cvec", [128, 3, 8]))
        L[1].update(w_dqkv=I("l1_w_dqkv", [D, 2112]), gq=I("l1_gq", [128, 12]), w_uq=I("l1_w_uq", [1536, 3072]),
                    gkv=I("l1_gkv", [128, 4]), w_ukv=I("l1_w_ukv", [512, 4096]))
        self.fg_d = I("final_norm_g", [1, D])
        self.L = L
        self.out_d = nc.dram_tensor("out", [S, D], F32, kind="ExternalOutput")
        sc = self.scratch
        self.XR = sc("XR", [T, D], F32)
        self.ADA = [sc(f"ADA{l}", [2, 6 * D], F32) for l in range(2)]
        self.QT = sc("QT", [1024, T], BF16)
        self.KT = sc("KT", [256, T], BF16)
        self.V0 = sc("V0", [T, 256], BF16)
        self.UG = sc("UG", [1024, T], BF16)
        self.AT = sc("AT", [D, T], BF16)
        self.H2 = sc("H2", [T, D], BF16)
        self.CQT = sc("CQT", [1536, S], BF16)
        self.CKVT = sc("CKVT", [512, T], BF16)
        self.KPET = sc("KPET", [64, T], BF16)
        self.QNT = sc("QNT", [2048, S], BF16)
        self.QPT = sc("QPT", [1024, S], BF16)
        self.KNT = sc("KNT", [2048, T], BF16)
        self.V1 = sc("V1", [T, 2048], BF16)
        self.WB = [dict(wg=sc("WG0", [16, D, 1024], BF16)),
                   dict(wg=sc("WG1", [16, D, 1024], BF16), wu=sc("WU1", [16, D, 1024], BF16), wd=sc("WD1", [16, 1024, D], BF16))]
        self._cvq = []
        self.cv_div = 3

        cb = self.tile([128, 7, 128], BF16, "cb")
        self.load("pool", cb, self.cst_d.ap().rearrange("c p f -> p c f"))
        self.cb = cb
        self.identb, self.o128, self.o1024, self.ones1 = cb[:, 0, :], cb[:, 1, :], cb[:, 2, :], cb[:, 3, :]
        self.rot128, self.rot64 = cb[:, 4, :], cb[0:64, 5, 0:64]
        idf = self.tile([128, 128], F32, "idf")
        self.load("sp", idf, self.cst_d[0])
        self.idf = idf
        self.affT = self.tile([16, T], F32)
        ept = self.tile([128, 1], F32)
        self.P.op("dve", lambda e: e.memset(ept.ap, EPS), writes=[ept.b])
        self._eps = ept
        self.idxT = self.tile([128, 3, 16], I32)
        self.gateT = self.tile([128, 3, 16], F32)

        m = self.mark()
        r = self.ring("x", [128, D], F32, 3)
        for tb in range(NB):
            t = r.next()
            src = self.x_d[tb * 128:(tb + 1) * 128, :] if tb < 16 else self.ctx_d[(tb - 16) * 128:(tb - 15) * 128, :]
            self.load("sp", t, src)
            self.store("act", t, self.XR[tb * 128:(tb + 1) * 128, :])
        self.release(m)

        def ada_then_mix0():
            m_bg = self.mark()
            self.ada_setup()
            for _ in self.ada_gen([(0, n) for n in range(8)]):
                pass
            self.P.barrier()
            self._bg = self.ada_gen([(0, n) for n in range(8, 24)] + [(1, n) for n in range(24)])
            self.layer0_mixer(m_bg)

        steps = [
            ("mix0", ada_then_mix0), ("moe0", lambda: self.moe(0)),
            ("mix1", self.layer1_mixer), ("moe1", lambda: self.moe(1)),
        ]
        for name, fn in steps:
            fn()
            if self.stop == name:
                break
        self.final()
        self.P.emit(final_tags=["fin0", "fin1"])
        return nc

    def ada_setup(self):
        ct = self.tile([128, 16, 2], F32, "c")
        sct = self.tile([128, 16, 2], BF16)
        self.load("sp", ct, self.cT_d.ap())
        self.act(sct.ap, ct.ap, AF.Silu, [ct.b], [sct.b])
        self._ada = dict(sct=sct, wr=self.ring("aw", [128, 16, 512], BF16, 2),
                         br=self.ring("ab", [2, 512], F32, 2), orr=self.ring("ao", [2, 512], F32, 1))

    def ada_gen(self, jobs):
        A = self._ada
        sct = A["sct"]
        loaded = {}

        def issue(idx):
            l, n = jobs[idx]
            Ld = self.L[l]
            w = A["wr"].next()
            self.load("pool", w, Ld["mod_w"][:, n * 512:(n + 1) * 512].rearrange("(c p) n -> p c n", p=128))
            bt = A["br"].next()
            self.load("sp", bt, Ld["mod_b"][0:1, n * 512:(n + 1) * 512].partition_broadcast(2))
            loaded[idx] = (w, bt)
        issue(0)
        for idx in range(len(jobs)):
            if idx + 1 < len(jobs):
                issue(idx + 1)
            l, n = jobs[idx]
            w, bt = loaded.pop(idx)
            ps = self.pst[7]
            for kc in range(16):
                self.mm(ps, ps[0:2, :], sct[:, kc, :], w[:, kc, :], kc == 0, kc == 15, [sct.b, w.b])
            o = A["orr"].next()
            self.tt("dve", o.ap, ps[0:2, :], bt.ap, ALU.add, [ps.b, bt.b], [o.b])
            self.store("sp", o, self.ADA[l][:, n * 512:(n + 1) * 512])
            yield

    def norm(self, l, which, nblk, HT, HTb, write_tok, post=()):
        m = self.mark()
        Ld = self.L[l]
        o_sh, o_sc = (0, 2048) if which == 1 else (6144, 8192)
        t1r = self.ring("t1", [128, D], F32, 2)
        xr = self.ring("x", [128, D], F32, 3)
        gt = t1r.t[0]
        self.load("sp", gt, Ld["g1" if which == 1 else "g2"].ap().partition_broadcast(128))
        mods = []
        for r in range(2 if nblk > 16 else 1):
            dt_ = F32 if r == 0 else BF16
            a = self.tile([128, D], dt_, f"a{r}")
            sh = self.tile([128, D], dt_, f"s{r}")
            if r == 0:
                self.load("sp", a, self.ADA[l][r:r + 1, o_sc:o_sc + D].partition_broadcast(128))
                self.load("sp", sh, self.ADA[l][r:r + 1, o_sh:o_sh + D].partition_broadcast(128))
                self.stt(a.ap, a.ap, 1.0, gt.ap, ALU.add, ALU.mult, [a.b, gt.b], [a.b])
            else:
                ta, ts = xr.t[0], xr.t[1]
                self.load("sp", ta, self.ADA[l][r:r + 1, o_sc:o_sc + D].partition_broadcast(128))
                self.load("sp", ts, self.ADA[l][r:r + 1, o_sh:o_sh + D].partition_broadcast(128))
                self.stt(a.ap, ta.ap, 1.0, gt.ap, ALU.add, ALU.mult, [ta.b, gt.b], [a.b])
                self.copy("pool", sh.ap, ts.ap, [ts.b], [sh.b])
            mods.append((a, sh))
        hbr = self.ring("hb", [128, D], BF16, 4)
        smr = self.ring("sm", [128, 1], F32, 15)
        tpr = Ring(self.pst[0:4])
        st = {}

        def s0(tb):
            x = xr.next()
            self.load("sp" if tb % 2 == 0 else "act", x, self.XR[tb * 128:(tb + 1) * 128, :])
            st[tb] = dict(x=x, hb=hbr.next())

        def s1(tb):
            x, hb = st[tb]["x"], st[tb]["hb"]
            ss, sq = smr.next(), smr.next()
            self.act(hb.ap, x.ap, AF.Square, [x.b], [hb.b, ss.b], scale=float(D ** -0.5), accum_out=ss.ap)
            self.act(sq.ap, ss.ap, AF.Sqrt, [ss.b], [sq.b], bias=EPS_AP(self), scale=1.0)
            st[tb]["sq"] = sq

        def s2(tb):
            a, sh = mods[0 if tb < 16 else 1]
            x, sq = st[tb]["x"], st[tb]["sq"]
            rs = smr.next()
            self.recip(rs.ap, sq.ap, [sq.b], [rs.b])
            t1 = t1r.t[tb % 2]
            self.stt(t1.ap, x.ap, rs.ap, a.ap, ALU.mult, ALU.mult, [x.b, rs.b, a.b], [t1.b])

        def s3(tb):
            a, sh = mods[0 if tb < 16 else 1]
            t1, hb = t1r.t[tb % 2], st[tb]["hb"]
            self.tt("pool", hb.ap, t1.ap, sh.ap, ALU.add, [t1.b, sh.b], [hb.b])
            if write_tok:
                self.store("sp", hb, self.H2[tb * 128:(tb + 1) * 128, :])

        def s4(tb):
            hb = st[tb]["hb"]
            pss = []
            for half in range(2):
                ps = tpr.next()
                pv = ps.ap.bitcast(BF16)
                for j in range(8):
                    fc = half * 8 + j
                    self.P.op("pe", lambda e, pv=pv, j=j, fc=fc, hb=hb: e.transpose(pv[:, j * 128:(j + 1) * 128], hb[:, fc * 128:(fc + 1) * 128], self.identb),
                              reads=[hb.b, self.cb.b], writes=[ps.b])
                pss.append((ps, pv))
            st[tb]["ps"] = pss

        def s5(tb):
            for half, (ps, pv) in enumerate(st[tb]["ps"]):
                self.copy("act" if half == 0 else "dve", HT[:, half * 8:(half + 1) * 8, tb * 128:(tb + 1) * 128],
                          pv.rearrange("p (c t) -> p c t", c=8), [ps.b], [HTb[tb]])
            self.bg_step()
        self.pipeline(nblk, [s0, s1, s2, s3, s4, s5] + list(post))
        self.release(m)

    def alloc_HT(self):
        HT = self.tile([128, 16, T], BF16)
        HTb = [self.P.buf(f"HT{i}") for i in range(NB)]
        return HT.ap, HTb

    def proj_fm(self, w, c0, mp, src, srcb, nk, t0, n):
        ps = self.pr.next()
        for kc in range(nk):
            self.mm(ps, ps[0:mp, 0:n], w[:, kc, c0:c0 + mp], src[:, kc, t0:t0 + n], kc == 0, kc == nk - 1, [w.b] + srcb)
        return ps

    def rope_tail(self, qn, mp, n, t0, cs, rot, dst, rings, is_ctx=False):
        if is_ctx:
            self.store("sp", qn, dst, ap=qn[0:mp, 0:n])
            return
        ps3 = self.pr.next()
        self.mm(ps3, ps3[0:mp, 0:n], rot, qn[0:mp, 0:n], True, True, [qn.b, self.cb.b])
        t1, t2, qo = rings["t1"].next(), rings["t2"].next(), rings["qo"].next()
        self.tt("pool", t1[0:mp, 0:n], qn[0:mp, 0:n], cs[0:mp, 0, t0:t0 + n], ALU.mult, [qn.b, cs.b], [t1.b])
        self.tt("dve", t2[0:mp, 0:n], ps3[0:mp, 0:n], cs[0:mp, 1, t0:t0 + n], ALU.mult, [ps3.b, cs.b], [t2.b])
        self.tt("pool", qo[0:mp, 0:n], t1[0:mp, 0:n], t2[0:mp, 0:n], ALU.add, [t1.b, t2.b], [qo.b])
        self.store("sp", qo, dst, ap=qo[0:mp, 0:n])

    def rope_rings(self):
        return dict(qn=self.ring("qn", [128, 512], BF16, 3), t1=self.ring("rt1", [128, 512], F32, 2),
                    t2=self.ring("rt2", [128, 512], F32, 2), qo=self.ring("qo", [128, 512], BF16, 2))

    def layer0_mixer(self, m_bg):
        Ld = self.L[0]
        m0 = self.mark()
        HT, HTb = self.alloc_HT()
        self.bg_div = 3
        self.norm(0, 1, NB, HT, HTb, False)
        self.bg_div = 3
        m = self.mark()
        gcol = self.tile([128, 2], F32, "gc")
        self.load("sp", gcol, Ld["qg"].ap(), ap=gcol[:, 0:1])
        self.load("sp", gcol, Ld["kg"].ap(), ap=gcol[:, 1:2])
        cs = self.tile([128, 2, S], BF16, "cs")
        self.load("pool", cs, self.cs0_d.ap().rearrange("c p t -> p c t"))
        wr = self.ring("w", [128, 16, 512], BF16, 3)
        rr = self.rope_rings()
        sqr = self.ring("sq", [128, 512], BF16, 3)
        sdr = self.ring("sd", [128, 512], F32, 1)
        rsr = self.ring("rs", [128, 512], F32, 2)
        TG = [(tg * 512, 512) for tg in range(4)] + [(2048, 256)]

        def hb_of(t0, n):
            return HTb[t0 // 128:(t0 + n) // 128]

        def loadw(p):
            w = wr.next()
            self.load("pool", w, Ld["w_in"][:, p * 512:(p + 1) * 512].rearrange("(c p) n -> p c n", p=128))
            return w

        items = []
        for p in range(2):
            for j in range(4):
                h = p * 4 + j
                for (t0, n) in TG:
                    items.append((p, j, gcol[:, 0:1], self.QT[h * 128:(h + 1) * 128, :], t0, n))
        for j in range(2):
            for (t0, n) in TG:
                items.append((2, j, gcol[:, 1:2], self.KT[j * 128:(j + 1) * 128, :], t0, n))
        wcur = {}

        def getw(p):
            if p not in wcur:
                wcur[p] = loadw(p)
            return wcur[p]
        stq = {}

        def qA(i):
            p, j, g_ap, dst, t0, n = items[i]
            w = getw(p)
            if j == 0 and t0 == 0 and p < 2:
                getw(p + 1)
            ps = self.proj_fm(w, j * 128, 128, HT, hb_of(t0, n), 16, t0, n)
            sq = sqr.next()
            self.act(sq[:, 0:n], ps[:, 0:n], AF.Square, [ps.b], [sq.b])
            stq[i] = [ps, sq]

        def qB(i):
            p, j, g_ap, dst, t0, n = items[i]
            ps, sq = stq[i]
            ps2 = self.pr.next()
            self.mm(ps2, ps2[:, 0:n], self.o128, sq[:, 0:n], True, True, [sq.b, self.cb.b])
            sd = sdr.next()
            self.act(sd[:, 0:n], ps2[:, 0:n], AF.Sqrt, [ps2.b], [sd.b], bias=EPS_AP(self), scale=1.0)
            rs = rsr.next()
            self.recip(rs[:, 0:n], sd[:, 0:n], [sd.b], [rs.b])
            qn = rr["qn"].next()
            self.stt(qn[:, 0:n], ps[:, 0:n], g_ap, rs[:, 0:n], ALU.mult, ALU.mult, [ps.b, rs.b, gcol.b], [qn.b])
            stq[i] = qn

        def qC(i):
            p, j, g_ap, dst, t0, n = items[i]
            self.rope_tail(stq.pop(i), 128, n, t0, cs, self.rot128, dst[:, t0:t0 + n], rr, is_ctx=(t0 >= S))
            self.bg_step()
        self.pipeline(len(items), [qA, qB, qC])
        w = getw(2)
        wun, wgn = loadw(3), loadw(5)
        vr = self.ring("v", [128, 256], BF16, 2)
        for tb in range(NB):
            ps = self.pr.next()
            for kc in range(16):
                self.mm(ps, ps[:, 0:256], HT[:, kc, tb * 128:(tb + 1) * 128], w[:, kc, 256:512], kc == 0, kc == 15, [w.b, HTb[tb]])
            v = vr.next()
            self.copy("act", v.ap, ps[:, 0:256], [ps.b], [v.b])
            self.store("sp", v, self.V0[tb * 128:(tb + 1) * 128, :])
        sgr = self.ring("sg", [128, 512], BF16, 2)
        ugr = self.ring("ug", [128, 512], BF16, 2)
        for p in range(2):
            if p == 1:
                wgn = loadw(6)
            wu, wg = wun, wgn
            if p == 0:
                wun = loadw(4)
            for j in range(4):
                c = p * 4 + j
                for (t0, n) in TG:
                    self.bg_step()
                    psu = self.proj_fm(wu, j * 128, 128, HT, hb_of(t0, n), 16, t0, n)
                    psg = self.proj_fm(wg, j * 128, 128, HT, hb_of(t0, n), 16, t0, n)
                    sg = sgr.next()
                    self.act(sg[:, 0:n], psg[:, 0:n], AF.Sigmoid, [psg.b], [sg.b])
                    ug = ugr.next()
                    self.tt("dve", ug[:, 0:n], psu[:, 0:n], sg[:, 0:n], ALU.mult, [psu.b, sg.b], [ug.b])
                    self.store("sp", ug, self.UG[c * 128:(c + 1) * 128, t0:t0 + n], ap=ug[:, 0:n])
        self.release(m0)
        self.bg_div = 1
        self.cv_start(0)
        self.attention(128 ** -0.5, 8, lambda h: (h // 4, self.KT[(h // 4) * 128:(h // 4 + 1) * 128, :], None),
                       lambda h: self.V0[:, (h // 4) * 128:(h // 4 + 1) * 128],
                       lambda h: (self.QT[h * 128:(h + 1) * 128, :], None), True, T)
        self.bg_drain()
        self.release(m_bg)
        if self.stop == "attn0":
            return
        self.conv0()
        self.outproj(0, 5)

    def attention(self, scale, nheads, k_src, v_src, q_src, with_ctx_q, qlen):
        m = self.mark()
        ktr = self.ring("kt", [128, T], BF16, 2)
        kpr = self.ring("kp", [64, T], BF16, 1)
        vr = self.ring("vv", [128, NB, 128], BF16, 2)
        qtr = self.ring("qt", [128, qlen], BF16, 2)
        qpr = self.ring("qp", [64, qlen], BF16, 2)
        ptr_ = self.ring("pT", [128, 512], BF16, 4)
        rsr = self.ring("rs", [128, 512], F32, 2)
        orr = self.ring("o", [128, 512], BF16, 2)
        Sr = Ring(self.pst[0:3])
        Or = Ring(self.pst[3:5])
        Ur = Ring(self.pst[5:7])
        groups = [(qg * 512, 512, list(range(NB))) for qg in range(4)]
        if with_ctx_q:
            groups.append((2048, 256, [16, 17]))
        kp = None
        last_k = None
        for h in range(nheads):
            kkey, ks, kps = k_src(h)
            if kkey != last_k:
                kt = ktr.next()
                self.load("sp", kt, ks)
                v = vr.next()
                self.load("act", v, v_src(h).rearrange("(b p) d -> p b d", p=128))
                last_k = kkey
            if kps is not None and kp is None:
                kp = kpr.next()
                self.load("sp", kp, kps)
            qs, qps = q_src(h)
            qt = qtr.next()
            self.load("sp", qt, qs)
            qp = None
            if qps is not None:
                qp = qpr.next()
                self.load("act", qp, qps)
            for (q0, n, kbs) in groups:
                self.bg_step()
                pso, psu = Or.next(), Ur.next()

                def smm(kb):
                    s = Sr.next()
                    rd = [kt.b, qt.b]
                    self.mm(s, s[:, 0:n], kt[:, kb * 128:(kb + 1) * 128], qt[:, q0:q0 + n], True, qp is None, rd)
                    if qp is not None:
                        self.mm(s, s[:, 0:n], kp[:, kb * 128:(kb + 1) * 128], qp[:, q0:q0 + n], False, True, [kp.b, qp.b])
                    return s
                pend = [smm(kb) for kb in kbs[:2]]
                for i, kb in enumerate(kbs):
                    s = pend.pop(0)
                    p = ptr_.next()
                    self.act(p[:, 0:n], s[:, 0:n], AF.Exp, [s.b], [p.b], scale=float(scale))
                    self.mm(pso, pso[:, 0:n], v[:, kb, :], p[:, 0:n], i == 0, i == len(kbs) - 1, [v.b, p.b])
                    self.mm(psu, psu[:, 0:n], self.ones1, p[:, 0:n], i == 0, i == len(kbs) - 1, [p.b, self.cb.b])
                    if i + 2 < len(kbs):
                        pend.append(smm(kbs[i + 2]))
                r = rsr.next()
                self.recip(r[:, 0:n], psu[:, 0:n], [psu.b], [r.b])
                o = orr.next()
                self.tt("dve", o[:, 0:n], pso[:, 0:n], r[:, 0:n], ALU.mult, [pso.b, r.b], [o.b])
                self.store("sp", o, self.AT[h * 128:(h + 1) * 128, q0:q0 + n], ap=o[:, 0:n])
        self.release(m)

    def conv0(self):
        Ld = self.L[0]
        m = self.mark()
        PW = 15 + S + 15 + 15 + C + 15
        CS0 = S + 30
        ugt = self.tile([128, 8, PW], BF16, "ugt")
        zb = self.P.buf("ugt_zero")
        self.P.op("pool", lambda e: e.memset(ugt.ap, 0.0), writes=[ugt.b, zb])
        for c in range(8):
            self.load("sp", ugt, self.UG[c * 128:(c + 1) * 128, 0:S], ap=ugt[:, c, 15:15 + S], reads=[zb])
            self.load("act", ugt, self.UG[c * 128:(c + 1) * 128, S:T], ap=ugt[:, c, CS0 + 15:CS0 + 15 + C], reads=[zb])
        dwt = self.tile([128, 8, 31], F32, "dwt")
        self.load("sp", dwt, Ld["dwT"].ap())
        cv = self.tile([128, 3, 8], F32, "cv")
        self.load("sp", cv, Ld["cvec"].ap())
        Dm = self.tile([128, 248, 128], BF16)
        for c in range(8):
            self.P.op("dve", lambda e, c=c: e.tensor_tensor(
                Dm[:, c * 31:(c + 1) * 31, :], self.identb.unsqueeze(1).to_broadcast([128, 31, 128]),
                dwt[:, c, :].unsqueeze(2).to_broadcast([128, 31, 128]), ALU.mult),
                reads=[self.cb.b, dwt.b], writes=[Dm.b])
        Yr = self.ring("Y", [128, 8, 512], F32, 2)
        ybr = self.ring("yb", [128, 512], BF16, 2)
        yqr = self.ring("yq", [128, 512], BF16, 2)
        st = [self.tile([128, 512], F32) for _ in range(5)]
        zr = self.ring("z", [128, 512], F32, 2)
        z2r = self.ring("z2", [128, 512], F32, 2)
        cor = self.ring("co", [128, 512], BF16, 2)
        Cr = Ring(self.pst[0:4])
        TG = [(tg * 512, 512, tg * 512) for tg in range(4)] + [(2048, 256, CS0)]
        for (t0, n, off) in TG:
            Y = Yr.next()
            s1, s2 = self.pst[4], self.pst[5]
            pend = None
            for c in range(8):
                self.bg_step()
                ps = Cr.next()
                for k in range(31):
                    self.mm(ps, ps[:, 0:n], Dm[:, c * 31 + k, :], ugt[:, c, off + k:off + k + n], k == 0, k == 30, [Dm.b, ugt.b])
                if pend is not None:
                    pc, pyb, pyq = pend
                    self.mm(s1, s1[:, 0:n], self.o1024, pyb[:, 0:n], pc == 0, False, [pyb.b, self.cb.b])
                    self.mm(s2, s2[:, 0:n], self.o1024, pyq[:, 0:n], pc == 0, False, [pyq.b, self.cb.b])
                self.act(Y[:, c, 0:n], ps[:, 0:n], AF.Identity, [ps.b, cv.b], [Y.b], bias=cv[:, 0, c:c + 1], scale=1.0)
                yb, yq = ybr.next(), yqr.next()
                self.copy("pool", yb[:, 0:n], Y[:, c, 0:n], [Y.b], [yb.b])
                self.tt("dve", yq[:, 0:n], Y[:, c, 0:n], Y[:, c, 0:n], ALU.mult, [Y.b], [yq.b])
                pend = (c, yb, yq)
            pc, pyb, pyq = pend
            self.mm(s1, s1[:, 0:n], self.o1024, pyb[:, 0:n], False, True, [pyb.b, self.cb.b])
            self.mm(s2, s2[:, 0:n], self.o1024, pyq[:, 0:n], False, True, [pyq.b, self.cb.b])
            mu, msq, var, rstd, nmr = st
            self.copy("act", mu[:, 0:n], s1[:, 0:n], [s1.b], [mu.b])
            self.tt("dve", msq[:, 0:n], mu[:, 0:n], mu[:, 0:n], ALU.mult, [mu.b], [msq.b])
            self.tt("dve", var[:, 0:n], s2[:, 0:n], msq[:, 0:n], ALU.subtract, [s2.b, msq.b], [var.b])
            self.act(msq[:, 0:n], var[:, 0:n], AF.Sqrt, [var.b], [msq.b], bias=EPS_AP(self), scale=1.0)
            self.recip(rstd[:, 0:n], msq[:, 0:n], [msq.b], [rstd.b])
            self.stt(nmr[:, 0:n], mu[:, 0:n], -1.0, rstd[:, 0:n], ALU.mult, ALU.mult, [mu.b, rstd.b], [nmr.b])
            for c in range(8):
                z, z2, co = zr.next(), z2r.next(), cor.next()
                self.tt("dve", z[:, 0:n], Y[:, c, 0:n], rstd[:, 0:n], ALU.mult, [Y.b, rstd.b], [z.b])
                self.tt("pool", z2[:, 0:n], z[:, 0:n], nmr[:, 0:n], ALU.add, [z.b, nmr.b], [z2.b])
                self.act(co[:, 0:n], z2[:, 0:n], AF.Silu, [z2.b, cv.b], [co.b], scale=cv[:, 1, c:c + 1], bias=cv[:, 2, c:c + 1])
                self.store("sp", co, self.AT[1024 + c * 128:1024 + (c + 1) * 128, t0:t0 + n], ap=co[:, 0:n])
        self.release(m)

    def outproj(self, l, ngroups):
        Ld = self.L[l]
        m = self.mark()
        W = self.tile([128, 16, D], BF16, "W")
        for p in range(4):
            self.load("pool", W, Ld["w_out"][:, p * 512:(p + 1) * 512].rearrange("(c p) n -> p c n", p=128), ap=W[:, :, p * 512:(p + 1) * 512])
        gas = []
        for r in range(2 if ngroups > 4 else 1):
            ga = self.tile([128, D], F32, f"ga{r}")
            self.load("sp", ga, self.ADA[l][r:r + 1, 4096:6144].partition_broadcast(128))
            gas.append(ga)
        atr = self.ring("at", [128, 16, 512], BF16, 2)
        xr = self.ring("x", [128, D], F32, 3)
        tr = self.ring("tm", [128, 512], F32, 3)
        for tg in range(ngroups):
            t0, n = tg * 512, (512 if tg < 4 else 256)
            ga = gas[0 if tg < 4 else 1]
            at = atr.next()
            self.load("sp", at, self.AT[:, t0:t0 + n].rearrange("(c p) t -> p c t", p=128), ap=at[:, :, 0:n])
            for bi in range(n // 128):
                tb = tg * 4 + bi
                self.bg_step()
                x = xr.next()
                self.load("act", x, self.XR[tb * 128:(tb + 1) * 128, :])
                for dc in range(4):
                    ps = self.pr.next()
                    for fc in range(16):
                        self.mm(ps, ps.ap, at[:, fc, bi * 128:(bi + 1) * 128], W[:, fc, dc * 512:(dc + 1) * 512], fc == 0, fc == 15, [at.b, W.b])
                    tm = tr.next()
                    self.tt("dve", tm.ap, ps.ap, ga[:, dc * 512:(dc + 1) * 512], ALU.mult, [ps.b, ga.b], [tm.b])
                    self.tt("pool", x[:, dc * 512:(dc + 1) * 512], x[:, dc * 512:(dc + 1) * 512], tm.ap, ALU.add, [x.b, tm.b], [x.b])
                self.store("sp", x, self.XR[tb * 128:(tb + 1) * 128, :])
        self.release(m)

    def moe(self, l):
        Ld = self.L[l]
        nblk = NB if l == 0 else 16
        nj = 288 if l == 0 else 256
        JB = [(0, 128), (128, 128), (256, 32)][:3 if l == 0 else 2]
        m0 = self.mark()
        HT, HTb = self.alloc_HT()
        rw = self.tile([128, 16, 16], BF16, "rw")
        self.load("pool", rw, Ld["router"].ap().rearrange("(c p) e -> p c e", p=128))
        smr = self.ring("sm2", [128, 1], F32, 12)
        exr = self.ring("ex", [128, 16], F32, 3)
        afr = self.ring("af", [128, 16], F32, 3)
        str_ = {}

        rpr = Ring(self.pst[4:8])

        def rA(tb):
            ps = rpr.next()
            for kc in range(16):
                self.mm(ps, ps[:, 0:16], HT[:, kc, tb * 128:(tb + 1) * 128], rw[:, kc, :], kc == 0, kc == 15, [rw.b, HTb[tb]])
            mx, nmx, ssum, rr = smr.next(), smr.next(), smr.next(), smr.next()
            self.P.op("dve", lambda e, mx=mx, ps=ps: e.reduce_max(mx.ap, ps[:, 0:16], AX.X), reads=[ps.b], writes=[mx.b])
            self.P.op("dve", lambda e, mx=mx, nmx=nmx: e.tensor_scalar(nmx.ap, mx.ap, -1.0, None, ALU.mult), reads=[mx.b], writes=[nmx.b])
            ex = exr.next()
            self.act(ex.ap, ps[:, 0:16], AF.Exp, [ps.b, nmx.b], [ex.b, ssum.b], bias=nmx.ap, scale=1.0, accum_out=ssum.ap)
            self.recip(rr.ap, ssum.ap, [ssum.b], [rr.b])
            af = afr.next()
            self.P.op("dve", lambda e, af=af, ex=ex, rr=rr: e.tensor_scalar(af.ap, ex.ap, rr.ap, None, ALU.mult), reads=[ex.b, rr.b], writes=[af.b])
            str_[tb] = af

        def rB(tb):
            af = str_.pop(tb)
            ps2 = rpr.next()
            self.P.op("pe", lambda e, ps2=ps2, af=af: e.transpose(ps2[0:16, 0:128], af.ap, self.idf.ap), reads=[af.b, self.idf.b], writes=[ps2.b])
            self.copy("act", self.affT[:, tb * 128:(tb + 1) * 128], ps2[0:16, 0:128], [ps2.b], [self.affT.b])
        self.norm(l, 2, nblk, HT, HTb, True, post=[rA, rB])
        self.cv_issue(len(self._cvq))
        self.release(m0)
        m = self.mark()
        gas = [self.tile([128, D], BF16) for r in range(2 if l == 0 else 1)]
        slots = self.ring("ws", [128, 16 * 1024], BF16, 4)
        xsr = self.ring("xs", [128, D], BF16, len(JB))
        xtr = self.ring("xT", [128, 16, 288], BF16, 1)
        hdr = self.ring("hd", [128, 8, 288], BF16, 1)
        sgr = self.ring("sg", [128, 288], BF16, 2)
        yr = self.ring("y", [128, D], F32, 3)
        xrb = self.P.buf("xr_scatter")
        pending = []
        for r, ga in enumerate(gas):
            stg = yr.t[1 + r]
            self.load("sp", stg, self.ADA[l][r:r + 1, 10240:12288].partition_broadcast(128))
            self.copy("dve", ga.ap, stg.ap, [stg.b], [ga.b])

        def gather(e):
            out = []
            for jb, (j0, n) in enumerate(JB):
                xs = xsr.next()
                self.P.dma("pool", xs.tag, lambda en, xs=xs, n=n, jb=jb, e=e: en.indirect_dma_start(
                    out=xs[0:n, :], out_offset=None, in_=self.H2.ap(),
                    in_offset=bass.IndirectOffsetOnAxis(ap=self.idxT[0:n, jb, e:e + 1], axis=0)),
                    reads=[self.idxT.b], writes=[xs.b])
                out.append(xs)
            return out

        wpieces = {}
        wstate = [0]

        def ensure(k):
            while wstate[0] <= k and wstate[0] < 48:
                kk = wstate[0]
                e, which = divmod(kk, 3)
                sl = slots.next()
                key = ("wg", "wu", "wd")[which]
                pre = key in self.WB[l]
                src = (self.WB[l][key] if pre else Ld[key])[e]
                q = "sp" if pre else "pool"
                if which < 2:
                    self.load(q, sl, src.rearrange("(c p) f -> p c f", p=128), ap=sl.ap.rearrange("p (c f) -> p c f", c=16))
                else:
                    self.load(q, sl, src.rearrange("(c p) d -> p c d", p=128), ap=sl.ap.rearrange("p (c d) -> p c d", c=8))
                wpieces[kk] = sl
                wstate[0] += 1

        ensure(1)
        m2 = self.mark()
        work = Tl(yr.t[0].ap[0:16, :], yr.t[0].b, None)
        work2 = self.tile([16, 288], F32)
        vals = self.tile([16, 288], F32)
        idx = self.tile([16, 288], U32)
        idxf = work2

        def topk(src0, wk, rounds, o0):
            for r in range(rounds):
                src = src0 if r == 0 else wk.ap
                vs = vals[:, o0 + r * 8:o0 + (r + 1) * 8]
                ix = idx[:, o0 + r * 8:o0 + (r + 1) * 8]
                rd = [self.affT.b, wk.b]
                self.P.op("dve", lambda e, vs=vs, src=src: e.max(out=vs, in_=src), reads=rd, writes=[vals.b])
                self.P.op("dve", lambda e, vs=vs, ix=ix, src=src: e.max_index(out=ix, in_max=vs, in_values=src), reads=rd + [vals.b], writes=[idx.b])
                if r < rounds - 1:
                    self.P.op("dve", lambda e, vs=vs, src=src, wk=wk: e.match_replace(out=wk.ap, in_to_replace=vs, in_values=src, imm_value=-1.0),
                              reads=rd + [vals.b], writes=[wk.b])
        topk(self.affT[:, 0:S], work, 32, 0)
        if l == 0:
            topk(self.affT[:, S:T], Tl(work2.ap[:, 0:C], work2.b, None), 4, 256)
        self.copy("dve", idxf[:, 0:nj], idx[:, 0:nj], [idx.b], [idxf.b])
        if l == 0:
            self.P.op("dve", lambda e: e.tensor_scalar(idxf[:, 256:288], idxf[:, 256:288], float(S), None, ALU.add), reads=[idxf.b], writes=[idxf.b])
        for jb, (j0, n) in enumerate(JB):
            ps = self.pr.next()
            self.P.op("pe", lambda e, ps=ps, j0=j0, n=n: e.transpose(ps[0:n, 0:16], idxf[:, j0:j0 + n], self.idf[0:16, 0:16]), reads=[idxf.b, self.idf.b], writes=[ps.b])
            self.copy("dve", self.idxT[0:n, jb, :], ps[0:n, 0:16], [ps.b], [self.idxT.b])
            ps = self.pr.next()
            self.P.op("pe", lambda e, ps=ps, j0=j0, n=n: e.transpose(ps[0:n, 0:16], vals[:, j0:j0 + n], self.idf[0:16, 0:16]), reads=[vals.b, self.idf.b], writes=[ps.b])
            self.copy("act", self.gateT[0:n, jb, :], ps[0:n, 0:16], [ps.b], [self.gateT.b])
        self.release(m2)
        xs_next = gather(0)
        for e in range(16):
            xs_l = xs_next
            xt = xtr.next()
            for jb, (j0, n) in enumerate(JB):
                xs = xs_l[jb]
                for half in range(2):
                    ps = self.pr.next()
                    pv = ps.ap.bitcast(BF16)
                    for j in range(8):
                        fc = half * 8 + j
                        self.P.op("pe", lambda en, pv=pv, j=j, fc=fc, xs=xs, n=n: en.transpose(pv[:, j * 128:j * 128 + n], xs[0:n, fc * 128:(fc + 1) * 128], self.identb[0:n, 0:n]),
                                  reads=[xs.b, self.cb.b], writes=[ps.b])
                    self.copy("act" if half == 0 else "dve", xt[:, half * 8:(half + 1) * 8, j0:j0 + n],
                              pv.rearrange("p (c t) -> p c t", c=8)[:, :, 0:n], [ps.b], [xt.b])
            for fn in pending:
                fn()
            pending = []
            ensure(3 * e + 3)
            wg, wu, wd = wpieces[3 * e], wpieces[3 * e + 1], wpieces[3 * e + 2]
            wg3 = wg.ap.rearrange("p (c f) -> p c f", c=16)
            wu3 = wu.ap.rearrange("p (c f) -> p c f", c=16)
            wd3 = wd.ap.rearrange("p (c d) -> p c d", c=8)
            hd = hdr.next()
            for Fc in range(8):
                psg, psu = self.pr.next(), self.pr.next()
                for kc in range(16):
                    self.mm(psg, psg[:, 0:nj], wg3[:, kc, Fc * 128:(Fc + 1) * 128], xt[:, kc, 0:nj], kc == 0, kc == 15, [wg.b, xt.b])
                for kc in range(16):
                    self.mm(psu, psu[:, 0:nj], wu3[:, kc, Fc * 128:(Fc + 1) * 128], xt[:, kc, 0:nj], kc == 0, kc == 15, [wu.b, xt.b])
                sg = sgr.next()
                self.act(sg[:, 0:nj], psg[:, 0:nj], AF.Silu, [psg.b], [sg.b])
                self.tt("dve", hd[:, Fc, 0:nj], psu[:, 0:nj], sg[:, 0:nj], ALU.mult, [psu.b, sg.b], [hd.b])
            if e + 1 < 16:
                xs_next = gather(e + 1)
                ensure(3 * e + 5)
            for jb, (j0, n) in enumerate(JB):
                y = yr.next()
                ga = gas[0 if jb < 2 else 1]
                for dc in range(4):
                    ps = self.pr.next()
                    for Fc in range(8):
                        self.mm(ps, ps[0:n, :], hd[:, Fc, j0:j0 + n], wd3[:, Fc, dc * 512:(dc + 1) * 512], Fc == 0, Fc == 7, [hd.b, wd.b])
                    self.stt(y[0:n, dc * 512:(dc + 1) * 512], ps[0:n, :], self.gateT[0:n, jb, e:e + 1], ga[0:n, dc * 512:(dc + 1) * 512],
                             ALU.mult, ALU.mult, [ps.b, self.gateT.b, ga.b], [y.b])
                pending.append(lambda y=y, n=n, jb=jb, e=e: self.P.dma("pool", y.tag, lambda en: en.indirect_dma_start(
                    out=self.XR.ap(), out_offset=bass.IndirectOffsetOnAxis(ap=self.idxT[0:n, jb, e:e + 1], axis=0),
                    in_=y[0:n, :], in_offset=None, compute_op=ALU.add),
                    reads=[y.b, self.idxT.b, xrb], writes=[xrb]))
        for fn in pending:
            fn()
        self.release(m)

    def layer1_mixer(self):
        Ld = self.L[1]
        self.cv_start(1)
        m0 = self.mark()
        HT, HTb = self.alloc_HT()
        self.norm(1, 1, NB, HT, HTb, False)
        m = self.mark()
        TG = [(tg * 512, 512) for tg in range(4)] + [(2048, 256)]

        def hb_of(t0, n):
            return HTb[t0 // 128:(t0 + n) // 128]
        gq = self.tile([128, 12], F32, "gq")
        self.load("sp", gq, Ld["gq"].ap())
        gkv = self.tile([128, 4], F32, "gkv")
        self.load("sp", gkv, Ld["gkv"].ap())
        cs = self.tile([64, 2, S], BF16, "cs")
        self.load("pool", cs, self.cs1_d.ap().rearrange("c p t -> p c t"))
        wq = [self.tile([128, 16, 512], BF16, f"wq{i}") for i in range(4)]
        for i in range(4):
            self.load("pool", wq[i], Ld["w_dqkv"][:, i * 512:(i + 1) * 512].rearrange("(c p) n -> p c n", p=128))
        wk = self.tile([128, 16, 64], BF16, "wk")
        self.load("pool", wk, Ld["w_dqkv"][:, 2048:2112].rearrange("(c p) n -> p c n", p=128))
        raw = self.tile([128, 12, 512], F32)
        rawb = [self.P.buf(f"raw{i}") for i in range(12)]
        sqr = self.ring("sq", [128, 512], BF16, 3)
        sd = self.tile([128, 512], F32)
        rstd = self.tile([128, 512], F32)
        cqr = self.ring("cq", [128, 512], BF16, 3)
        rr = self.rope_rings()

        def lora(chunks, groups, gcol, nfeat, dst):
            nch = len(chunks)
            for (t0, n) in groups:
                pss = self.pst[7]
                stl = {}

                def lA(ci, t0=t0, n=n):
                    w, j = chunks[ci]
                    ps = self.proj_fm(w, j * 128, 128, HT, hb_of(t0, n), 16, t0, n)
                    self.copy("act", raw[:, ci, 0:n], ps[:, 0:n], [ps.b], [rawb[ci]])
                    sq = sqr.next()
                    self.tt("pool", sq[:, 0:n], raw[:, ci, 0:n], raw[:, ci, 0:n], ALU.mult, [rawb[ci]], [sq.b])
                    stl[ci] = sq

                def lB(ci, n=n):
                    self.bg_step()
                    sq = stl.pop(ci)
                    self.mm(pss, pss[:, 0:n], self.ones1, sq[:, 0:n], ci == 0, ci == nch - 1, [sq.b, self.cb.b])
                self.pipeline(nch, [lA, lB])
                self.act(sd[:, 0:n], pss[:, 0:n], AF.Sqrt, [pss.b], [sd.b], bias=EPS_AP(self), scale=1.0 / nfeat)
                self.recip(rstd[:, 0:n], sd[:, 0:n], [sd.b], [rstd.b])
                for ci in range(nch):
                    cq = cqr.next()
                    self.stt(cq[:, 0:n], raw[:, ci, 0:n], gcol[:, ci:ci + 1], rstd[:, 0:n], ALU.mult, ALU.mult, [rawb[ci], rstd.b, gcol.b], [cq.b])
                    self.store("sp", cq, dst[ci * 128:(ci + 1) * 128, t0:t0 + n], ap=cq[:, 0:n])
        lora([(wq[i // 4], i % 4) for i in range(12)], TG[:4], gq, 1536.0, self.CQT)
        lora([(wq[3], i) for i in range(4)], TG, gkv, 512.0, self.CKVT)
        stk = {}

        def kA(i):
            t0, n = TG[i]
            ps = self.proj_fm(wk, 0, 64, HT, hb_of(t0, n), 16, t0, n)
            qn = rr["qn"].next()
            self.copy("act", qn[0:64, 0:n], ps[0:64, 0:n], [ps.b], [qn.b])
            stk[i] = qn

        def kB(i):
            t0, n = TG[i]
            self.rope_tail(stk.pop(i), 64, n, t0, cs, self.rot64, self.KPET[:, t0:t0 + n], rr, is_ctx=(t0 >= S))
        self.pipeline(len(TG), [kA, kB])
        self.release(m)
        self.release(m0)
        m = self.mark()
        cs = self.tile([64, 2, S], BF16, "cs")
        self.load("pool", cs, self.cs1_d.ap().rearrange("c p t -> p c t"))
        cqt = self.tile([128, 12, S], BF16, "cqt")
        for c in range(12):
            self.load("sp" if c % 2 == 0 else "act", cqt, self.CQT[c * 128:(c + 1) * 128, :], ap=cqt[:, c, :])
        wr = self.ring("w", [128, 12, 192], BF16, 3)
        rr = self.rope_rings()
        qnr = self.ring("qnn", [128, 512], BF16, 3)
        items = [(h, t0, n) for h in range(16) for (t0, n) in TG[:4]]
        wcur = {}

        def getw(h):
            if h not in wcur:
                w = wr.next()
                self.load("pool", w, Ld["w_uq"][:, h * 192:(h + 1) * 192].rearrange("(c p) n -> p c n", p=128))
                wcur[h] = w
            return wcur[h]
        stu = {}

        def uA(i):
            h, t0, n = items[i]
            w = getw(h)
            if t0 == 0 and h + 1 < 16:
                getw(h + 1)
            ps = self.proj_fm(w, 0, 128, cqt.ap, [cqt.b], 12, t0, n)
            qn = qnr.next()
            self.copy("act", qn[:, 0:n], ps[:, 0:n], [ps.b], [qn.b])
            self.store("sp", qn, self.QNT[h * 128:(h + 1) * 128, t0:t0 + n], ap=qn[:, 0:n])
            ps = self.proj_fm(w, 128, 64, cqt.ap, [cqt.b], 12, t0, n)
            qp = rr["qn"].next()
            self.copy("dve", qp[0:64, 0:n], ps[0:64, 0:n], [ps.b], [qp.b])
            stu[i] = qp

        def uB(i):
            self.bg_step()
            h, t0, n = items[i]
            self.rope_tail(stu.pop(i), 64, n, t0, cs, self.rot64, self.QPT[h * 64:(h + 1) * 64, t0:t0 + n], rr)
        self.pipeline(len(items), [uA, uB])
        self.release(m)
        m = self.mark()
        ckt = self.tile([128, 4, T], BF16, "ckt")
        self.load("sp", ckt, self.CKVT.ap().rearrange("(c p) t -> p c t", p=128))
        W = self.tile([128, 4, 4096], BF16, "W")
        self.load("pool", W, Ld["w_ukv"].ap().rearrange("(c p) n -> p c n", p=128))
        knr = self.ring("kn", [128, 512], BF16, 3)
        for h in range(16):
            self.bg_step()
            for (t0, n) in TG:
                ps = self.proj_fm(W, h * 256, 128, ckt.ap, [ckt.b], 4, t0, n)
                kn = knr.next()
                self.copy("act" if h % 2 == 0 else "dve", kn[:, 0:n], ps[:, 0:n], [ps.b], [kn.b])
                self.store("sp", kn, self.KNT[h * 128:(h + 1) * 128, t0:t0 + n], ap=kn[:, 0:n])
        vr = self.ring("v", [128, 512], BF16, 3)
        for tb in range(NB):
            for hg in range(4):
                ps = self.pr.next()
                for hh in range(4):
                    h = hg * 4 + hh
                    for c in range(4):
                        self.mm(ps, ps[:, hh * 128:(hh + 1) * 128], ckt[:, c, tb * 128:(tb + 1) * 128], W[:, c, h * 256 + 128:h * 256 + 256], c == 0, c == 3, [ckt.b, W.b])
                v = vr.next()
                self.copy("act" if hg % 2 == 0 else "dve", v.ap, ps.ap, [ps.b], [v.b])
                self.store("sp", v, self.V1[tb * 128:(tb + 1) * 128, hg * 512:(hg + 1) * 512])
        self.release(m)
        kn_aps = [self.KNT[h * 128:(h + 1) * 128, :] for h in range(16)]
        kp_ap = self.KPET.ap()
        self.attention(192 ** -0.5, 16, lambda h: (h, kn_aps[h], kp_ap), lambda h: self.V1[:, h * 128:(h + 1) * 128],
                       lambda h: (self.QNT[h * 128:(h + 1) * 128, :], self.QPT[h * 64:(h + 1) * 64, :]), False, S)
        self.outproj(1, 4)

    def final(self):
        m = self.mark()
        gt = self.tile([128, D], F32, "g")
        self.load("sp", gt, self.fg_d.ap().partition_broadcast(128))
        xr = self.ring("x", [128, D], F32, 3)
        orr = self.ring("fin", [128, D], F32, 2)
        junk = self.ring("jk", [128, D], BF16, 2)
        smr = self.ring("sm", [128, 1], F32, 12)
        st = {}

        def s0(tb):
            x = xr.next()
            self.load("sp", x, self.XR[tb * 128:(tb + 1) * 128, :])
            st[tb] = dict(x=x)

        def s1(tb):
            x = st[tb]["x"]
            ss, sq, jk = smr.next(), smr.next(), junk.next()
            self.act(jk.ap, x.ap, AF.Square, [x.b], [jk.b, ss.b], scale=float(D ** -0.5), accum_out=ss.ap)
            self.act(sq.ap, ss.ap, AF.Sqrt, [ss.b], [sq.b], bias=EPS_AP(self), scale=1.0)
            st[tb]["sq"] = sq

        def s2(tb):
            d = st.pop(tb)
            rs, o = smr.next(), orr.next()
            self.recip(rs.ap, d["sq"].ap, [d["sq"].b], [rs.b])
            self.stt(o.ap, d["x"].ap, rs.ap, gt.ap, ALU.mult, ALU.mult, [d["x"].b, rs.b, gt.b], [o.b])
            self.store("act", o, self.out_d[tb * 128:(tb + 1) * 128, :])
        self.pipeline(16, [s0, s1, s2])
        self.release(m)


def EPS_AP(k):
    return k._eps.ap


def _rope_tables(d_rot):
    rows = S // 64
    row = np.repeat(np.arange(rows, dtype=np.float32), 64)
    col = np.tile(np.arange(64, dtype=np.float32), rows)
    n_axis = d_rot // 4
    inv = (np.float32(10000.0) ** (-np.arange(n_axis, dtype=np.float32) / np.float32(n_axis))).astype(np.float32)
    ang = np.concatenate([row[:, None] * inv, col[:, None] * inv], axis=-1).astype(np.float32)
    half = d_rot // 2
    idx = np.arange(d_rot) % half
    cs = np.stack([np.cos(ang)[:, idx].T, np.sin(ang)[:, idx].T]).astype(np.float32)
    return np.ascontiguousarray(cs)


def _rot_lhsT(d_rot, pad=128):
    half = d_rot // 2
    m = np.zeros((pad, pad), np.float32)
    for j in range(half):
        m[j + half, j] = -1.0
        m[j, j + half] = 1.0
    return m


def _colmajor(v, nchunk):
    return np.ascontiguousarray(np.asarray(v, np.float32).reshape(nchunk, 128).T)


def _shared(inputs):
    f = lambda k: np.ascontiguousarray(np.asarray(inputs[k], np.float32))
    sh = {}
    consts = np.zeros((7, 128, 128), np.float32)
    consts[0] = np.eye(128, dtype=np.float32)
    consts[1] = 1.0 / 128
    consts[2] = 1.0 / 1024
    consts[3] = 1.0
    consts[4] = _rot_lhsT(128)
    consts[5] = _rot_lhsT(64)
    sh["consts"] = consts
    sh["cs0"] = _rope_tables(128)
    sh["cs1"] = _rope_tables(64)
    for l in range(2):
        sh[f"l{l}_mod_w"] = f(f"l{l}_mod_w")
        sh[f"l{l}_mod_b"] = f(f"l{l}_mod_b").reshape(1, -1)
        sh[f"l{l}_norm1_g"] = f(f"l{l}_norm1_g").reshape(1, -1)
        sh[f"l{l}_norm2_g"] = f(f"l{l}_norm2_g").reshape(1, -1)
        sh[f"l{l}_router"] = f(f"l{l}_router")
        sh[f"l{l}_w_gate"] = f(f"l{l}_w_gate")
        sh[f"l{l}_w_up"] = f(f"l{l}_w_up")
        sh[f"l{l}_w_down"] = f(f"l{l}_w_down")
        sh[f"l{l}_w_out"] = f(f"l{l}_w_out")
    sh["l0_w_in"] = f("l0_w_in")
    sh["l0_q_norm_g"] = f("l0_q_norm_g").reshape(128, 1)
    sh["l0_k_norm_g"] = f("l0_k_norm_g").reshape(128, 1)
    dw = f("l0_dw_w").reshape(31, 8, 128)
    sh["l0_dwT"] = np.ascontiguousarray(dw.transpose(2, 1, 0))
    sh["l0_cvec"] = np.ascontiguousarray(np.stack([_colmajor(inputs["l0_dw_b"], 8), _colmajor(inputs["l0_conv_ln_g"], 8),
                                                   _colmajor(inputs["l0_conv_ln_b"], 8)], axis=1))
    sh["l1_w_dqkv"] = f("l1_w_dqkv")
    sh["l1_gq"] = _colmajor(inputs["l1_q_lora_norm_g"], 12)
    sh["l1_w_uq"] = f("l1_w_uq")
    sh["l1_gkv"] = _colmajor(inputs["l1_kv_lora_norm_g"], 4)
    sh["l1_w_ukv"] = f("l1_w_ukv")
    sh["final_norm_g"] = f("final_norm_g").reshape(1, -1)
    return sh


def _core_map(inputs, sh, b):
    m = dict(sh)
    m["x"] = np.ascontiguousarray(np.asarray(inputs["x"][b], np.float32))
    m["ctx"] = np.ascontiguousarray(np.asarray(inputs["ctx"][b], np.float32))
    cc = np.stack([np.asarray(inputs["c"][b], np.float32), np.asarray(inputs["c_ctx"], np.float32)], axis=-1)
    m["cT"] = np.ascontiguousarray(cc.reshape(16, 128, 2).transpose(1, 0, 2))
    return m


def kernel(**inputs):
    nc = K().build()
    sh = _shared(inputs)
    nb = np.asarray(inputs["x"]).shape[0]
    in_maps = [_core_map(inputs, sh, b) for b in range(nb)]
    res = run_bass_kernel_spmd(nc, in_maps, core_ids=list(range(nb)))
    return np.stack([np.asarray(r["out"], np.float32) for r in res.results], axis=0)
```
